# Optimizing a Trainium2 kernel written in Bass

```python
import math
import jax, jax.numpy as jnp
from jax import lax
import numpy as np

D_MODEL = 1024
BATCH = 8
SEQ = 4096
DEPTH = 1

HEAD_DIM = 64
ROPE_DIMS = HEAD_DIM // 4
ROPE_THETA = 500000.0
NORM_EPS = 1e-6
NEG_INF = -1e30

NSA_HEADS = 8
NSA_KV_GROUPS = 2
NSA_Q_PER_KV = NSA_HEADS // NSA_KV_GROUPS
CMP_BLOCK = 32
CMP_STRIDE = 16
CMP_HIDDEN = 256
SLC_BLOCK = 64
SLC_TOPN = 16
FORCE_SCORE = 1e3
WIN = 512
NSA_QBLOCK = 64

DIL_PATTERNS = ((128, 1), (512, 4), (2048, 16))
DIL_GROUPS = len(DIL_PATTERNS)
DIL_HEADS_PER_GROUP = 4
DIL_WIDTH = DIL_HEADS_PER_GROUP * HEAD_DIM
DIL_BLOCK = 128

NSA_Q_COLS = NSA_HEADS * HEAD_DIM
NSA_KV_COLS = NSA_KV_GROUPS * HEAD_DIM
NSA_GATE_COLS = 3 * NSA_HEADS
DIL_COLS = DIL_GROUPS * 3 * DIL_WIDTH
MERGE_COLS = 2 * D_MODEL
IN_COLS = NSA_Q_COLS + 6 * NSA_KV_COLS + NSA_GATE_COLS + DIL_COLS + MERGE_COLS

PEER_HEADS = 8
PEER_NKEYS = 128
PEER_TOPK = 16
PEER_QDIM = 128
PEER_N_EXPERTS = PEER_NKEYS ** 2
PEER_CHUNK = 128

kernel_name = "hybrid_nsa_dilated_peer_block"


def rms_norm(z, g):
    zf = z.astype(jnp.float32)
    zf = zf * lax.rsqrt(jnp.mean(zf * zf, axis=-1, keepdims=True) + NORM_EPS)
    return (zf * g.astype(jnp.float32)).astype(z.dtype)


def rope(z, pos):
    half = ROPE_DIMS // 2
    inv = ROPE_THETA ** (-(jnp.arange(half, dtype=jnp.float32) * 2.0 / ROPE_DIMS))
    ang = pos.astype(jnp.float32)[:, None] * inv[None, :]
    cos = jnp.cos(ang)[None, :, None, :]
    sin = jnp.sin(ang)[None, :, None, :]
    zf = z.astype(jnp.float32)
    z1 = zf[..., :half]
    z2 = zf[..., half:ROPE_DIMS]
    out = jnp.concatenate([z1 * cos - z2 * sin, z1 * sin + z2 * cos, zf[..., ROPE_DIMS:]], axis=-1)
    return out.astype(z.dtype)


def compress_blocks(z, idx, pe, w1, w2):
    B, _, G, dh = z.shape
    blk = z[:, idx] + pe[None, None, :, None, :]
    n_cmp = idx.shape[0]
    flat = blk.transpose(0, 1, 3, 2, 4).reshape(B, n_cmp, G, CMP_BLOCK * dh)
    return jax.nn.gelu(flat @ w1) @ w2


def nsa_attention(q, k_c, v_c, k_s, v_s, k_w, v_w, gate_logits, q_norm, k_norm,
                  cmp_pe_k, cmp_w1_k, cmp_w2_k, cmp_pe_v, cmp_w1_v, cmp_w2_v):
    B, S, _ = q.shape
    G, HG, dh = NSA_KV_GROUPS, NSA_Q_PER_KV, HEAD_DIM
    scale = HEAD_DIM ** -0.5
    dtype = q.dtype
    pos = jnp.arange(S, dtype=jnp.int32)
    q = rope(rms_norm(q.reshape(B, S, NSA_HEADS, dh), q_norm), pos)
    shp = (B, S, G, dh)
    k_s = rope(rms_norm(k_s.reshape(shp), k_norm[1]), pos)
    k_w = rope(rms_norm(k_w.reshape(shp), k_norm[2]), pos)
    v_s = v_s.reshape(shp)
    v_w = v_w.reshape(shp)

    n_cmp = (S - CMP_BLOCK) // CMP_STRIDE + 1
    starts = jnp.arange(n_cmp, dtype=jnp.int32) * CMP_STRIDE
    idx = starts[:, None] + jnp.arange(CMP_BLOCK, dtype=jnp.int32)[None, :]
    cmp_end = starts + CMP_BLOCK - 1
    k_cmp = compress_blocks(k_c.reshape(shp), idx, cmp_pe_k, cmp_w1_k, cmp_w2_k)
    v_cmp = compress_blocks(v_c.reshape(shp), idx, cmp_pe_v, cmp_w1_v, cmp_w2_v)
    k_cmp = rope(rms_norm(k_cmp, k_norm[0]), cmp_end)

    n_slc = S // SLC_BLOCK
    n_sel = min(SLC_TOPN, n_slc)
    s0 = np.arange(n_cmp) * CMP_STRIDE
    s1 = s0 + CMP_BLOCK
    b0 = np.arange(n_slc) * SLC_BLOCK
    b1 = b0 + SLC_BLOCK
    ov = np.clip(np.minimum(s1[:, None], b1[None, :]) - np.maximum(s0[:, None], b0[None, :]), 0, None) / CMP_BLOCK
    overlap = jnp.asarray(ov, dtype=jnp.float32)

    ks_blk = k_s.reshape(B, n_slc, SLC_BLOCK, G, dh).transpose(0, 3, 1, 2, 4)
    vs_blk = v_s.reshape(B, n_slc, SLC_BLOCK, G, dh).transpose(0, 3, 1, 2, 4)
    kw_pad = jnp.pad(k_w, ((0, 0), (WIN, 0), (0, 0), (0, 0)))
    vw_pad = jnp.pad(v_w, ((0, 0), (WIN, 0), (0, 0), (0, 0)))
    gates = jax.nn.sigmoid(gate_logits.astype(jnp.float32)).reshape(B, S, NSA_HEADS, 3)
    bi = jnp.arange(B)[:, None, None, None]
    gi = jnp.arange(G)[None, :, None, None]
    blk_ids = jnp.arange(n_slc, dtype=jnp.int32)
    QB = NSA_QBLOCK

    def body(n):
        t0 = n * QB
        t = t0 + jnp.arange(QB, dtype=jnp.int32)
        qb = lax.dynamic_slice_in_dim(q, t0, QB, axis=1).reshape(B, QB, G, HG, dh)
        sc = jnp.einsum('bqgjd,bcgd->bgjqc', qb, k_cmp).astype(jnp.float32) * scale
        valid_c = cmp_end[None, :] <= t[:, None]
        pc = jax.nn.softmax(jnp.where(valid_c, sc, NEG_INF), axis=-1)
        pc = jnp.where(valid_c, pc, 0.0)
        o_c = jnp.einsum('bgjqc,bcgd->bqgjd', pc.astype(dtype), v_cmp)
        imp = jnp.einsum('bgjqc,cn->bgqn', pc, overlap)
        cur = t // SLC_BLOCK
        forced = (blk_ids[None, :] == 0) | (blk_ids[None, :] == cur[:, None]) | (blk_ids[None, :] == cur[:, None] - 1)
        causal_b = blk_ids[None, :] * SLC_BLOCK <= t[:, None]
        score = jnp.where(forced, FORCE_SCORE, jnp.where(causal_b, imp, -1.0))
        _, sel = lax.top_k(score, n_sel)
        kg = ks_blk[bi, gi, sel]
        vg = vs_blk[bi, gi, sel]
        ss = jnp.einsum('bqgjd,bgqnkd->bgjqnk', qb, kg).astype(jnp.float32) * scale
        kpos = sel[..., None] * SLC_BLOCK + jnp.arange(SLC_BLOCK, dtype=jnp.int32)
        valid_s = kpos <= t[None, None, :, None, None]
        ss = jnp.where(valid_s[:, :, None], ss, NEG_INF).reshape(B, G, HG, QB, n_sel * SLC_BLOCK)
        ps = jax.nn.softmax(ss, axis=-1).reshape(B, G, HG, QB, n_sel, SLC_BLOCK)
        o_s = jnp.einsum('bgjqnk,bgqnkd->bqgjd', ps.astype(dtype), vg)
        kwb = lax.dynamic_slice_in_dim(kw_pad, t0, QB + WIN, axis=1)
        vwb = lax.dynamic_slice_in_dim(vw_pad, t0, QB + WIN, axis=1)
        kp = t0 - WIN + jnp.arange(QB + WIN, dtype=jnp.int32)
        dist = t[:, None] - kp[None, :]
        valid_w = (dist >= 0) & (dist < WIN) & (kp[None, :] >= 0)
        sw = jnp.einsum('bqgjd,bkgd->bgjqk', qb, kwb).astype(jnp.float32) * scale
        pw = jax.nn.softmax(jnp.where(valid_w, sw, NEG_INF), axis=-1)
        o_w = jnp.einsum('bgjqk,bkgd->bqgjd', pw.astype(dtype), vwb)
        gb = lax.dynamic_slice_in_dim(gates, t0, QB, axis=1).reshape(B, QB, G, HG, 3)
        o = gb[..., 0:1] * o_c + gb[..., 1:2] * o_s + gb[..., 2:3] * o_w
        return o.reshape(B, QB, NSA_HEADS * dh).astype(dtype)

    out = lax.map(body, jnp.arange(S // QB, dtype=jnp.int32))
    return out.transpose(1, 0, 2, 3).reshape(B, S, NSA_HEADS * dh)


def dilated_group(q, k, v, window, dilation):
    B, S, H, dh = q.shape
    scale = HEAD_DIM ** -0.5
    n_back = window // dilation
    L = -(-S // dilation)
    L = -(-L // DIL_BLOCK) * DIL_BLOCK
    Sp = L * dilation
    nb = L // DIL_BLOCK
    pad = ((0, 0), (0, Sp - S), (0, 0), (0, 0))

    def to_blocks(z):
        z = jnp.pad(z, pad).reshape(B, L, dilation, H, dh).transpose(0, 2, 3, 1, 4)
        return z.reshape(B, dilation, H, nb, DIL_BLOCK, dh)

    def with_prev(z):
        prev = jnp.pad(z[:, :, :, :-1], ((0, 0), (0, 0), (0, 0), (1, 0), (0, 0), (0, 0)))
        return jnp.concatenate([prev, z], axis=4)

    qb = to_blocks(q)
    kk = with_prev(to_blocks(k))
    vv = with_prev(to_blocks(v))
    s = jnp.einsum('brhnqd,brhnkd->brhnqk', qb, kk).astype(jnp.float32) * scale
    blk = jnp.arange(nb, dtype=jnp.int32)[:, None]
    pq = blk * DIL_BLOCK + jnp.arange(DIL_BLOCK, dtype=jnp.int32)[None, :]
    pk = (blk - 1) * DIL_BLOCK + jnp.arange(2 * DIL_BLOCK, dtype=jnp.int32)[None, :]
    dist = pq[:, :, None] - pk[:, None, :]
    valid = (dist >= 0) & (dist <= n_back) & (pk[:, None, :] >= 0)
    s = jnp.where(valid, s, NEG_INF)
    m = jnp.max(s, axis=-1, keepdims=True)
    e = jnp.exp(s - m)
    den = jnp.sum(e, axis=-1)
    o = jnp.einsum('brhnqk,brhnkd->brhnqd', e, vv.astype(jnp.float32)) / den[..., None]
    lse = m[..., 0] + jnp.log(den)
    o = o.reshape(B, dilation, H, L, dh).transpose(0, 3, 1, 2, 4).reshape(B, Sp, H, dh)[:, :S]
    lse = lse.reshape(B, dilation, H, L).transpose(0, 3, 1, 2).reshape(B, Sp, H)[:, :S]
    return o, lse


def dilated_attention(d_cols, q_norm, k_norm):
    B, S, _ = d_cols.shape
    pos = jnp.arange(S, dtype=jnp.int32)
    d = d_cols.reshape(B, S, DIL_GROUPS, 3, DIL_HEADS_PER_GROUP, HEAD_DIM)
    outs, lses = [], []
    for g, (window, dilation) in enumerate(DIL_PATTERNS):
        qg = rope(rms_norm(d[:, :, g, 0], q_norm[g]), pos)
        kg = rope(rms_norm(d[:, :, g, 1], k_norm[g]), pos)
        o, lse = dilated_group(qg, kg, d[:, :, g, 2], window, dilation)
        outs.append(o)
        lses.append(lse)
    alpha = jax.nn.softmax(jnp.stack(lses, axis=0), axis=0)
    o = jnp.sum(alpha[..., None] * jnp.stack(outs, axis=0), axis=0)
    return o.reshape(B, S, DIL_WIDTH).astype(d_cols.dtype)


def peer(hn, w_q, subkeys, u, v):
    B, S, D = hn.shape
    T = B * S
    H, K = PEER_HEADS, PEER_TOPK
    xt = hn.reshape(T, D)
    q = (xt @ w_q).reshape(T, H, 2, PEER_QDIM // 2)
    s = jnp.einsum('thcd,cnd->thcn', q, subkeys).astype(jnp.float32)
    s1, i1 = lax.top_k(s[:, :, 0], K)
    s2, i2 = lax.top_k(s[:, :, 1], K)
    cand = (s1[..., :, None] + s2[..., None, :]).reshape(T, H, K * K)
    sc, ci = lax.top_k(cand, K)
    e1 = jnp.take_along_axis(i1, ci // K, axis=-1)
    e2 = jnp.take_along_axis(i2, ci % K, axis=-1)
    idx = e1 * PEER_NKEYS + e2
    g = jax.nn.softmax(sc, axis=-1)
    C = PEER_CHUNK
    nc = T // C

    def body(args):
        xc, ic, gc = args
        a = jnp.einsum('cd,chkd->chk', xc, u[ic])
        act = (jax.nn.gelu(a.astype(jnp.float32)) * gc).astype(xc.dtype)
        return jnp.einsum('chk,chkd->cd', act, v[ic])

    out = lax.map(body, (xt.reshape(nc, C, D), idx.reshape(nc, C, H, K), g.reshape(nc, C, H, K)))
    return out.reshape(B, S, D)


def setup_inputs(seed: int = 0) -> dict:
    key = jax.random.key(seed)
    ks = jax.random.split(key, 24)
    f32 = jnp.float32

    def nrm(k, shape, scale):
        return jax.random.normal(k, shape, f32) * scale

    def gain(k, shape):
        return 1.0 + 0.02 * jax.random.normal(k, shape, f32)

    flat = CMP_BLOCK * HEAD_DIM
    nsa_w = NSA_HEADS * HEAD_DIM
    return {
        "x": nrm(ks[0], (BATCH, SEQ, D_MODEL), 1.0),
        "norm1_g": gain(ks[1], (D_MODEL,)),
        "w_in": nrm(ks[2], (D_MODEL, IN_COLS), D_MODEL ** -0.5),
        "nsa_q_norm": gain(ks[3], (HEAD_DIM,)),
        "nsa_k_norm": gain(ks[4], (3, HEAD_DIM)),
        "cmp_pe_k": nrm(ks[5], (CMP_BLOCK, HEAD_DIM), 0.1),
        "cmp_w1_k": nrm(ks[6], (flat, CMP_HIDDEN), flat ** -0.5),
        "cmp_w2_k": nrm(ks[7], (CMP_HIDDEN, HEAD_DIM), CMP_HIDDEN ** -0.5),
        "cmp_pe_v": nrm(ks[8], (CMP_BLOCK, HEAD_DIM), 0.1),
        "cmp_w1_v": nrm(ks[9], (flat, CMP_HIDDEN), flat ** -0.5),
        "cmp_w2_v": nrm(ks[10], (CMP_HIDDEN, HEAD_DIM), CMP_HIDDEN ** -0.5),
        "dil_q_norm": gain(ks[11], (DIL_GROUPS, HEAD_DIM)),
        "dil_k_norm": gain(ks[12], (DIL_GROUPS, HEAD_DIM)),
        "w_up_nsa": nrm(ks[13], (nsa_w, D_MODEL), nsa_w ** -0.5),
        "w_up_dil": nrm(ks[14], (DIL_WIDTH, D_MODEL), DIL_WIDTH ** -0.5),
        "w_o": nrm(ks[15], (D_MODEL, D_MODEL), D_MODEL ** -0.5),
        "norm2_g": gain(ks[16], (D_MODEL,)),
        "peer_wq": nrm(ks[17], (D_MODEL, PEER_HEADS * PEER_QDIM), D_MODEL ** -0.5),
        "peer_subkeys": nrm(ks[18], (2, PEER_NKEYS, PEER_QDIM // 2), (PEER_QDIM // 2) ** -0.5),
        "peer_u": nrm(ks[19], (PEER_N_EXPERTS, D_MODEL), D_MODEL ** -0.5),
        "peer_v": nrm(ks[20], (PEER_N_EXPERTS, D_MODEL), PEER_HEADS ** -0.5),
    }


def reference(x, norm1_g, w_in, nsa_q_norm, nsa_k_norm, cmp_pe_k, cmp_w1_k, cmp_w2_k,
              cmp_pe_v, cmp_w1_v, cmp_w2_v, dil_q_norm, dil_k_norm, w_up_nsa, w_up_dil,
              w_o, norm2_g, peer_wq, peer_subkeys, peer_u, peer_v):
    B, S, D = x.shape
    sizes = [NSA_Q_COLS] + [NSA_KV_COLS] * 6 + [NSA_GATE_COLS, DIL_COLS, MERGE_COLS]
    splits = [int(c) for c in np.cumsum(sizes)[:-1]]
    for _ in range(DEPTH):
        h = rms_norm(x, norm1_g)
        z = jnp.einsum('bsd,dc->bsc', h, w_in)
        q_n, kc, vc, ksl, vsl, kwn, vwn, g_nsa, dil, mg = jnp.split(z, splits, axis=-1)
        y_nsa = nsa_attention(q_n, kc, vc, ksl, vsl, kwn, vwn, g_nsa, nsa_q_norm, nsa_k_norm,
                              cmp_pe_k, cmp_w1_k, cmp_w2_k, cmp_pe_v, cmp_w1_v, cmp_w2_v)
        y_dil = dilated_attention(dil, dil_q_norm, dil_k_norm)
        gm = jax.nn.sigmoid(mg.astype(jnp.float32)).reshape(B, S, 2, D)
        merged = gm[:, :, 0] * (y_nsa @ w_up_nsa) + gm[:, :, 1] * (y_dil @ w_up_dil)
        x = x + merged.astype(x.dtype) @ w_o
        x = x + peer(rms_norm(x, norm2_g), peer_wq, peer_subkeys, peer_u, peer_v)
    return x
```

```python
import contextlib
import math
import numpy as np
import ml_dtypes
import concourse.bass as bass
import concourse.mybir as mybir
from concourse.bass_utils import run_bass_kernel_spmd

F32 = mybir.dt.float32
BF16 = mybir.dt.bfloat16
ALU = mybir.AluOpType
AF = mybir.ActivationFunctionType
AX = mybir.AxisListType

S = 4096
D = 1024
NT = S // 128
NB = S // 512
EPS = 1e-6


class Slot:
    __slots__ = ("name", "writers", "readers", "dcount")

    def __init__(self, name):
        self.name = name
        self.writers = {}
        self.readers = {}
        self.dcount = 0


class Op:
    __slots__ = ("eng", "fn", "deps", "signal", "value", "key", "is_dma")

    def __init__(self, eng, fn, key, is_dma=False, value=None):
        self.eng = eng
        self.fn = fn
        self.deps = []
        self.signal = False
        self.value = value
        self.key = key
        self.is_dma = is_dma


class Prog:
    COMPUTE = ("pe", "act", "dve", "pool")

    def __init__(self, nc):
        self.nc = nc
        self.ops = {e: [] for e in ("pe", "act", "dve", "pool", "sp")}
        self.slots = {}
        self.last = {}
        self.fence_deps = {e: [] for e in self.ops}
        self.n_ops = 0

    def slot(self, name):
        s = self.slots.get(name)
        if s is None:
            s = self.slots[name] = Slot(name)
        return s

    def _track(self, op, reads, writes):
        deps = op.deps
        fd = self.fence_deps[op.eng]
        if fd:
            deps.extend(fd)
            self.fence_deps[op.eng] = []
        for r in reads:
            s = self.slot(r)
            deps.extend(s.writers.values())
            s.readers[op.key] = op
        for w in writes:
            s = self.slot(w)
            deps.extend(s.readers.values())
            deps.extend(s.writers.values())
            if any(o is not op for o in s.readers.values()):
                s.writers = {op.key: op}
                s.readers = {}
            else:
                s.readers = {}
                s.writers[op.key] = op
        op.deps = [d for d in deps if d is not op and (d.key != op.key or (not op.is_dma and op.eng != "pe"))]
        self.last[op.key] = op
        self.n_ops += 1

    def add(self, eng, fn, reads=(), writes=()):
        op = Op(eng, fn, eng)
        self.ops[eng].append(op)
        self._track(op, reads, writes)
        return op

    def dma(self, fn, reads=(), writes=(), queue="sp"):
        assert len(writes) == 1
        s = self.slot(writes[0])
        s.dcount += 1
        op = Op(queue, fn, ("d", s.name), is_dma=True, value=16 * s.dcount)
        self.ops[queue].append(op)
        self._track(op, reads, writes)
        return op

    def fence(self):
        allops = list(self.last.values())
        for e in self.fence_deps:
            self.fence_deps[e] = list(allops)

    def emit(self, final_slots=()):
        nc = self.nc
        fin = Op("sp", None, "fin")
        for name in final_slots:
            fin.deps.extend(self.slot(name).writers.values())
        self.ops["sp"].append(fin)
        for e, lst in self.ops.items():
            for op in lst:
                for d in op.deps:
                    if not d.is_dma:
                        d.signal = True
        for e in self.COMPUTE:
            c = 0
            for op in self.ops[e]:
                if op.signal and not op.is_dma:
                    c += 1
                    op.value = c
        keys = list(self.COMPUTE)
        for lst in self.ops.values():
            for op in lst:
                if op.is_dma and op.key not in keys:
                    keys.append(op.key)
        self.n_sems = len(keys)
        with contextlib.ExitStack() as st:
            sems = {}
            for i, k in enumerate(keys):
                sems[k] = st.enter_context(nc.semaphore("s%d" % i))
            block = st.enter_context(nc.Block())

            def run(eng_name, eng):
                waited = {}
                for op in self.ops[eng_name]:
                    need = {}
                    for d in op.deps:
                        v = d.value
                        if v > need.get(d.key, 0):
                            need[d.key] = v
                    for k, v in need.items():
                        if waited.get(k, 0) < v:
                            eng.wait_ge(sems[k], v)
                            waited[k] = v
                    if op.fn is None:
                        continue
                    ins = op.fn(eng)
                    if op.is_dma:
                        ins.then_inc(sems[op.key], 16)
                    elif op.signal:
                        ins.then_inc(sems[op.key], 1)

            @block.sync
            def _(eng):
                run("sp", eng)

            @block.tensor
            def _(eng):
                run("pe", eng)

            @block.scalar
            def _(eng):
                run("act", eng)

            @block.vector
            def _(eng):
                run("dve", eng)

            @block.gpsimd
            def _(eng):
                run("pool", eng)


class Arena:
    def __init__(self, t, total_f32):
        self.t = t
        self.total = total_f32
        self.off = 0
        self.peak = 0

    def mark(self):
        return self.off

    def release(self, m):
        self.off = m

    def alloc(self, cols, dtype=F32):
        n32 = cols if dtype == F32 else (cols + 1) // 2
        a = self.off
        self.off += n32
        self.peak = max(self.peak, self.off)
        assert self.off <= self.total, ("SBUF arena overflow", self.off, self.total)
        v = self.t[:, a:a + n32]
        if dtype != F32:
            v = v.bitcast(dtype)[:, 0:cols]
        return v


class Ring:
    def __init__(self, items):
        self.items = list(items)
        self.i = 0

    def next(self):
        r = self.items[self.i % len(self.items)]
        self.i += 1
        return r


CID_Q = 0
CID_KC = 4
CID_VC = 5
CID_KS = 6
CID_KW = 8
CID_DIL = 10
NFM = 22
NTM = 1048
DIL_PAT = ((128, 1), (512, 4), (2048, 16))


def build_program(debug=False, stop_after=None):
    nc = bass.Bass("TRN2", target_bir_lowering=False)

    def din(name, shape, dt=F32):
        return nc.dram_tensor(name, list(shape), dt, kind="ExternalInput").ap()

    skind = "ExternalOutput" if debug else "Internal"

    def dscr(name, shape, dt):
        return nc.dram_tensor(name, list(shape), dt, kind=skind).ap()

    x_d = din("x", [S, D])
    wfm_d = din("wfm", [D, NFM * 128])
    wtm_d = din("wtm", [D, NTM])
    wmg_d = din("wmg", [D, 2048])
    g1_d = din("g1", [128, 8])
    g2_d = din("g2", [128, 8])
    gains_d = din("gains", [128, NFM + 1])
    w1k_d = din("w1k", [2048, 256])
    w1v_d = din("w1v", [2048, 256])
    w2k_d = din("w2k", [256, 64])
    w2v_d = din("w2v", [256, 64])
    pek_d = din("pek", [128, 16])
    pev_d = din("pev", [128, 16])
    wun_d = din("wun", [512, D])
    wud_d = din("wud", [256, D])
    wo_d = din("wo", [D, D])
    wq_d = din("wq", [D, D])
    skbd_d = din("skbd", [128, 256])
    ut_d = din("ut", [128, 128, 1024])
    v_d = din("pv", [128, 128, 1024])
    ident_d = din("ident", [128, 128], BF16)
    bd_d = din("bdones", [128, 128], BF16)
    rot_d = din("rotT", [128, 128], BF16)
    cos_d = din("cosT", [128, S])
    sin_d = din("sinT", [128, S])
    cosc_d = din("cosC", [128, 256])
    sinc_d = din("sinC", [128, 256])
    cmask_d = din("cmask", [2, 128, S], BF16)
    masks_d = din("masks", [128, 9, 128], BF16)
    ov_d = din("ov", [2, 128, 64], BF16)
    selmul_d = din("selmul", [S, 64])
    seladd_d = din("seladd", [S, 64])
    esel_d = din("esel", [64, S], BF16)
    out_d = nc.dram_tensor("out", [S, D], F32, kind="ExternalOutput").ap()

    qkt_s = dscr("qkt_s", [NFM, 128, S], BF16)
    vtm_s = dscr("vtm_s", [S, 1024], BF16)
    gates_s = dscr("gates_s", [S, 24], F32)
    ht_s = dscr("ht_s", [8, 128, S], BF16)
    yt_s = dscr("yt_s", [6, 128, S], BF16)
    x2_s = dscr("x2_s", [S, D], F32)
    h2t_s = dscr("h2t_s", [8, 128, S], BF16)
    utb_s = dscr("utb_s", [32, 128, 4096], BF16)
    vb_s = dscr("vb_s", [32, 128, 4096], BF16)
    wt_s = dscr("wt_s", [2, 32, 128, 1024], BF16)

    TOT = 53100
    with contextlib.ExitStack() as st:
        at = st.enter_context(nc.sbuf_tensor("arena", [128, TOT], F32))
        pst = [st.enter_context(nc.psum_tensor("ps%d" % i, [128, 512], F32)) for i in range(8)]
        ps = [t.ap() for t in pst]
        psb = [t.ap().bitcast(BF16) for t in pst]
        A = Arena(at, TOT)
        P = Prog(nc)
        uid = [0]

        def nm(prefix):
            uid[0] += 1
            return "%s_%d" % (prefix, uid[0])

        def mkring(prefix, n, cols, dtype=F32):
            return Ring([(nm(prefix), A.alloc(cols, dtype)) for _ in range(n)])

        ident = A.alloc(128, BF16)
        bdones = A.alloc(128, BF16)
        rotT = A.alloc(128, BF16)
        masks = A.alloc(9 * 128, BF16).rearrange("p (a b) -> p a b", a=9)
        P.dma(lambda e: e.dma_start(out=ident, in_=ident_d), writes=["ident"])
        P.dma(lambda e: e.dma_start(out=bdones, in_=bd_d), writes=["bdones"])
        P.dma(lambda e: e.dma_start(out=rotT, in_=rot_d), writes=["rotT"])
        P.dma(lambda e: e.dma_start(out=masks, in_=masks_d), writes=["masks"])
        gains = A.alloc(NFM + 1)
        P.dma(lambda e: e.dma_start(out=gains, in_=gains_d), writes=["gains"])
        persist_mark = A.mark()

        def norm_rope(zps_name, zps, n, gain_ap, cos_ap, sin_ap, cos_slots, out_name, out_ap, R):
            sqn, sq = R["sq"].next()
            P.add("act", lambda e: e.activation(out=sq[:, 0:n], in_=zps, func=AF.Square), reads=[zps_name], writes=[sqn])
            P.add("pe", lambda e: e.matmul(ps[3][:, 0:n], lhsT=bdones, rhs=sq[:, 0:n], start=True, stop=True), reads=[sqn, "bdones"], writes=["ps3"])
            rsn, rs = R["rs"].next()
            P.add("act", lambda e: e.activation(out=rs[:, 0:n], in_=ps[3][:, 0:n], func=AF.Sqrt, scale=1.0 / 64, bias=EPS), reads=["ps3"], writes=[rsn])
            P.add("dve", lambda e: e.reciprocal(out=rs[:, 0:n], in_=rs[:, 0:n]), reads=[rsn], writes=[rsn])
            znn, zn = R["zn"].next()
            P.add("dve", lambda e: e.scalar_tensor_tensor(out=zn[:, 0:n], in0=zps, scalar=gain_ap, in1=rs[:, 0:n], op0=ALU.mult, op1=ALU.mult), reads=[zps_name, rsn, "gains"], writes=[znn])
            zbn, zb = R["zb"].next()
            P.add("act", lambda e: e.activation(out=zb[:, 0:n], in_=zn[:, 0:n], func=AF.Copy), reads=[znn], writes=[zbn])
            P.add("pe", lambda e: e.matmul(ps[4][:, 0:n], lhsT=rotT, rhs=zb[:, 0:n], start=True, stop=True), reads=[zbn, "rotT"], writes=["ps4"])
            P.add("dve", lambda e: e.tensor_tensor(out=zn[:, 0:n], in0=zn[:, 0:n], in1=cos_ap, op=ALU.mult), reads=[znn] + cos_slots, writes=[znn])
            t2n, t2 = R["t2"].next()
            P.add("dve", lambda e: e.tensor_tensor(out=t2[:, 0:n], in0=ps[4][:, 0:n], in1=sin_ap, op=ALU.mult), reads=["ps4"] + cos_slots, writes=[t2n])
            P.add("dve", lambda e: e.tensor_tensor(out=out_ap, in0=zn[:, 0:n], in1=t2[:, 0:n], op=ALU.add), reads=[znn, t2n], writes=[out_name])

        m0 = A.mark()
        stg = mkring("pstg", 2, 4096)
        stb = mkring("pstb", 2, 4096, BF16)
        k = 0
        for src, dst in ((ut_d, utb_s), (v_d, vb_s)):
            for i in range(32):
                sn, sa = stg.next()
                bn, ba = stb.next()
                P.dma(lambda e, sa=sa, src=src, i=i: e.dma_start(out=sa.rearrange("p (a b) -> p a b", a=4), in_=src[4 * i:4 * i + 4].rearrange("a p c -> p a c")), writes=[sn])
                eng = ("dve", "act", "pool")[k % 3]
                k += 1
                if eng == "act":
                    P.add("act", lambda e, sa=sa, ba=ba: e.activation(out=ba, in_=sa, func=AF.Copy), reads=[sn], writes=[bn])
                else:
                    P.add(eng, lambda e, sa=sa, ba=ba: e.tensor_copy(out=ba, in_=sa), reads=[sn], writes=[bn])
                P.dma(lambda e, ba=ba, dst=dst, i=i: e.dma_start(out=dst[i], in_=ba), reads=[bn], writes=["utb_s" if dst is utb_s else "vb_s"], queue="pool")
        A.release(m0)
        P.fence()

        m0 = A.mark()
        wfm = A.alloc(8 * NFM * 128, BF16).rearrange("p (a b) -> p a b", a=8)
        wtm = A.alloc(8 * NTM, BF16).rearrange("p (a b) -> p a b", a=8)
        g1t = A.alloc(8)
        cosT = A.alloc(S)
        sinT = A.alloc(S)
        P.dma(lambda e: e.dma_start(out=g1t, in_=g1_d), writes=["g1t"])
        P.dma(lambda e: e.dma_start(out=cosT, in_=cos_d), writes=["cosT"])
        P.dma(lambda e: e.dma_start(out=sinT, in_=sin_d), writes=["sinT"])
        m1 = A.mark()
        wst = mkring("wst", 2, NFM * 128)
        for kc in range(8):
            sn, sa = wst.next()
            P.dma(lambda e, sa=sa, kc=kc: e.dma_start(out=sa, in_=wfm_d[kc * 128:(kc + 1) * 128, :]), writes=[sn])
            P.add("dve", lambda e, sa=sa, kc=kc: e.tensor_scalar(out=wfm[:, kc, :], in0=sa, scalar1=g1t[:, kc:kc + 1], scalar2=None, op0=ALU.mult), reads=[sn, "g1t"], writes=["wfm"])
        for kc in range(8):
            sn, sa = wst.next()
            P.dma(lambda e, sa=sa, kc=kc: e.dma_start(out=sa[:, 0:NTM], in_=wtm_d[kc * 128:(kc + 1) * 128, :]), writes=[sn])
            P.add("dve", lambda e, sa=sa, kc=kc: e.tensor_scalar(out=wtm[:, kc, :], in0=sa[:, 0:NTM], scalar1=g1t[:, kc:kc + 1], scalar2=None, op0=ALU.mult), reads=[sn, "g1t"], writes=["wtm"])
        A.release(m1)
        P.fence()
        xr = mkring("xt", 2, 1024)
        junk = A.alloc(1024)
        ssr = mkring("ss", 2, 1)
        hbr = mkring("hb", 2, 1024, BF16)
        hTr = mkring("hTb", 2, 8 * 512, BF16)
        R = {"sq": mkring("sq", 2, 512, BF16), "rs": mkring("rs", 2, 512), "zn": mkring("zn", 2, 512),
             "zb": mkring("zb", 2, 512, BF16), "t2": mkring("t2", 2, 512)}
        fmo = mkring("fmo", 3, 512, BF16)
        tmo = mkring("tmo", 2, 1024, BF16)
        gto = mkring("gto", 2, 24)
        fmps = Ring([1, 2])
        for b in range(NB):
            hTn, hTb_ = hTr.next()
            hTb = hTb_.rearrange("p (a b) -> p a b", a=8)
            c0 = b * 512
            for u in range(4):
                t0 = c0 + u * 128
                xn, xt = xr.next()
                P.dma(lambda e, xt=xt, t0=t0: e.dma_start(out=xt, in_=x_d[t0:t0 + 128, :]), writes=[xn])
                sn, ss = ssr.next()
                P.add("act", lambda e, xt=xt, ss=ss: e.activation(out=junk, in_=xt, func=AF.Square, accum_out=ss), reads=[xn], writes=["junk", sn])
                P.add("act", lambda e, ss=ss: e.activation(out=ss, in_=ss, func=AF.Sqrt, scale=1.0 / D, bias=EPS), reads=[sn], writes=[sn])
                P.add("dve", lambda e, ss=ss: e.reciprocal(out=ss, in_=ss), reads=[sn], writes=[sn])
                hn_, hb = hbr.next()
                P.add("dve", lambda e, xt=xt, ss=ss, hb=hb: e.tensor_scalar(out=hb, in0=xt, scalar1=ss, scalar2=None, op0=ALU.mult), reads=[xn, sn], writes=[hn_])
                for c in range(8):
                    P.add("pe", lambda e, c=c, hb=hb: e.transpose(out=psb[0][:, c * 128:(c + 1) * 128], in_=hb[:, c * 128:(c + 1) * 128], identity=ident), reads=[hn_, "ident"], writes=["ps0"])
                P.add("act", lambda e, u=u, hTb=hTb: e.activation(out=hTb[:, :, u * 128:(u + 1) * 128], in_=psb[0].rearrange("p (a b) -> p a b", a=8), func=AF.Copy), reads=["ps0"], writes=[hTn])
            P.dma(lambda e, hTb=hTb, c0=c0: e.dma_start(out=ht_s[:, :, c0:c0 + 512].rearrange("c p t -> p c t"), in_=hTb), reads=[hTn], writes=["ht_s"], queue="pool")
            for cid in range(NFM):
                pi = fmps.next()
                for kc in range(8):
                    P.add("pe", lambda e, pi=pi, kc=kc, cid=cid, hTb=hTb: e.matmul(ps[pi], lhsT=wfm[:, kc, cid * 128:(cid + 1) * 128], rhs=hTb[:, kc, :], start=(kc == 0), stop=(kc == 7)), reads=["wfm", hTn], writes=["ps%d" % pi])
                on, oa = fmo.next()
                if cid in (CID_KC, CID_VC):
                    P.add("act", lambda e, pi=pi, oa=oa: e.activation(out=oa, in_=ps[pi], func=AF.Copy), reads=["ps%d" % pi], writes=[on])
                else:
                    norm_rope("ps%d" % pi, ps[pi], 512, gains[:, cid:cid + 1], cosT[:, c0:c0 + 512], sinT[:, c0:c0 + 512], ["cosT", "sinT"], on, oa, R)
                P.dma(lambda e, oa=oa, cid=cid, c0=c0: e.dma_start(out=qkt_s[cid, :, c0:c0 + 512], in_=oa), reads=[on], writes=["qkt_s"], queue="pool")
            for u in range(4):
                t0 = c0 + u * 128
                for r, (a0, a1) in enumerate(((0, 512), (512, 1024), (1024, NTM))):
                    for kc in range(8):
                        P.add("pe", lambda e, r=r, kc=kc, u=u, a0=a0, a1=a1, hTb=hTb: e.matmul(ps[5 + r][:, 0:a1 - a0], lhsT=hTb[:, kc, u * 128:(u + 1) * 128], rhs=wtm[:, kc, a0:a1], start=(kc == 0), stop=(kc == 7)), reads=["wtm", hTn], writes=["ps%d" % (5 + r)])
                tn, ta = tmo.next()
                P.add("act", lambda e, ta=ta: e.activation(out=ta[:, 0:512], in_=ps[5], func=AF.Copy), reads=["ps5"], writes=[tn])
                P.add("dve", lambda e, ta=ta: e.tensor_copy(out=ta[:, 512:1024], in_=ps[6]), reads=["ps6"], writes=[tn])
                P.dma(lambda e, ta=ta, t0=t0: e.dma_start(out=vtm_s[t0:t0 + 128, :], in_=ta), reads=[tn], writes=["vtm_s"], queue="pool")
                gn, ga = gto.next()
                P.add("act", lambda e, ga=ga: e.activation(out=ga, in_=ps[7][:, 0:24], func=AF.Sigmoid), reads=["ps7"], writes=[gn])
                P.dma(lambda e, ga=ga, t0=t0: e.dma_start(out=gates_s[t0:t0 + 128, :], in_=ga), reads=[gn], writes=["gates_s"], queue="pool")
        A.release(m0)
        P.fence()
        if stop_after == "A":
            P.emit(final_slots=["qkt_s", "vtm_s", "gates_s", "ht_s", "utb_s", "vb_s"])
            return nc, P, A

        mB = A.mark()
        kcmp = A.alloc(2 * 256, BF16).rearrange("p (g c) -> p g c", g=2)
        vc1 = A.alloc(2 * 2 * 129, BF16).rearrange("p (t g c) -> p t g c", t=2, g=2)
        P.add("dve", lambda e: e.memset(kcmp, 0.0), writes=["kcmp"])
        P.add("dve", lambda e: e.memset(vc1, 0.0), writes=["vc1"])
        P.add("dve", lambda e: e.memset(vc1[:, 0, :, 64:65], 1.0), writes=["vc1"])
        P.add("dve", lambda e: e.memset(vc1[0:127, 1, :, 64:65], 1.0), writes=["vc1"])
        for ct in range(2):
            for g in range(2):
                P.dma(lambda e, ct=ct, g=g: e.dma_start(out=vc1[:, ct, g, 65:129], in_=ov_d[ct]), writes=["vc1"])
        mB1 = A.mark()
        x2c = A.alloc(2 * 2 * S, BF16).rearrange("p (k g t) -> p k g t", k=2, g=2)
        P.add("pool", lambda e: e.memset(x2c[:, :, :, S - 1:S], 0.0), writes=["x2c"])
        for kv in range(2):
            for g in range(2):
                P.dma(lambda e, kv=kv, g=g: e.dma_start(out=x2c[0:64, kv, g, :], in_=qkt_s[CID_KC + kv, g * 64:(g + 1) * 64, :]), reads=["qkt_s"], writes=["x2c"])
                P.dma(lambda e, kv=kv, g=g: e.dma_start(out=x2c[64:128, kv, g, 0:S - 1], in_=qkt_s[CID_KC + kv, g * 64:(g + 1) * 64, 1:S]), reads=["qkt_s"], writes=["x2c"])
        w1s = A.alloc(16 * 256).rearrange("p (a h) -> p a h", a=16)
        w1b = A.alloc(2 * 16 * 256, BF16).rearrange("p (k a h) -> p k a h", k=2, a=16)
        pes = A.alloc(32)
        peb = A.alloc(32, BF16)
        w2s = A.alloc(2 * 2 * 64).rearrange("p (k c d) -> p k c d", k=2, c=2)
        w2kd = A.alloc(2 * 128, BF16).rearrange("p (c d) -> p c d", c=2)
        w2vb = A.alloc(2 * 64, BF16).rearrange("p (c d) -> p c d", c=2)
        cosC = A.alloc(256)
        sinC = A.alloc(256)
        P.dma(lambda e: e.dma_start(out=cosC, in_=cosc_d), writes=["cosC"])
        P.dma(lambda e: e.dma_start(out=sinC, in_=sinc_d), writes=["sinC"])
        P.dma(lambda e: e.dma_start(out=pes[:, 0:16], in_=pek_d), writes=["pes"])
        P.dma(lambda e: e.dma_start(out=pes[:, 16:32], in_=pev_d), writes=["pes"])
        P.add("dve", lambda e: e.tensor_copy(out=peb, in_=pes), reads=["pes"], writes=["peb"])
        for kv, (w1d, w2d) in enumerate(((w1k_d, w2k_d), (w1v_d, w2v_d))):
            P.dma(lambda e, w1d=w1d: e.dma_start(out=w1s, in_=w1d.rearrange("(a p) h -> p a h", p=128)), writes=["w1s"])
            P.add("dve", lambda e, kv=kv: e.tensor_copy(out=w1b[:, kv], in_=w1s), reads=["w1s"], writes=["w1b"])
            P.dma(lambda e, kv=kv, w2d=w2d: e.dma_start(out=w2s[:, kv], in_=w2d.rearrange("(c p) d -> p c d", p=128)), writes=["w2s"])
        P.add("dve", lambda e: e.tensor_copy(out=w2kd[:, :, 0:64], in_=w2s[:, 0]), reads=["w2s"], writes=["w2kd"])
        P.add("dve", lambda e: e.tensor_copy(out=w2kd[:, :, 64:128], in_=w2s[:, 0]), reads=["w2s"], writes=["w2kd"])
        P.add("dve", lambda e: e.tensor_copy(out=w2vb, in_=w2s[:, 1]), reads=["w2s"], writes=["w2vb"])
        biasT = A.alloc(4)
        gT = A.alloc(2 * 256, BF16).rearrange("p (c n) -> p c n", c=2)
        RB = {"sq": mkring("sqB", 1, 256, BF16), "rs": mkring("rsB", 1, 256), "zn": mkring("znB", 1, 256),
              "zb": mkring("zbB", 1, 256, BF16), "t2": mkring("t2B", 1, 256)}
        for kv in range(2):
            for hc in range(2):
                for a in range(16):
                    P.add("pe", lambda e, kv=kv, hc=hc, a=a: e.matmul(ps[2][:, 0:1], lhsT=w1b[:, kv, a, hc * 128:(hc + 1) * 128], rhs=peb[:, kv * 16 + a:kv * 16 + a + 1], start=(a == 0), stop=(a == 15)), reads=["w1b", "peb"], writes=["ps2"])
                P.add("dve", lambda e, kv=kv, hc=hc: e.tensor_copy(out=biasT[:, kv * 2 + hc:kv * 2 + hc + 1], in_=ps[2][:, 0:1]), reads=["ps2"], writes=["biasT"])
        for kv in range(2):
            for g in range(2):
                P.add("dve", lambda e: e.memset(gT, 0.0), writes=["gT"])
                for hc in range(2):
                    pi = hc
                    for a in range(16):
                        P.add("pe", lambda e, kv=kv, g=g, hc=hc, a=a, pi=pi: e.matmul(ps[pi][:, 0:255], lhsT=w1b[:, kv, a, hc * 128:(hc + 1) * 128], rhs=x2c[:, kv, g, 2 * a:2 * a + 16 * 254 + 1:16], start=(a == 0), stop=(a == 15)), reads=["w1b", "x2c"], writes=["ps%d" % pi])
                    P.add("act", lambda e, kv=kv, hc=hc, pi=pi: e.activation(out=gT[:, hc, 0:255], in_=ps[pi][:, 0:255], func=AF.Gelu_apprx_tanh, bias=biasT[:, kv * 2 + hc:kv * 2 + hc + 1]), reads=["ps%d" % pi, "biasT"], writes=["gT"])
                if kv == 0:
                    for hc in range(2):
                        P.add("pe", lambda e, hc=hc: e.matmul(ps[5][:, 0:256], lhsT=w2kd[:, hc, :], rhs=gT[:, hc, :], start=(hc == 0), stop=(hc == 1)), reads=["w2kd", "gT"], writes=["ps5"])
                    norm_rope("ps5", ps[5][:, 0:256], 256, gains[:, NFM:NFM + 1], cosC, sinC, ["cosC", "sinC"], "kcmp", kcmp[:, g, :], RB)
                    P.add("dve", lambda e, g=g: e.memset(kcmp[:, g, 255:256], 0.0), writes=["kcmp"])
                else:
                    for ct in range(2):
                        for hc in range(2):
                            P.add("pe", lambda e, hc=hc, ct=ct: e.matmul(ps[6][:, 0:64], lhsT=gT[:, hc, ct * 128:(ct + 1) * 128], rhs=w2vb[:, hc, :], start=(hc == 0), stop=(hc == 1)), reads=["w2vb", "gT"], writes=["ps6"])
                        P.add("act", lambda e, g=g, ct=ct: e.activation(out=vc1[:, ct, g, 0:64], in_=ps[6][:, 0:64], func=AF.Copy), reads=["ps6"], writes=["vc1"])
        A.release(mB1)
        P.fence()

        Sps = Ring([0, 1])

        def banded(qb, sources, o_of_u, o_slot, er, o_clear, mul_of_kt=None):
            P.add("dve", lambda e: e.memset(o_clear, 0.0), writes=[o_slot])
            items = []
            for si, src in enumerate(sources):
                dmax = src[6]
                for kt in range(max(0, 4 * qb - dmax), 4 * qb + 4):
                    u0 = max(0, kt - 4 * qb)
                    u1 = min(3, kt + dmax - 4 * qb)
                    if u0 <= u1:
                        items.append((si, kt, u0, u1))
            first = {}
            last = {}
            for idx, (si, kt, u0, u1) in enumerate(items):
                for u in range(u0, u1 + 1):
                    first.setdefault(u, idx)
                    last[u] = idx
            for idx, (si, kt, u0, u1) in enumerate(items):
                qf, qs, kf, ks, vf, vs, dmax, mf = sources[si]
                n = (u1 - u0 + 1) * 128
                cq = qb * 512 + u0 * 128
                pi = Sps.next()
                P.add("pe", lambda e, pi=pi, kf=kf, kt=kt, qf=qf, cq=cq, n=n: e.matmul(ps[pi][:, 0:n], lhsT=kf(kt), rhs=qf(cq, n), start=True, stop=True), reads=list(qs) + list(ks), writes=["ps%d" % pi])
                en, ea = er.next()
                P.add("act", lambda e, pi=pi, ea=ea, n=n: e.activation(out=ea[:, 0:n], in_=ps[pi][:, 0:n], func=AF.Exp, scale=0.125), reads=["ps%d" % pi], writes=[en])
                if mul_of_kt is not None:
                    mn, ma = mul_of_kt(kt)
                    P.add("dve", lambda e, ea=ea, ma=ma, n=n, u0=u0: e.tensor_tensor(out=ea[:, 0:n], in0=ea[:, 0:n], in1=ma[:, u0 * 128:u0 * 128 + n], op=ALU.mult), reads=[en, mn], writes=[en])
                for u in range(u0, u1 + 1):
                    dl = 4 * qb + u - kt
                    mi = mf(dl)
                    lo = (u - u0) * 128
                    if mi is not None:
                        P.add("dve", lambda e, ea=ea, lo=lo, mi=mi: e.tensor_tensor(out=ea[:, lo:lo + 128], in0=ea[:, lo:lo + 128], in1=masks[:, mi, :], op=ALU.mult), reads=[en, "masks"], writes=[en])
                for u in range(u0, u1 + 1):
                    lo = (u - u0) * 128
                    P.add("pe", lambda e, ea=ea, lo=lo, u=u, vf=vf, kt=kt, idx=idx: e.matmul(o_of_u(u), lhsT=ea[:, lo:lo + 128], rhs=vf(kt), start=False, stop=(last[u] == idx), skip_group_check=True), reads=[en] + list(vs), writes=[o_slot])

        mC = A.mark()
        gat = A.alloc(NT * 24).rearrange("p (k c) -> p k c", k=NT)
        for i in range(4):
            P.dma(lambda e, i=i: e.dma_start(out=gat[:, 8 * i:8 * i + 8, :], in_=gates_s[1024 * i:1024 * (i + 1), :].rearrange("(k p) c -> p k c", p=128)), reads=["gates_s"], writes=["gat"])
        cmask = A.alloc(2 * S, BF16).rearrange("p (a t) -> p a t", a=2)
        P.dma(lambda e: e.dma_start(out=cmask, in_=cmask_d.rearrange("a p t -> p a t")), writes=["cmask"])
        selmul = A.alloc(NT * 64).rearrange("p (k c) -> p k c", k=NT)
        seladd = A.alloc(NT * 64).rearrange("p (k c) -> p k c", k=NT)
        for i in range(4):
            P.dma(lambda e, i=i: e.dma_start(out=selmul[:, 8 * i:8 * i + 8, :], in_=selmul_d[1024 * i:1024 * (i + 1), :].rearrange("(k p) c -> p k c", p=128)), writes=["selmul"])
            P.dma(lambda e, i=i: e.dma_start(out=seladd[:, 8 * i:8 * i + 8, :], in_=seladd_d[1024 * i:1024 * (i + 1), :].rearrange("(k p) c -> p k c", p=128)), writes=["seladd"])
        esel = A.alloc(S, BF16)
        P.dma(lambda e: e.dma_start(out=esel[0:64, :], in_=esel_d), writes=["esel"])
        Qn = A.alloc(2 * S, BF16).rearrange("p (c t) -> p c t", c=2)
        KS = A.alloc(S, BF16)
        KW = A.alloc(S, BF16)
        VS1 = A.alloc(NT * 65, BF16).rearrange("p (k c) -> p k c", k=NT)
        VW1 = A.alloc(NT * 65, BF16).rearrange("p (k c) -> p k c", k=NT)
        P.add("dve", lambda e: e.memset(VS1[:, :, 64:65], 1.0), writes=["VS1"])
        P.add("dve", lambda e: e.memset(VW1[:, :, 64:65], 1.0), writes=["VW1"])
        er = mkring("E", 3, 512, BF16)
        msbr = mkring("Msb", 2, 512, BF16)
        impacc = A.alloc(4 * 64).rearrange("p (u c) -> p u c", u=4)
        Y = A.alloc(4 * 256).rearrange("p (u c) -> p u c", u=4)
        Yb = A.alloc(4 * 256, BF16).rearrange("p (u c) -> p u c", u=4)
        rd = A.alloc(4)
        gd = A.alloc(4)
        sc = A.alloc(64)
        sct = A.alloc(64)
        m8 = A.alloc(16)
        selm = A.alloc(64, BF16)
        selT = A.alloc(512, BF16)
        yst = mkring("yst", 2, 2 * 512, BF16)

        def wmask(dl):
            return 0 if dl == 0 else (1 if dl == 4 else None)

        def smask(dl):
            return 0 if dl == 0 else None

        def finish_branch(o_views, o_slots, qb, g, j, br, first_branch):
            hh = 4 * g + j
            for u in range(4):
                P.add("dve", lambda e, u=u: e.tensor_scalar(out=rd[:, u:u + 1], in0=o_views[u][:, 64:65], scalar1=1e-30, scalar2=None, op0=ALU.max), reads=o_slots, writes=["rd"])
            P.add("dve", lambda e: e.reciprocal(out=rd, in_=rd), reads=["rd"], writes=["rd"])
            P.add("dve", lambda e: e.tensor_tensor(out=gd, in0=rd, in1=gat[:, 4 * qb:4 * qb + 4, hh * 3 + br], op=ALU.mult), reads=["rd", "gat"], writes=["gd"])
            for u in range(4):
                if first_branch:
                    P.add("dve", lambda e, u=u: e.tensor_scalar(out=Y[:, u, j * 64:(j + 1) * 64], in0=o_views[u][:, 0:64], scalar1=gd[:, u:u + 1], scalar2=None, op0=ALU.mult), reads=o_slots + ["gd"], writes=["Y"])
                else:
                    P.add("dve", lambda e, u=u: e.scalar_tensor_tensor(out=Y[:, u, j * 64:(j + 1) * 64], in0=o_views[u][:, 0:64], scalar=gd[:, u:u + 1], in1=Y[:, u, j * 64:(j + 1) * 64], op0=ALU.mult, op1=ALU.add), reads=o_slots + ["gd", "Y"], writes=["Y"])

        for g in range(2):
            for c in range(2):
                P.dma(lambda e, c=c, g=g: e.dma_start(out=Qn[:, c, :], in_=qkt_s[CID_Q + 2 * g + c]), reads=["qkt_s"], writes=["Qn"])
            P.dma(lambda e, g=g: e.dma_start(out=KS, in_=qkt_s[CID_KS + g]), reads=["qkt_s"], writes=["KS"])
            P.dma(lambda e, g=g: e.dma_start(out=KW, in_=qkt_s[CID_KW + g]), reads=["qkt_s"], writes=["KW"])
            for i in range(4):
                P.dma(lambda e, i=i, g=g: e.dma_start(out=VS1[:, 8 * i:8 * i + 8, 0:64], in_=vtm_s[1024 * i:1024 * (i + 1), g * 64:(g + 1) * 64].rearrange("(k p) c -> p k c", p=128)), reads=["vtm_s"], writes=["VS1"])
                P.dma(lambda e, i=i, g=g: e.dma_start(out=VW1[:, 8 * i:8 * i + 8, 0:64], in_=vtm_s[1024 * i:1024 * (i + 1), 128 + g * 64:128 + (g + 1) * 64].rearrange("(k p) c -> p k c", p=128)), reads=["vtm_s"], writes=["VW1"])
            for qb in range(NB):
                c0 = qb * 512
                cts = [0, 1] if qb >= 4 else [0]
                for j in range(4):
                    base = 64 * (j % 2)
                    cj = j // 2
                    oa = ps[2].rearrange("p (u c) -> p u c", u=2)
                    ob = ps[3].rearrange("p (u c) -> p u c", u=2)
                    ov_ = [oa[:, 0, 0:129], oa[:, 1, 0:129], ob[:, 0, 0:129], ob[:, 1, 0:129]]
                    osl = ["ps2", "ps2", "ps3", "ps3"]
                    P.add("dve", lambda e: e.memset(ps[2], 0.0), writes=["ps2"])
                    P.add("dve", lambda e: e.memset(ps[3], 0.0), writes=["ps3"])
                    for ct in cts:
                        pi = Sps.next()
                        P.add("pe", lambda e, pi=pi, ct=ct, base=base, cj=cj, g=g, c0=c0: e.matmul(ps[pi], lhsT=kcmp[base:base + 64, g, ct * 128:(ct + 1) * 128], rhs=Qn[base:base + 64, cj, c0:c0 + 512], start=True, stop=True), reads=["kcmp", "Qn"], writes=["ps%d" % pi])
                        en, ea = er.next()
                        P.add("act", lambda e, pi=pi, ea=ea: e.activation(out=ea, in_=ps[pi], func=AF.Exp, scale=0.125), reads=["ps%d" % pi], writes=[en])
                        P.add("dve", lambda e, ea=ea, ct=ct, c0=c0: e.tensor_tensor(out=ea, in0=ea, in1=cmask[:, ct, c0:c0 + 512], op=ALU.mult), reads=[en, "cmask"], writes=[en])
                        for u in range(4):
                            P.add("pe", lambda e, ea=ea, u=u, ct=ct, g=g, ov_=ov_, sp_=(ct == cts[-1]): e.matmul(ov_[u], lhsT=ea[:, u * 128:(u + 1) * 128], rhs=vc1[:, ct, g, :], start=False, stop=sp_, skip_group_check=True), reads=[en, "vc1"], writes=[osl[u]])
                    finish_branch(ov_, ["ps2", "ps3"], qb, g, j, 0, True)
                    for u in range(4):
                        if j == 0:
                            P.add("dve", lambda e, u=u: e.tensor_scalar(out=impacc[:, u, :], in0=ov_[u][:, 65:129], scalar1=rd[:, u:u + 1], scalar2=None, op0=ALU.mult), reads=["ps2", "ps3", "rd"], writes=["impacc"])
                        else:
                            P.add("dve", lambda e, u=u: e.scalar_tensor_tensor(out=impacc[:, u, :], in0=ov_[u][:, 65:129], scalar=rd[:, u:u + 1], in1=impacc[:, u, :], op0=ALU.mult, op1=ALU.add), reads=["ps2", "ps3", "rd", "impacc"], writes=["impacc"])
                for u in range(4):
                    tt = 4 * qb + u
                    P.add("dve", lambda e, u=u, tt=tt: e.tensor_tensor(out=sc, in0=impacc[:, u, :], in1=selmul[:, tt, :], op=ALU.mult), reads=["impacc", "selmul"], writes=["sc"])
                    P.add("dve", lambda e, tt=tt: e.tensor_tensor(out=sc, in0=sc, in1=seladd[:, tt, :], op=ALU.add), reads=["sc", "seladd"], writes=["sc"])
                    P.add("dve", lambda e: e.max(out=m8[:, 0:8], in_=sc), reads=["sc"], writes=["m8"])
                    P.add("dve", lambda e: e.match_replace(out=sct, in_to_replace=m8[:, 0:8], in_values=sc, imm_value=-1e30), reads=["sc", "m8"], writes=["sct"])
                    P.add("dve", lambda e: e.max(out=m8[:, 8:16], in_=sct), reads=["sct"], writes=["m8"])
                    P.add("dve", lambda e: e.tensor_scalar(out=selm, in0=sc, scalar1=m8[:, 15:16], scalar2=None, op0=ALU.is_ge), reads=["sc", "m8"], writes=["selm"])
                    P.add("pe", lambda e: e.transpose(out=psb[7][0:64, 0:128], in_=selm, identity=ident), reads=["selm", "ident"], writes=["ps7"])
                    P.add("act", lambda e, u=u: e.activation(out=selT[0:64, u * 128:(u + 1) * 128], in_=psb[7][0:64, 0:128], func=AF.Copy), reads=["ps7"], writes=["selT"])
                nkt = 4 * qb + 4
                for j in range(4):
                    base = 64 * (j % 2)
                    cj = j // 2
                    o5 = ps[5].rearrange("p (u c) -> p u c", u=4)
                    o6 = ps[6].rearrange("p (u c) -> p u c", u=4)
                    qf = lambda cq, n, base=base, cj=cj: Qn[base:base + 64, cj, cq:cq + n]

                    def mul_of_kt(kt):
                        mn, ma = msbr.next()
                        P.add("pe", lambda e, kt=kt: e.matmul(ps[4], lhsT=esel[0:64, kt * 128:(kt + 1) * 128], rhs=selT[0:64, :], start=True, stop=True), reads=["esel", "selT"], writes=["ps4"])
                        P.add("act", lambda e, ma=ma: e.activation(out=ma, in_=ps[4], func=AF.Copy), reads=["ps4"], writes=[mn])
                        return mn, ma

                    banded(qb, [(qf, ["Qn"], lambda kt, base=base: KS[base:base + 64, kt * 128:(kt + 1) * 128], ["KS"], lambda kt: VS1[:, kt, :], ["VS1"], 64, smask)],
                           lambda u: o5[:, u, 0:65], "ps5", er, ps[5], mul_of_kt=mul_of_kt)
                    finish_branch([o5[:, u, :] for u in range(4)], ["ps5"], qb, g, j, 1, False)
                    banded(qb, [(qf, ["Qn"], lambda kt, base=base: KW[base:base + 64, kt * 128:(kt + 1) * 128], ["KW"], lambda kt: VW1[:, kt, :], ["VW1"], 4, wmask)],
                           lambda u: o6[:, u, 0:65], "ps6", er, ps[6])
                    finish_branch([o6[:, u, :] for u in range(4)], ["ps6"], qb, g, j, 2, False)
                P.add("act", lambda e: e.activation(out=Yb, in_=Y, func=AF.Copy), reads=["Y"], writes=["Yb"])
                ysn, ys_ = yst.next()
                ys = ys_.rearrange("p (c t) -> p c t", c=2)
                for u in range(4):
                    for c in range(2):
                        P.add("pe", lambda e, u=u, c=c: e.transpose(out=psb[7][:, c * 128:(c + 1) * 128], in_=Yb[:, u, c * 128:(c + 1) * 128], identity=ident), reads=["Yb", "ident"], writes=["ps7"])
                    P.add("act", lambda e, u=u, ys=ys: e.activation(out=ys[:, :, u * 128:(u + 1) * 128], in_=psb[7][:, 0:256].rearrange("p (c t) -> p c t", c=2), func=AF.Copy), reads=["ps7"], writes=[ysn])
                P.dma(lambda e, ys=ys, g=g, c0=c0: e.dma_start(out=yt_s[2 * g:2 * g + 2, :, c0:c0 + 512].rearrange("c p t -> p c t"), in_=ys), reads=[ysn], writes=["yt_s"], queue="pool")
        A.release(mB)
        P.fence()
        if stop_after == "C":
            P.emit(final_slots=["yt_s"])
            return nc, P, A

        mD = A.mark()
        DQ = A.alloc(3 * S, BF16).rearrange("p (g t) -> p g t", g=3)
        DK = A.alloc(3 * S, BF16).rearrange("p (g t) -> p g t", g=3)
        DV = A.alloc(6 * NT * 65, BF16).rearrange("p (a k c) -> p a k c", a=6, k=NT)
        P.add("dve", lambda e: e.memset(DV[:, :, :, 64:65], 1.0), writes=["DV"])
        er = mkring("Ed", 3, 512, BF16)
        Yd = A.alloc(4 * 128).rearrange("p (u c) -> p u c", u=4)
        Ydb = A.alloc(4 * 128, BF16).rearrange("p (u c) -> p u c", u=4)
        rdd = A.alloc(4)
        ydst = mkring("ydst", 2, 512, BF16)
        ops_ring = Ring([5, 6])

        def dmask_fn(gi):
            d = DIL_PAT[gi][1]
            if d == 1:
                return lambda dl: 0 if dl == 0 else 2
            b = 3 if d == 4 else 6
            return lambda dl, b=b, d=d: (b + 1) if dl == 0 else ((b + 2) if dl == d else b)

        for pr in range(2):
            for gi in range(3):
                P.dma(lambda e, gi=gi, pr=pr: e.dma_start(out=DQ[:, gi, :], in_=qkt_s[CID_DIL + gi * 4 + pr]), reads=["qkt_s"], writes=["DQ"])
                P.dma(lambda e, gi=gi, pr=pr: e.dma_start(out=DK[:, gi, :], in_=qkt_s[CID_DIL + gi * 4 + 2 + pr]), reads=["qkt_s"], writes=["DK"])
                for hh in range(2):
                    col = 256 + (gi * 4 + pr * 2 + hh) * 64
                    for i in range(4):
                        P.dma(lambda e, i=i, gi=gi, hh=hh, col=col: e.dma_start(out=DV[:, gi * 2 + hh, 8 * i:8 * i + 8, 0:64], in_=vtm_s[1024 * i:1024 * (i + 1), col:col + 64].rearrange("(k p) c -> p k c", p=128)), reads=["vtm_s"], writes=["DV"])
            for qb in range(NB):
                c0 = qb * 512
                for hh in range(2):
                    base = 64 * hh
                    opi = ops_ring.next()
                    ov4 = ps[opi].rearrange("p (u c) -> p u c", u=4)
                    srcs = []
                    for gi in range(3):
                        srcs.append((lambda cq, n, gi=gi, base=base: DQ[base:base + 64, gi, cq:cq + n], ["DQ"],
                                     lambda kt, gi=gi, base=base: DK[base:base + 64, gi, kt * 128:(kt + 1) * 128], ["DK"],
                                     lambda kt, gi=gi, hh=hh: DV[:, gi * 2 + hh, kt, :], ["DV"], DIL_PAT[gi][1], dmask_fn(gi)))
                    banded(qb, srcs, lambda u, ov4=ov4: ov4[:, u, 0:65], "ps%d" % opi, er, ps[opi])
                    P.add("dve", lambda e, ov4=ov4: e.reciprocal(out=rdd, in_=ov4[:, :, 64]), reads=["ps%d" % opi], writes=["rdd"])
                    for u in range(4):
                        P.add("dve", lambda e, u=u, ov4=ov4, hh=hh: e.tensor_scalar(out=Ydb[:, u, hh * 64:(hh + 1) * 64], in0=ov4[:, u, 0:64], scalar1=rdd[:, u:u + 1], scalar2=None, op0=ALU.mult), reads=["ps%d" % opi, "rdd"], writes=["Ydb"])
                ysn, ys = ydst.next()
                for u in range(4):
                    P.add("pe", lambda e, u=u: e.transpose(out=psb[7][:, 0:128], in_=Ydb[:, u, :], identity=ident), reads=["Ydb", "ident"], writes=["ps7"])
                    P.add("act", lambda e, u=u, ys=ys: e.activation(out=ys[:, u * 128:(u + 1) * 128], in_=psb[7][:, 0:128], func=AF.Copy), reads=["ps7"], writes=[ysn])
                P.dma(lambda e, ys=ys, pr=pr, c0=c0: e.dma_start(out=yt_s[4 + pr, :, c0:c0 + 512], in_=ys), reads=[ysn], writes=["yt_s"], queue="pool")
        A.release(mD)
        P.fence()
        if stop_after == "D":
            P.emit(final_slots=["yt_s"])
            return nc, P, A

        mE = A.mark()
        wun = A.alloc(4 * D, BF16).rearrange("p (a b) -> p a b", a=4)
        wud = A.alloc(2 * D, BF16).rearrange("p (a b) -> p a b", a=2)
        wmg = A.alloc(8 * 2048, BF16).rearrange("p (a b) -> p a b", a=8)
        wo = A.alloc(8 * D, BF16).rearrange("p (a b) -> p a b", a=8)
        g1tE = A.alloc(8)
        P.dma(lambda e: e.dma_start(out=g1tE, in_=g1_d), writes=["g1tE"])
        mE1 = A.mark()
        wst = mkring("wstE", 2, 2048)
        for a in range(4):
            sn, sa = wst.next()
            P.dma(lambda e, sa=sa, a=a: e.dma_start(out=sa[:, 0:D], in_=wun_d[a * 128:(a + 1) * 128, :]), writes=[sn])
            P.add("dve", lambda e, sa=sa, a=a: e.tensor_copy(out=wun[:, a, :], in_=sa[:, 0:D]), reads=[sn], writes=["wun"])
        for a in range(2):
            sn, sa = wst.next()
            P.dma(lambda e, sa=sa, a=a: e.dma_start(out=sa[:, 0:D], in_=wud_d[a * 128:(a + 1) * 128, :]), writes=[sn])
            P.add("dve", lambda e, sa=sa, a=a: e.tensor_copy(out=wud[:, a, :], in_=sa[:, 0:D]), reads=[sn], writes=["wud"])
        for a in range(8):
            sn, sa = wst.next()
            P.dma(lambda e, sa=sa, a=a: e.dma_start(out=sa[:, 0:D], in_=wo_d[a * 128:(a + 1) * 128, :]), writes=[sn])
            P.add("dve", lambda e, sa=sa, a=a: e.tensor_copy(out=wo[:, a, :], in_=sa[:, 0:D]), reads=[sn], writes=["wo"])
        for a in range(8):
            sn, sa = wst.next()
            P.dma(lambda e, sa=sa, a=a: e.dma_start(out=sa, in_=wmg_d[a * 128:(a + 1) * 128, :]), writes=[sn])
            P.add("dve", lambda e, sa=sa, a=a: e.tensor_scalar(out=wmg[:, a, :], in0=sa, scalar1=g1tE[:, a:a + 1], scalar2=None, op0=ALU.mult), reads=[sn, "g1tE"], writes=["wmg"])
        A.release(mE1)
        P.fence()
        ytr = mkring("ytE", 2, 6 * 512, BF16)
        htr = mkring("htE", 2, 8 * 512, BF16)
        sg0 = A.alloc(512)
        sg1 = A.alloc(512)
        t1 = A.alloc(512)
        mT = A.alloc(8 * 512, BF16).rearrange("p (a b) -> p a b", a=8)
        xr = mkring("xtE", 2, 1024)
        x2r = mkring("x2E", 2, 1024)
        junkE = A.alloc(1024)
        ssr = mkring("ssE", 2, 1)
        hbr = mkring("hbE", 2, 1024, BF16)
        h2r = mkring("h2E", 2, 8 * 512, BF16)
        for b in range(NB):
            c0 = b * 512
            yn, ya_ = ytr.next()
            ya = ya_.rearrange("p (a b) -> p a b", a=6)
            hn, ha_ = htr.next()
            ha = ha_.rearrange("p (a b) -> p a b", a=8)
            P.dma(lambda e, ya=ya, c0=c0: e.dma_start(out=ya, in_=yt_s[:, :, c0:c0 + 512].rearrange("c p t -> p c t")), reads=["yt_s"], writes=[yn])
            P.dma(lambda e, ha=ha, c0=c0: e.dma_start(out=ha, in_=ht_s[:, :, c0:c0 + 512].rearrange("c p t -> p c t")), reads=["ht_s"], writes=[hn])
            for dc in range(8):
                dsl = slice(dc * 128, (dc + 1) * 128)
                for a in range(4):
                    P.add("pe", lambda e, a=a, dsl=dsl, ya=ya: e.matmul(ps[0], lhsT=wun[:, a, dsl], rhs=ya[:, a, :], start=(a == 0), stop=(a == 3)), reads=["wun", yn], writes=["ps0"])
                for a in range(2):
                    P.add("pe", lambda e, a=a, dsl=dsl, ya=ya: e.matmul(ps[1], lhsT=wud[:, a, dsl], rhs=ya[:, 4 + a, :], start=(a == 0), stop=(a == 1)), reads=["wud", yn], writes=["ps1"])
                for hf in range(2):
                    for a in range(8):
                        P.add("pe", lambda e, a=a, hf=hf, dc=dc, ha=ha: e.matmul(ps[2 + hf], lhsT=wmg[:, a, hf * 1024 + dc * 128:hf * 1024 + (dc + 1) * 128], rhs=ha[:, a, :], start=(a == 0), stop=(a == 7)), reads=["wmg", hn], writes=["ps%d" % (2 + hf)])
                P.add("act", lambda e: e.activation(out=sg0, in_=ps[2], func=AF.Sigmoid), reads=["ps2"], writes=["sg0"])
                P.add("act", lambda e: e.activation(out=sg1, in_=ps[3], func=AF.Sigmoid), reads=["ps3"], writes=["sg1"])
                P.add("dve", lambda e: e.tensor_tensor(out=t1, in0=sg0, in1=ps[0], op=ALU.mult), reads=["sg0", "ps0"], writes=["t1"])
                P.add("dve", lambda e: e.tensor_tensor(out=sg1, in0=sg1, in1=ps[1], op=ALU.mult), reads=["sg1", "ps1"], writes=["sg1"])
                P.add("dve", lambda e, dc=dc: e.tensor_tensor(out=mT[:, dc, :], in0=t1, in1=sg1, op=ALU.add), reads=["t1", "sg1"], writes=["mT"])
            h2n, h2_ = h2r.next()
            h2 = h2_.rearrange("p (a b) -> p a b", a=8)
            for u in range(4):
                t0 = c0 + u * 128
                for hf in range(2):
                    for a in range(8):
                        P.add("pe", lambda e, a=a, hf=hf, u=u: e.matmul(ps[4 + hf], lhsT=mT[:, a, u * 128:(u + 1) * 128], rhs=wo[:, a, hf * 512:(hf + 1) * 512], start=(a == 0), stop=(a == 7)), reads=["wo", "mT"], writes=["ps%d" % (4 + hf)])
                xn, xt = xr.next()
                P.dma(lambda e, xt=xt, t0=t0: e.dma_start(out=xt, in_=x_d[t0:t0 + 128, :]), writes=[xn])
                x2n, x2 = x2r.next()
                for hf in range(2):
                    P.add("dve", lambda e, hf=hf, xt=xt, x2=x2: e.tensor_tensor(out=x2[:, hf * 512:(hf + 1) * 512], in0=xt[:, hf * 512:(hf + 1) * 512], in1=ps[4 + hf], op=ALU.add), reads=[xn, "ps%d" % (4 + hf)], writes=[x2n])
                P.dma(lambda e, x2=x2, t0=t0: e.dma_start(out=x2_s[t0:t0 + 128, :], in_=x2), reads=[x2n], writes=["x2_s"], queue="pool")
                sn, ss = ssr.next()
                P.add("act", lambda e, x2=x2, ss=ss: e.activation(out=junkE, in_=x2, func=AF.Square, accum_out=ss), reads=[x2n], writes=["junkE", sn])
                P.add("act", lambda e, ss=ss: e.activation(out=ss, in_=ss, func=AF.Sqrt, scale=1.0 / D, bias=EPS), reads=[sn], writes=[sn])
                P.add("dve", lambda e, ss=ss: e.reciprocal(out=ss, in_=ss), reads=[sn], writes=[sn])
                hbn, hb = hbr.next()
                P.add("dve", lambda e, x2=x2, ss=ss, hb=hb: e.tensor_scalar(out=hb, in0=x2, scalar1=ss, scalar2=None, op0=ALU.mult), reads=[x2n, sn], writes=[hbn])
                for c in range(8):
                    P.add("pe", lambda e, c=c, hb=hb: e.transpose(out=psb[6][:, c * 128:(c + 1) * 128], in_=hb[:, c * 128:(c + 1) * 128], identity=ident), reads=[hbn, "ident"], writes=["ps6"])
                P.add("act", lambda e, u=u, h2=h2: e.activation(out=h2[:, :, u * 128:(u + 1) * 128], in_=psb[6].rearrange("p (a b) -> p a b", a=8), func=AF.Copy), reads=["ps6"], writes=[h2n])
            P.dma(lambda e, h2=h2, c0=c0: e.dma_start(out=h2t_s[:, :, c0:c0 + 512].rearrange("c p t -> p c t"), in_=h2), reads=[h2n], writes=["h2t_s"], queue="pool")
        A.release(mE)
        P.fence()
        if stop_after == "E":
            P.emit(final_slots=["x2_s", "h2t_s"])
            return nc, P, A

        wq = A.alloc(8 * D, BF16).rearrange("p (a b) -> p a b", a=8)
        g2t = A.alloc(8)
        skbd = A.alloc(256, BF16)
        P.dma(lambda e: e.dma_start(out=g2t, in_=g2_d), writes=["g2t"])
        mF1 = A.mark()
        wst = mkring("wstF", 2, 1024)
        for a in range(8):
            sn, sa = wst.next()
            P.dma(lambda e, sa=sa, a=a: e.dma_start(out=sa, in_=wq_d[a * 128:(a + 1) * 128, :]), writes=[sn])
            P.add("dve", lambda e, sa=sa, a=a: e.tensor_scalar(out=wq[:, a, :], in0=sa, scalar1=g2t[:, a:a + 1], scalar2=None, op0=ALU.mult), reads=[sn, "g2t"], writes=["wq"])
        sn, sa = wst.next()
        P.dma(lambda e, sa=sa: e.dma_start(out=sa[:, 0:256], in_=skbd_d), writes=[sn])
        P.add("dve", lambda e, sa=sa: e.tensor_copy(out=skbd, in_=sa[:, 0:256]), reads=[sn], writes=["skbd"])
        A.release(mF1)
        P.fence()

        hgr = mkring("hg", 2, 8 * 256, BF16)
        qtg = A.alloc(8 * 256, BF16).rearrange("p (a b) -> p a b", a=8)
        ssb = A.alloc(8 * 256).rearrange("p (h n) -> p h n", h=8)
        stmp = A.alloc(256)
        T16 = A.alloc(256).rearrange("p (a b) -> p a b", a=16)
        negm = A.alloc(16)
        ab = A.alloc(8 * 256).rearrange("p (h n) -> p h n", h=8)
        abt = A.alloc(256).rearrange("p (a b) -> p a b", a=16)
        Pc = A.alloc(8 * 256).rearrange("p (h n) -> p h n", h=8)
        P16 = A.alloc(128).rearrange("p (h n) -> p h n", h=8)
        Zs = A.alloc(8)
        a2 = A.alloc(8 * 128).rearrange("p (h n) -> p h n", h=8)
        a2t = A.alloc(128).rearrange("p (h n) -> p h n", h=8)
        pdr = mkring("Pd", 2, 2048)
        whr = mkring("Wh", 12, 2048, BF16)
        wsr = mkring("wstage", 3, 512, BF16)
        usr = mkring("Us", 2, 4096, BF16)
        vsr = mkring("Vs", 2, 4096, BF16)
        wlr = mkring("Wl", 2, 1024, BF16)
        ggr = mkring("Gg", 2, 256, BF16)
        gtr = mkring("GT", 2, 256, BF16)
        xr = mkring("xtG", 2, 1024)
        wps = Ring([2])
        halfA = Ring([("ps0", ps[0][:, 0:256]), ("ps1", ps[1][:, 0:256])])
        halfF = Ring([("ps3", ps[3][:, 0:256])])
        NG = S // 256
        hg_of = {}

        def F_steps(G):
            g0 = G * 256
            par = G % 2
            hgn, hg_ = hgr.next()
            hg = hg_.rearrange("p (a b) -> p a b", a=8)
            hg_of[G] = (hgn, hg)
            P.dma(lambda e, g0=g0, hg=hg: e.dma_start(out=hg, in_=h2t_s[:, :, g0:g0 + 256].rearrange("c p t -> p c t")), reads=["h2t_s"], writes=[hgn])
            for h in range(8):
                pn, pa = halfF.next()
                for a in range(8):
                    P.add("pe", lambda e, pa=pa, a=a, h=h, hg=hg: e.matmul(pa, lhsT=wq[:, a, h * 128:(h + 1) * 128], rhs=hg[:, a, :], start=(a == 0), stop=(a == 7)), reads=["wq", hgn], writes=[pn])
                P.add("act", lambda e, pa=pa, h=h: e.activation(out=qtg[:, h, :], in_=pa, func=AF.Copy), reads=[pn], writes=["qtg"])
            yield
            for w in range(2):
                for h in range(8):
                    pn, pa = halfF.next()
                    P.add("pe", lambda e, pa=pa, h=h, w=w: e.matmul(pa, lhsT=qtg[:, h, w * 128:(w + 1) * 128], rhs=skbd, start=True, stop=True), reads=["qtg", "skbd"], writes=[pn])
                    P.add("act", lambda e, pa=pa, h=h: e.activation(out=ssb[:, h, :], in_=pa, func=AF.Copy), reads=[pn], writes=["ssb"])
                for hc in range(16):
                    h, c = hc // 2, hc % 2
                    src = ssb[:, h, c * 128:(c + 1) * 128]
                    P.add("dve", lambda e, src=src, hc=hc: e.max(out=T16[:, hc, 0:8], in_=src), reads=["ssb"], writes=["T16"])
                    P.add("dve", lambda e, src=src, hc=hc: e.match_replace(out=stmp[:, 0:128], in_to_replace=T16[:, hc, 0:8], in_values=src, imm_value=-1e30), reads=["ssb", "T16"], writes=["stmp"])
                    P.add("dve", lambda e, hc=hc: e.max(out=T16[:, hc, 8:16], in_=stmp[:, 0:128]), reads=["stmp"], writes=["T16"])
                P.add("dve", lambda e: e.tensor_scalar(out=negm, in0=T16[:, :, 0], scalar1=-1.0, scalar2=None, op0=ALU.mult), reads=["T16"], writes=["negm"])
                for hc in range(16):
                    h, c = hc // 2, hc % 2
                    P.add("act", lambda e, h=h, c=c, hc=hc: e.activation(out=ab[:, h, c * 128:(c + 1) * 128], in_=ssb[:, h, c * 128:(c + 1) * 128], func=AF.Exp, bias=negm[:, hc:hc + 1]), reads=["ssb", "negm"], writes=["ab"])
                    P.add("act", lambda e, hc=hc: e.activation(out=abt[:, hc, :], in_=T16[:, hc, :], func=AF.Exp, bias=negm[:, hc:hc + 1]), reads=["T16", "negm"], writes=["abt"])
                abt4 = abt.rearrange("p (h c) n -> p h c n", c=2)

                def cand_top(at_ap):
                    P.add("dve", lambda e: e.tensor_tensor(out=Pc.rearrange("p h (i j) -> p h i j", i=16), in0=at_ap.unsqueeze(3).to_broadcast([128, 8, 16, 16]), in1=abt4[:, :, 1, :].unsqueeze(2).to_broadcast([128, 8, 16, 16]), op=ALU.mult), reads=["abt", "a2t"], writes=["Pc"])
                    for h in range(8):
                        P.add("dve", lambda e, h=h: e.max(out=P16[:, h, 0:8], in_=Pc[:, h, :]), reads=["Pc"], writes=["P16"])
                        P.add("dve", lambda e, h=h: e.match_replace(out=stmp, in_to_replace=P16[:, h, 0:8], in_values=Pc[:, h, :], imm_value=-1.0), reads=["Pc", "P16"], writes=["stmp"])
                        P.add("dve", lambda e, h=h: e.max(out=P16[:, h, 8:16], in_=stmp), reads=["stmp"], writes=["P16"])

                cand_top(abt4[:, :, 0, :])
                P.add("dve", lambda e: e.reduce_sum(out=Zs, in_=P16, axis=AX.X), reads=["P16"], writes=["Zs"])
                P.add("dve", lambda e: e.reciprocal(out=Zs, in_=Zs), reads=["Zs"], writes=["Zs"])
                P.add("dve", lambda e: e.tensor_tensor(out=a2, in0=ab[:, :, 0:128], in1=Zs.unsqueeze(2).to_broadcast([128, 8, 128]), op=ALU.mult), reads=["ab", "Zs"], writes=["a2"])
                P.add("dve", lambda e: e.tensor_tensor(out=a2t, in0=abt4[:, :, 0, :], in1=Zs.unsqueeze(2).to_broadcast([128, 8, 16]), op=ALU.mult), reads=["abt", "Zs"], writes=["a2t"])
                cand_top(a2t)
                yield
                for eb in range(8):
                    whs = []
                    for h in range(8):
                        pn, pd = pdr.next()
                        peng = "dve"
                        P.add(peng, lambda e, pd=pd, h=h, eb=eb: e.tensor_tensor(out=pd.rearrange("p (i j) -> p i j", i=16), in0=a2[:, h, eb * 16:(eb + 1) * 16].unsqueeze(2).to_broadcast([128, 16, 128]), in1=ab[:, h, 128:256].unsqueeze(1).to_broadcast([128, 16, 128]), op=ALU.mult), reads=["a2", "ab"], writes=[pn])
                        wn, wh = whr.next()
                        P.add("dve", lambda e, pd=pd, wh=wh, h=h: e.scalar_tensor_tensor(out=wh, in0=pd, scalar=P16[:, h, 15:16], in1=pd, op0=ALU.is_ge, op1=ALU.mult), reads=[pn, "P16"], writes=[wn])
                        whs.append((wn, wh))
                    for q4 in range(4):
                        pi = wps.next()
                        for ci in range(4):
                            cc = q4 * 4 + ci
                            for h in range(8):
                                wn, wh = whs[h]
                                P.add("pe", lambda e, pi=pi, ci=ci, cc=cc, wh=wh, h=h: e.matmul(ps[pi][:, ci * 128:(ci + 1) * 128], lhsT=wh[:, cc * 128:(cc + 1) * 128], rhs=ident, start=(h == 0), stop=(h == 7)), reads=[wn, "ident"], writes=["ps%d" % pi])
                        e1 = eb * 16 + q4 * 4
                        sgn, sg = wsr.next()
                        P.add("act", lambda e, pi=pi, sg=sg: e.activation(out=sg, in_=ps[pi], func=AF.Copy), reads=["ps%d" % pi], writes=[sgn])
                        P.dma(lambda e, sg=sg, e1=e1, w=w, par=par: e.dma_start(out=wt_s[par, e1 // 4, :, w * 512:(w + 1) * 512], in_=sg), reads=[sgn], writes=["wt_s%d" % par], queue="pool")
                    yield

        def G_steps(G):
            g0 = G * 256
            par = G % 2
            hgn, hg = hg_of[G]
            for eg in range(32):
                un, us4 = usr.next()
                vn, vs4 = vsr.next()
                wln, wl4_ = wlr.next()
                wl4 = wl4_.rearrange("p (w a t) -> p w a t", w=2, a=4)
                P.dma(lambda e, us4=us4, eg=eg: e.dma_start(out=us4, in_=utb_s[eg]), reads=["utb_s"], writes=[un])
                P.dma(lambda e, vs4=vs4, eg=eg: e.dma_start(out=vs4, in_=vb_s[eg]), reads=["vb_s"], writes=[vn])
                P.dma(lambda e, wl4_=wl4_, eg=eg, par=par: e.dma_start(out=wl4_, in_=wt_s[par, eg]), reads=["wt_s%d" % par], writes=[wln])
                for q in range(4):
                    e1 = eg * 4 + q
                    us = us4[:, q * 1024:(q + 1) * 1024].rearrange("p (a b) -> p a b", a=8)
                    vs = vs4[:, q * 1024:(q + 1) * 1024]
                    pn, pa = halfA.next()
                    for a in range(8):
                        P.add("pe", lambda e, pa=pa, a=a, us=us, hg=hg: e.matmul(pa, lhsT=us[:, a, :], rhs=hg[:, a, :], start=(a == 0), stop=(a == 7)), reads=[un, hgn], writes=[pn])
                    gn, gg = ggr.next()
                    P.add("act", lambda e, pa=pa, gg=gg: e.activation(out=gg, in_=pa, func=AF.Gelu_apprx_tanh), reads=[pn], writes=[gn])
                    tn, gt = gtr.next()
                    P.add("dve", lambda e, gg=gg, gt=gt, wl4=wl4, q=q: e.tensor_tensor(out=gt.rearrange("p (w t) -> p w t", w=2), in0=gg.rearrange("p (w t) -> p w t", w=2), in1=wl4[:, :, q, :], op=ALU.mult), reads=[gn, wln], writes=[tn])
                    for w in range(2):
                        for hf in range(2):
                            pj = 4 + w * 2 + hf
                            P.add("pe", lambda e, pj=pj, w=w, hf=hf, gt=gt, vs=vs, e1=e1: e.matmul(ps[pj], lhsT=gt[:, w * 128:(w + 1) * 128], rhs=vs[:, hf * 512:(hf + 1) * 512], start=(e1 == 0), stop=(e1 == 127)), reads=[tn, vn], writes=["ps%d" % pj])
                    yield
            for w in range(2):
                t0 = g0 + w * 128
                xn, xt = xr.next()
                P.dma(lambda e, xt=xt, t0=t0: e.dma_start(out=xt, in_=x2_s[t0:t0 + 128, :]), reads=["x2_s"], writes=[xn])
                for hf in range(2):
                    pj = 4 + w * 2 + hf
                    P.add("dve", lambda e, xt=xt, hf=hf, pj=pj: e.tensor_tensor(out=xt[:, hf * 512:(hf + 1) * 512], in0=xt[:, hf * 512:(hf + 1) * 512], in1=ps[pj], op=ALU.add), reads=[xn, "ps%d" % pj], writes=[xn])
                P.dma(lambda e, xt=xt, t0=t0: e.dma_start(out=out_d[t0:t0 + 128, :], in_=xt), reads=[xn], writes=["out"], queue="pool")
            yield

        for G in range(NG + 1):
            fs = F_steps(G) if G < NG else iter(())
            gs = G_steps(G - 1) if G >= 1 else iter(())
            fdone = gdone = False
            while not (fdone and gdone):
                if not fdone:
                    try:
                        next(fs)
                    except StopIteration:
                        fdone = True
                for _ in range(7):
                    if gdone:
                        break
                    try:
                        next(gs)
                    except StopIteration:
                        gdone = True

        P.emit(final_slots=["out"])
    return nc, P, A


def _consts():
    bf = ml_dtypes.bfloat16
    c = {}
    c["ident"] = np.eye(128, dtype=np.float32).astype(bf)
    bd = np.zeros((128, 128), np.float32)
    bd[:64, :64] = 1
    bd[64:, 64:] = 1
    c["bdones"] = bd.astype(bf)
    Rm = np.zeros((128, 128), np.float32)
    for hb in (0, 64):
        for i in range(8):
            Rm[hb + i, hb + i + 8] = -1.0
            Rm[hb + i + 8, hb + i] = 1.0
    c["rotT"] = np.ascontiguousarray(Rm.T).astype(bf)
    half = 8
    inv = (500000.0 ** (-(np.arange(half, dtype=np.float32) * 2.0 / 16))).astype(np.float32)

    def tables(pos):
        ang = pos.astype(np.float32)[None, :] * inv[:, None]
        cs = np.ones((128, pos.shape[0]), np.float32)
        sn = np.zeros((128, pos.shape[0]), np.float32)
        for hb in (0, 64):
            cs[hb:hb + 8] = np.cos(ang)
            cs[hb + 8:hb + 16] = np.cos(ang)
            sn[hb:hb + 8] = np.sin(ang)
            sn[hb + 8:hb + 16] = np.sin(ang)
        return cs, sn

    c["cosT"], c["sinT"] = tables(np.arange(S))
    pc = np.arange(256) * 16 + 31
    c["cosC"], c["sinC"] = tables(pc)
    t = np.arange(S)
    cm = np.zeros((2, 128, S), np.float32)
    for ct in range(2):
        cidx = ct * 128 + np.arange(128)
        cm[ct] = ((cidx[:, None] * 16 + 31 <= t[None, :]) & (cidx[:, None] < 255)).astype(np.float32)
    c["cmask"] = cm.astype(bf)
    k = np.arange(128)[:, None]
    q = np.arange(128)[None, :]
    ms = np.zeros((128, 9, 128), np.float32)
    ms[:, 0] = k <= q
    ms[:, 1] = k > q
    ms[:, 2] = k >= q
    for b, d in ((3, 4), (6, 16)):
        res = ((q - k) % d) == 0
        ms[:, b] = res
        ms[:, b + 1] = res & (k <= q)
        ms[:, b + 2] = res & (k >= q)
    c["masks"] = ms.astype(bf)
    n_cmp = 255
    s0 = np.arange(n_cmp) * 16
    s1 = s0 + 32
    b0 = np.arange(64) * 64
    b1 = b0 + 64
    ovm = np.clip(np.minimum(s1[:, None], b1[None, :]) - np.maximum(s0[:, None], b0[None, :]), 0, None) / 32.0
    ovp = np.zeros((256, 64), np.float32)
    ovp[:255] = ovm
    c["ov"] = ovp.reshape(2, 128, 64).astype(bf)
    blk = np.arange(64)[None, :]
    cur = (t // 64)[:, None]
    forced = (blk == 0) | (blk == cur) | (blk == cur - 1)
    causal = blk * 64 <= t[:, None]
    c["selmul"] = (causal & ~forced).astype(np.float32)
    c["seladd"] = np.where(forced, 1e3, np.where(causal, 0.0, -1.0)).astype(np.float32)
    es = np.zeros((64, S), np.float32)
    es[(t // 64), t] = 1.0
    c["esel"] = es.astype(bf)
    return c


def _layout_weights(inp):
    w_in = np.asarray(inp["w_in"], np.float32)
    o = {}
    cols = []
    for cch in range(4):
        cols.append(np.arange(cch * 128, (cch + 1) * 128))
    cols.append(np.arange(512, 640))
    cols.append(np.arange(640, 768))
    for base in (768, 1024):
        for g in range(2):
            cc = base + g * 64 + np.arange(64)
            cols.append(np.concatenate([cc, cc]))
    for g in range(3):
        for r in range(2):
            for pr in range(2):
                cols.append(1304 + g * 768 + r * 256 + pr * 128 + np.arange(128))
    fm_cols = np.concatenate(cols)
    assert fm_cols.shape[0] == NFM * 128
    o["wfm"] = np.ascontiguousarray(w_in[:, fm_cols])
    tm_cols = np.concatenate([np.arange(896, 1024), np.arange(1152, 1280)] +
                             [1304 + g * 768 + 512 + np.arange(256) for g in range(3)] + [np.arange(1280, 1304)])
    assert tm_cols.shape[0] == NTM
    o["wtm"] = np.ascontiguousarray(w_in[:, tm_cols])
    o["wmg"] = np.ascontiguousarray(w_in[:, 3608:5656])
    o["g1"] = np.ascontiguousarray(np.asarray(inp["norm1_g"], np.float32).reshape(8, 128).T)
    o["g2"] = np.ascontiguousarray(np.asarray(inp["norm2_g"], np.float32).reshape(8, 128).T)
    gains = np.ones((128, NFM + 1), np.float32)
    qn = np.asarray(inp["nsa_q_norm"], np.float32)
    kn = np.asarray(inp["nsa_k_norm"], np.float32)
    dq = np.asarray(inp["dil_q_norm"], np.float32)
    dk = np.asarray(inp["dil_k_norm"], np.float32)
    for cch in range(4):
        gains[:, cch] = np.tile(qn, 2)
    for g in range(2):
        gains[:, CID_KS + g] = np.tile(kn[1], 2)
        gains[:, CID_KW + g] = np.tile(kn[2], 2)
    for g in range(3):
        for pr in range(2):
            gains[:, CID_DIL + g * 4 + pr] = np.tile(dq[g], 2)
            gains[:, CID_DIL + g * 4 + 2 + pr] = np.tile(dk[g], 2)
    gains[:, NFM] = np.tile(kn[0], 2)
    o["gains"] = gains
    o["w1k"] = np.asarray(inp["cmp_w1_k"], np.float32)
    o["w1v"] = np.asarray(inp["cmp_w1_v"], np.float32)
    o["w2k"] = np.asarray(inp["cmp_w2_k"], np.float32)
    o["w2v"] = np.asarray(inp["cmp_w2_v"], np.float32)
    o["pek"] = np.ascontiguousarray(np.asarray(inp["cmp_pe_k"], np.float32).reshape(16, 128).T)
    o["pev"] = np.ascontiguousarray(np.asarray(inp["cmp_pe_v"], np.float32).reshape(16, 128).T)
    o["wun"] = np.asarray(inp["w_up_nsa"], np.float32)
    o["wud"] = np.asarray(inp["w_up_dil"], np.float32)
    o["wo"] = np.asarray(inp["w_o"], np.float32)
    o["wq"] = np.asarray(inp["peer_wq"], np.float32)
    sk = np.asarray(inp["peer_subkeys"], np.float32)
    skbd = np.zeros((128, 256), np.float32)
    skbd[0:64, 0:128] = sk[0].T
    skbd[64:128, 128:256] = sk[1].T
    o["skbd"] = skbd
    u = np.asarray(inp["peer_u"], np.float32)
    o["ut"] = np.ascontiguousarray(u.reshape(128, 128, 8, 128).transpose(0, 3, 2, 1)).reshape(128, 128, 1024)
    o["pv"] = np.asarray(inp["peer_v"], np.float32).reshape(128, 128, 1024)
    return o


_CACHE = {}


def kernel(**inputs):
    if "nc" not in _CACHE:
        _CACHE["nc"] = build_program()[0]
        _CACHE["consts"] = _consts()
    nc = _CACHE["nc"]
    shared = dict(_CACHE["consts"])
    shared.update(_layout_weights(inputs))
    x = np.asarray(inputs["x"], np.float32)
    in_maps = []
    for b in range(8):
        m = dict(shared)
        m["x"] = np.ascontiguousarray(x[b])
        in_maps.append(m)
    res = run_bass_kernel_spmd(nc, in_maps, core_ids=list(range(8)))
    return np.stack([np.asarray(r["out"], np.float32) for r in res.results], axis=0)
```

```python
import contextlib
import math
import numpy as np
import ml_dtypes
import concourse.bass as bass
import concourse.mybir as mybir
from concourse.bass_utils import run_bass_kernel_spmd

F32 = mybir.dt.float32
BF16 = mybir.dt.bfloat16
ALU = mybir.AluOpType
AF = mybir.ActivationFunctionType
AX = mybir.AxisListType

S = 4096
D = 1024
NT = S // 128
NB = S // 512
EPS = 1e-6


class Slot:
    __slots__ = ("name", "writers", "readers", "dcount")

    def __init__(self, name):
        self.name = name
        self.writers = {}
        self.readers = {}
        self.dcount = 0


class Op:
    __slots__ = ("eng", "fn", "deps", "signal", "value", "key", "is_dma")

    def __init__(self, eng, fn, key, is_dma=False, value=None):
        self.eng = eng
        self.fn = fn
        self.deps = []
        self.signal = False
        self.value = value
        self.key = key
        self.is_dma = is_dma


class Prog:
    COMPUTE = ("pe", "act", "dve", "pool")

    def __init__(self, nc):
        self.nc = nc
        self.ops = {e: [] for e in ("pe", "act", "dve", "pool", "sp")}
        self.slots = {}
        self.last = {}
        self.fence_deps = {e: [] for e in self.ops}
        self.n_ops = 0

    def slot(self, name):
        s = self.slots.get(name)
        if s is None:
            s = self.slots[name] = Slot(name)
        return s

    def _track(self, op, reads, writes):
        deps = op.deps
        fd = self.fence_deps[op.eng]
        if fd:
            deps.extend(fd)
            self.fence_deps[op.eng] = []
        for r in reads:
            s = self.slot(r)
            deps.extend(s.writers.values())
            s.readers[op.key] = op
        for w in writes:
            s = self.slot(w)
            deps.extend(s.readers.values())
            deps.extend(s.writers.values())
            if any(o is not op for o in s.readers.values()):
                s.writers = {op.key: op}
                s.readers = {}
            else:
                s.readers = {}
                s.writers[op.key] = op
        op.deps = [d for d in deps if d is not op and (d.key != op.key or (not op.is_dma and op.eng != "pe"))]
        self.last[op.key] = op
        self.n_ops += 1

    def add(self, eng, fn, reads=(), writes=()):
        op = Op(eng, fn, eng)
        self.ops[eng].append(op)
        self._track(op, reads, writes)
        return op

    def dma(self, fn, reads=(), writes=(), queue="sp"):
        assert len(writes) == 1
        s = self.slot(writes[0])
        s.dcount += 1
        op = Op(queue, fn, ("d", s.name), is_dma=True, value=16 * s.dcount)
        self.ops[queue].append(op)
        self._track(op, reads, writes)
        return op

    def fence(self):
        allops = list(self.last.values())
        for e in self.fence_deps:
            self.fence_deps[e] = list(allops)

    def emit(self, final_slots=()):
        nc = self.nc
        fin = Op("sp", None, "fin")
        for name in final_slots:
            fin.deps.extend(self.slot(name).writers.values())
        self.ops["sp"].append(fin)
        for e, lst in self.ops.items():
            for op in lst:
                for d in op.deps:
                    if not d.is_dma:
                        d.signal = True
        for e in self.COMPUTE:
            c = 0
            for op in self.ops[e]:
                if op.signal and not op.is_dma:
                    c += 1
                    op.value = c
        keys = list(self.COMPUTE)
        for lst in self.ops.values():
            for op in lst:
                if op.is_dma and op.key not in keys:
                    keys.append(op.key)
        self.n_sems = len(keys)
        with contextlib.ExitStack() as st:
            sems = {}
            for i, k in enumerate(keys):
                sems[k] = st.enter_context(nc.semaphore("s%d" % i))
            block = st.enter_context(nc.Block())

            def run(eng_name, eng):
                waited = {}
                for op in self.ops[eng_name]:
                    need = {}
                    for d in op.deps:
                        v = d.value
                        if v > need.get(d.key, 0):
                            need[d.key] = v
                    for k, v in need.items():
                        if waited.get(k, 0) < v:
                            eng.wait_ge(sems[k], v)
                            waited[k] = v
                    if op.fn is None:
                        continue
                    ins = op.fn(eng)
                    if op.is_dma:
                        ins.then_inc(sems[op.key], 16)
                    elif op.signal:
                        ins.then_inc(sems[op.key], 1)

            @block.sync
            def _(eng):
                run("sp", eng)

            @block.tensor
            def _(eng):
                run("pe", eng)

            @block.scalar
            def _(eng):
                run("act", eng)

            @block.vector
            def _(eng):
                run("dve", eng)

            @block.gpsimd
            def _(eng):
                run("pool", eng)


class Arena:
    def __init__(self, t, total_f32):
        self.t = t
        self.total = total_f32
        self.off = 0
        self.peak = 0

    def mark(self):
        return self.off

    def release(self, m):
        self.off = m

    def alloc(self, cols, dtype=F32):
        n32 = cols if dtype == F32 else (cols + 1) // 2
        a = self.off
        self.off += n32
        self.peak = max(self.peak, self.off)
        assert self.off <= self.total, ("SBUF arena overflow", self.off, self.total)
        v = self.t[:, a:a + n32]
        if dtype != F32:
            v = v.bitcast(dtype)[:, 0:cols]
        return v


class Ring:
    def __init__(self, items):
        self.items = list(items)
        self.i = 0

    def next(self):
        r = self.items[self.i % len(self.items)]
        self.i += 1
        return r


CID_Q = 0
CID_KC = 4
CID_VC = 5
CID_KS = 6
CID_KW = 8
CID_DIL = 10
NFM = 22
NTM = 1048
DIL_PAT = ((128, 1), (512, 4), (2048, 16))


def build_program(debug=False, stop_after=None):
    nc = bass.Bass("TRN2", target_bir_lowering=False)

    def din(name, shape, dt=F32):
        return nc.dram_tensor(name, list(shape), dt, kind="ExternalInput").ap()

    skind = "ExternalOutput" if debug else "Internal"

    def dscr(name, shape, dt):
        return nc.dram_tensor(name, list(shape), dt, kind=skind).ap()

    x_d = din("x", [S, D])
    wfm_d = din("wfm", [D, NFM * 128])
    wtm_d = din("wtm", [D, NTM])
    wmg_d = din("wmg", [D, 2048])
    g1_d = din("g1", [128, 8])
    g2_d = din("g2", [128, 8])
    gains_d = din("gains", [128, NFM + 1])
    w1k_d = din("w1k", [2048, 256])
    w1v_d = din("w1v", [2048, 256])
    w2k_d = din("w2k", [256, 64])
    w2v_d = din("w2v", [256, 64])
    pek_d = din("pek", [128, 16])
    pev_d = din("pev", [128, 16])
    wun_d = din("wun", [512, D])
    wud_d = din("wud", [256, D])
    wo_d = din("wo", [D, D])
    wq_d = din("wq", [D, D])
    skbd_d = din("skbd", [128, 256])
    ut_d = din("ut", [128, 128, 1024])
    v_d = din("pv", [128, 128, 1024])
    ident_d = din("ident", [128, 128], BF16)
    bd_d = din("bdones", [128, 128], BF16)
    rot_d = din("rotT", [128, 128], BF16)
    cos_d = din("cosT", [128, S])
    sin_d = din("sinT", [128, S])
    cosc_d = din("cosC", [128, 256])
    sinc_d = din("sinC", [128, 256])
    cmask_d = din("cmask", [2, 128, S], BF16)
    masks_d = din("masks", [128, 9, 128], BF16)
    ov_d = din("ov", [2, 128, 64], BF16)
    selmul_d = din("selmul", [S, 64])
    seladd_d = din("seladd", [S, 64])
    esel_d = din("esel", [64, S], BF16)
    out_d = nc.dram_tensor("out", [S, D], F32, kind="ExternalOutput").ap()

    qkt_s = dscr("qkt_s", [NFM, 128, S], BF16)
    vtm_s = dscr("vtm_s", [S, 1024], BF16)
    gates_s = dscr("gates_s", [S, 24], F32)
    ht_s = dscr("ht_s", [8, 128, S], BF16)
    yt_s = dscr("yt_s", [6, 128, S], BF16)
    x2_s = dscr("x2_s", [S, D], F32)
    h2t_s = dscr("h2t_s", [8, 128, S], BF16)
    utb_s = dscr("utb_s", [128, 128, 1024], BF16)
    vb_s = dscr("vb_s", [128, 128, 1024], BF16)

    TOT = 53100
    with contextlib.ExitStack() as st:
        at = st.enter_context(nc.sbuf_tensor("arena", [128, TOT], F32))
        pst = [st.enter_context(nc.psum_tensor("ps%d" % i, [128, 512], F32)) for i in range(8)]
        ps = [t.ap() for t in pst]
        psb = [t.ap().bitcast(BF16) for t in pst]
        A = Arena(at, TOT)
        P = Prog(nc)
        uid = [0]

        def nm(prefix):
            uid[0] += 1
            return "%s_%d" % (prefix, uid[0])

        def mkring(prefix, n, cols, dtype=F32):
            return Ring([(nm(prefix), A.alloc(cols, dtype)) for _ in range(n)])

        ident = A.alloc(128, BF16)
        bdones = A.alloc(128, BF16)
        rotT = A.alloc(128, BF16)
        masks = A.alloc(9 * 128, BF16).rearrange("p (a b) -> p a b", a=9)
        P.dma(lambda e: e.dma_start(out=ident, in_=ident_d), writes=["ident"])
        P.dma(lambda e: e.dma_start(out=bdones, in_=bd_d), writes=["bdones"])
        P.dma(lambda e: e.dma_start(out=rotT, in_=rot_d), writes=["rotT"])
        P.dma(lambda e: e.dma_start(out=masks, in_=masks_d), writes=["masks"])
        gains = A.alloc(NFM + 1)
        P.dma(lambda e: e.dma_start(out=gains, in_=gains_d), writes=["gains"])
        persist_mark = A.mark()

        def norm_rope(zps_name, zps, n, gain_ap, cos_ap, sin_ap, cos_slots, out_name, out_ap, R):
            sqn, sq = R["sq"].next()
            P.add("act", lambda e: e.activation(out=sq[:, 0:n], in_=zps, func=AF.Square), reads=[zps_name], writes=[sqn])
            P.add("pe", lambda e: e.matmul(ps[3][:, 0:n], lhsT=bdones, rhs=sq[:, 0:n], start=True, stop=True), reads=[sqn, "bdones"], writes=["ps3"])
            rsn, rs = R["rs"].next()
            P.add("act", lambda e: e.activation(out=rs[:, 0:n], in_=ps[3][:, 0:n], func=AF.Sqrt, scale=1.0 / 64, bias=EPS), reads=["ps3"], writes=[rsn])
            P.add("dve", lambda e: e.reciprocal(out=rs[:, 0:n], in_=rs[:, 0:n]), reads=[rsn], writes=[rsn])
            znn, zn = R["zn"].next()
            P.add("dve", lambda e: e.scalar_tensor_tensor(out=zn[:, 0:n], in0=zps, scalar=gain_ap, in1=rs[:, 0:n], op0=ALU.mult, op1=ALU.mult), reads=[zps_name, rsn, "gains"], writes=[znn])
            zbn, zb = R["zb"].next()
            P.add("act", lambda e: e.activation(out=zb[:, 0:n], in_=zn[:, 0:n], func=AF.Copy), reads=[znn], writes=[zbn])
            P.add("pe", lambda e: e.matmul(ps[4][:, 0:n], lhsT=rotT, rhs=zb[:, 0:n], start=True, stop=True), reads=[zbn, "rotT"], writes=["ps4"])
            P.add("dve", lambda e: e.tensor_tensor(out=zn[:, 0:n], in0=zn[:, 0:n], in1=cos_ap, op=ALU.mult), reads=[znn] + cos_slots, writes=[znn])
            t2n, t2 = R["t2"].next()
            P.add("dve", lambda e: e.tensor_tensor(out=t2[:, 0:n], in0=ps[4][:, 0:n], in1=sin_ap, op=ALU.mult), reads=["ps4"] + cos_slots, writes=[t2n])
            P.add("dve", lambda e: e.tensor_tensor(out=out_ap, in0=zn[:, 0:n], in1=t2[:, 0:n], op=ALU.add), reads=[znn, t2n], writes=[out_name])

        m0 = A.mark()
        stg = mkring("pstg", 2, 4096)
        stb = mkring("pstb", 2, 4096, BF16)
        k = 0
        for src, dst in ((ut_d, utb_s), (v_d, vb_s)):
            for i in range(32):
                sn, sa = stg.next()
                bn, ba = stb.next()
                P.dma(lambda e, sa=sa, src=src, i=i: e.dma_start(out=sa.rearrange("p (a b) -> p a b", a=4), in_=src[4 * i:4 * i + 4].rearrange("a p c -> p a c")), writes=[sn])
                eng = ("dve", "act", "pool")[k % 3]
                k += 1
                if eng == "act":
                    P.add("act", lambda e, sa=sa, ba=ba: e.activation(out=ba, in_=sa, func=AF.Copy), reads=[sn], writes=[bn])
                else:
                    P.add(eng, lambda e, sa=sa, ba=ba: e.tensor_copy(out=ba, in_=sa), reads=[sn], writes=[bn])
                P.dma(lambda e, ba=ba, dst=dst, i=i: e.dma_start(out=dst[4 * i:4 * i + 4].rearrange("a p c -> p a c"), in_=ba.rearrange("p (a b) -> p a b", a=4)), reads=[bn], writes=["utb_s" if dst is utb_s else "vb_s"], queue="pool")
        A.release(m0)
        P.fence()

        m0 = A.mark()
        wfm = A.alloc(8 * NFM * 128, BF16).rearrange("p (a b) -> p a b", a=8)
        wtm = A.alloc(8 * NTM, BF16).rearrange("p (a b) -> p a b", a=8)
        g1t = A.alloc(8)
        cosT = A.alloc(S)
        sinT = A.alloc(S)
        P.dma(lambda e: e.dma_start(out=g1t, in_=g1_d), writes=["g1t"])
        P.dma(lambda e: e.dma_start(out=cosT, in_=cos_d), writes=["cosT"])
        P.dma(lambda e: e.dma_start(out=sinT, in_=sin_d), writes=["sinT"])
        m1 = A.mark()
        wst = mkring("wst", 2, NFM * 128)
        for kc in range(8):
            sn, sa = wst.next()
            P.dma(lambda e, sa=sa, kc=kc: e.dma_start(out=sa, in_=wfm_d[kc * 128:(kc + 1) * 128, :]), writes=[sn])
            P.add("dve", lambda e, sa=sa, kc=kc: e.tensor_scalar(out=wfm[:, kc, :], in0=sa, scalar1=g1t[:, kc:kc + 1], scalar2=None, op0=ALU.mult), reads=[sn, "g1t"], writes=["wfm"])
        for kc in range(8):
            sn, sa = wst.next()
            P.dma(lambda e, sa=sa, kc=kc: e.dma_start(out=sa[:, 0:NTM], in_=wtm_d[kc * 128:(kc + 1) * 128, :]), writes=[sn])
            P.add("dve", lambda e, sa=sa, kc=kc: e.tensor_scalar(out=wtm[:, kc, :], in0=sa[:, 0:NTM], scalar1=g1t[:, kc:kc + 1], scalar2=None, op0=ALU.mult), reads=[sn, "g1t"], writes=["wtm"])
        A.release(m1)
        P.fence()
        xr = mkring("xt", 2, 1024)
        junk = A.alloc(1024)
        ssr = mkring("ss", 2, 1)
        hbr = mkring("hb", 2, 1024, BF16)
        hTr = mkring("hTb", 2, 8 * 512, BF16)
        R = {"sq": mkring("sq", 2, 512, BF16), "rs": mkring("rs", 2, 512), "zn": mkring("zn", 2, 512),
             "zb": mkring("zb", 2, 512, BF16), "t2": mkring("t2", 2, 512)}
        fmo = mkring("fmo", 3, 512, BF16)
        tmo = mkring("tmo", 2, 1024, BF16)
        gto = mkring("gto", 2, 24)
        fmps = Ring([1, 2])
        for b in range(NB):
            hTn, hTb_ = hTr.next()
            hTb = hTb_.rearrange("p (a b) -> p a b", a=8)
            c0 = b * 512
            for u in range(4):
                t0 = c0 + u * 128
                xn, xt = xr.next()
                P.dma(lambda e, xt=xt, t0=t0: e.dma_start(out=xt, in_=x_d[t0:t0 + 128, :]), writes=[xn])
                sn, ss = ssr.next()
                P.add("act", lambda e, xt=xt, ss=ss: e.activation(out=junk, in_=xt, func=AF.Square, accum_out=ss), reads=[xn], writes=["junk", sn])
                P.add("act", lambda e, ss=ss: e.activation(out=ss, in_=ss, func=AF.Sqrt, scale=1.0 / D, bias=EPS), reads=[sn], writes=[sn])
                P.add("dve", lambda e, ss=ss: e.reciprocal(out=ss, in_=ss), reads=[sn], writes=[sn])
                hn_, hb = hbr.next()
                P.add("dve", lambda e, xt=xt, ss=ss, hb=hb: e.tensor_scalar(out=hb, in0=xt, scalar1=ss, scalar2=None, op0=ALU.mult), reads=[xn, sn], writes=[hn_])
                for c in range(8):
                    P.add("pe", lambda e, c=c, hb=hb: e.transpose(out=psb[0][:, c * 128:(c + 1) * 128], in_=hb[:, c * 128:(c + 1) * 128], identity=ident), reads=[hn_, "ident"], writes=["ps0"])
                P.add("act", lambda e, u=u, hTb=hTb: e.activation(out=hTb[:, :, u * 128:(u + 1) * 128], in_=psb[0].rearrange("p (a b) -> p a b", a=8), func=AF.Copy), reads=["ps0"], writes=[hTn])
            P.dma(lambda e, hTb=hTb, c0=c0: e.dma_start(out=ht_s[:, :, c0:c0 + 512].rearrange("c p t -> p c t"), in_=hTb), reads=[hTn], writes=["ht_s"], queue="pool")
            for cid in range(NFM):
                pi = fmps.next()
                for kc in range(8):
                    P.add("pe", lambda e, pi=pi, kc=kc, cid=cid, hTb=hTb: e.matmul(ps[pi], lhsT=wfm[:, kc, cid * 128:(cid + 1) * 128], rhs=hTb[:, kc, :], start=(kc == 0), stop=(kc == 7)), reads=["wfm", hTn], writes=["ps%d" % pi])
                on, oa = fmo.next()
                if cid in (CID_KC, CID_VC):
                    P.add("act", lambda e, pi=pi, oa=oa: e.activation(out=oa, in_=ps[pi], func=AF.Copy), reads=["ps%d" % pi], writes=[on])
                else:
                    norm_rope("ps%d" % pi, ps[pi], 512, gains[:, cid:cid + 1], cosT[:, c0:c0 + 512], sinT[:, c0:c0 + 512], ["cosT", "sinT"], on, oa, R)
                P.dma(lambda e, oa=oa, cid=cid, c0=c0: e.dma_start(out=qkt_s[cid, :, c0:c0 + 512], in_=oa), reads=[on], writes=["qkt_s"], queue="pool")
            for u in range(4):
                t0 = c0 + u * 128
                for r, (a0, a1) in enumerate(((0, 512), (512, 1024), (1024, NTM))):
                    for kc in range(8):
                        P.add("pe", lambda e, r=r, kc=kc, u=u, a0=a0, a1=a1, hTb=hTb: e.matmul(ps[5 + r][:, 0:a1 - a0], lhsT=hTb[:, kc, u * 128:(u + 1) * 128], rhs=wtm[:, kc, a0:a1], start=(kc == 0), stop=(kc == 7)), reads=["wtm", hTn], writes=["ps%d" % (5 + r)])
                tn, ta = tmo.next()
                P.add("act", lambda e, ta=ta: e.activation(out=ta[:, 0:512], in_=ps[5], func=AF.Copy), reads=["ps5"], writes=[tn])
                P.add("dve", lambda e, ta=ta: e.tensor_copy(out=ta[:, 512:1024], in_=ps[6]), reads=["ps6"], writes=[tn])
                P.dma(lambda e, ta=ta, t0=t0: e.dma_start(out=vtm_s[t0:t0 + 128, :], in_=ta), reads=[tn], writes=["vtm_s"], queue="pool")
                gn, ga = gto.next()
                P.add("act", lambda e, ga=ga: e.activation(out=ga, in_=ps[7][:, 0:24], func=AF.Sigmoid), reads=["ps7"], writes=[gn])
                P.dma(lambda e, ga=ga, t0=t0: e.dma_start(out=gates_s[t0:t0 + 128, :], in_=ga), reads=[gn], writes=["gates_s"], queue="pool")
        A.release(m0)
        P.fence()
        if stop_after == "A":
            P.emit(final_slots=["qkt_s", "vtm_s", "gates_s", "ht_s", "utb_s", "vb_s"])
            return nc, P, A

        mB = A.mark()
        kcmp = A.alloc(2 * 256, BF16).rearrange("p (g c) -> p g c", g=2)
        vc1 = A.alloc(2 * 2 * 129, BF16).rearrange("p (t g c) -> p t g c", t=2, g=2)
        P.add("dve", lambda e: e.memset(kcmp, 0.0), writes=["kcmp"])
        P.add("dve", lambda e: e.memset(vc1, 0.0), writes=["vc1"])
        P.add("dve", lambda e: e.memset(vc1[:, 0, :, 64:65], 1.0), writes=["vc1"])
        P.add("dve", lambda e: e.memset(vc1[0:127, 1, :, 64:65], 1.0), writes=["vc1"])
        for ct in range(2):
            for g in range(2):
                P.dma(lambda e, ct=ct, g=g: e.dma_start(out=vc1[:, ct, g, 65:129], in_=ov_d[ct]), writes=["vc1"])
        mB1 = A.mark()
        x2c = A.alloc(2 * 2 * S, BF16).rearrange("p (k g t) -> p k g t", k=2, g=2)
        P.add("pool", lambda e: e.memset(x2c[:, :, :, S - 1:S], 0.0), writes=["x2c"])
        for kv in range(2):
            for g in range(2):
                P.dma(lambda e, kv=kv, g=g: e.dma_start(out=x2c[0:64, kv, g, :], in_=qkt_s[CID_KC + kv, g * 64:(g + 1) * 64, :]), reads=["qkt_s"], writes=["x2c"])
                P.dma(lambda e, kv=kv, g=g: e.dma_start(out=x2c[64:128, kv, g, 0:S - 1], in_=qkt_s[CID_KC + kv, g * 64:(g + 1) * 64, 1:S]), reads=["qkt_s"], writes=["x2c"])
        w1s = A.alloc(16 * 256).rearrange("p (a h) -> p a h", a=16)
        w1b = A.alloc(2 * 16 * 256, BF16).rearrange("p (k a h) -> p k a h", k=2, a=16)
        pes = A.alloc(32)
        peb = A.alloc(32, BF16)
        w2s = A.alloc(2 * 2 * 64).rearrange("p (k c d) -> p k c d", k=2, c=2)
        w2kd = A.alloc(2 * 128, BF16).rearrange("p (c d) -> p c d", c=2)
        w2vb = A.alloc(2 * 64, BF16).rearrange("p (c d) -> p c d", c=2)
        cosC = A.alloc(256)
        sinC = A.alloc(256)
        P.dma(lambda e: e.dma_start(out=cosC, in_=cosc_d), writes=["cosC"])
        P.dma(lambda e: e.dma_start(out=sinC, in_=sinc_d), writes=["sinC"])
        P.dma(lambda e: e.dma_start(out=pes[:, 0:16], in_=pek_d), writes=["pes"])
        P.dma(lambda e: e.dma_start(out=pes[:, 16:32], in_=pev_d), writes=["pes"])
        P.add("dve", lambda e: e.tensor_copy(out=peb, in_=pes), reads=["pes"], writes=["peb"])
        for kv, (w1d, w2d) in enumerate(((w1k_d, w2k_d), (w1v_d, w2v_d))):
            P.dma(lambda e, w1d=w1d: e.dma_start(out=w1s, in_=w1d.rearrange("(a p) h -> p a h", p=128)), writes=["w1s"])
            P.add("dve", lambda e, kv=kv: e.tensor_copy(out=w1b[:, kv], in_=w1s), reads=["w1s"], writes=["w1b"])
            P.dma(lambda e, kv=kv, w2d=w2d: e.dma_start(out=w2s[:, kv], in_=w2d.rearrange("(c p) d -> p c d", p=128)), writes=["w2s"])
        P.add("dve", lambda e: e.tensor_copy(out=w2kd[:, :, 0:64], in_=w2s[:, 0]), reads=["w2s"], writes=["w2kd"])
        P.add("dve", lambda e: e.tensor_copy(out=w2kd[:, :, 64:128], in_=w2s[:, 0]), reads=["w2s"], writes=["w2kd"])
        P.add("dve", lambda e: e.tensor_copy(out=w2vb, in_=w2s[:, 1]), reads=["w2s"], writes=["w2vb"])
        biasT = A.alloc(4)
        gT = A.alloc(2 * 256, BF16).rearrange("p (c n) -> p c n", c=2)
        RB = {"sq": mkring("sqB", 1, 256, BF16), "rs": mkring("rsB", 1, 256), "zn": mkring("znB", 1, 256),
              "zb": mkring("zbB", 1, 256, BF16), "t2": mkring("t2B", 1, 256)}
        for kv in range(2):
            for hc in range(2):
                for a in range(16):
                    P.add("pe", lambda e, kv=kv, hc=hc, a=a: e.matmul(ps[2][:, 0:1], lhsT=w1b[:, kv, a, hc * 128:(hc + 1) * 128], rhs=peb[:, kv * 16 + a:kv * 16 + a + 1], start=(a == 0), stop=(a == 15)), reads=["w1b", "peb"], writes=["ps2"])
                P.add("dve", lambda e, kv=kv, hc=hc: e.tensor_copy(out=biasT[:, kv * 2 + hc:kv * 2 + hc + 1], in_=ps[2][:, 0:1]), reads=["ps2"], writes=["biasT"])
        for kv in range(2):
            for g in range(2):
                P.add("dve", lambda e: e.memset(gT, 0.0), writes=["gT"])
                for hc in range(2):
                    pi = hc
                    for a in range(16):
                        P.add("pe", lambda e, kv=kv, g=g, hc=hc, a=a, pi=pi: e.matmul(ps[pi][:, 0:255], lhsT=w1b[:, kv, a, hc * 128:(hc + 1) * 128], rhs=x2c[:, kv, g, 2 * a:2 * a + 16 * 254 + 1:16], start=(a == 0), stop=(a == 15)), reads=["w1b", "x2c"], writes=["ps%d" % pi])
                    P.add("act", lambda e, kv=kv, hc=hc, pi=pi: e.activation(out=gT[:, hc, 0:255], in_=ps[pi][:, 0:255], func=AF.Gelu_apprx_tanh, bias=biasT[:, kv * 2 + hc:kv * 2 + hc + 1]), reads=["ps%d" % pi, "biasT"], writes=["gT"])
                if kv == 0:
                    for hc in range(2):
                        P.add("pe", lambda e, hc=hc: e.matmul(ps[5][:, 0:256], lhsT=w2kd[:, hc, :], rhs=gT[:, hc, :], start=(hc == 0), stop=(hc == 1)), reads=["w2kd", "gT"], writes=["ps5"])
                    norm_rope("ps5", ps[5][:, 0:256], 256, gains[:, NFM:NFM + 1], cosC, sinC, ["cosC", "sinC"], "kcmp", kcmp[:, g, :], RB)
                    P.add("dve", lambda e, g=g: e.memset(kcmp[:, g, 255:256], 0.0), writes=["kcmp"])
                else:
                    for ct in range(2):
                        for hc in range(2):
                            P.add("pe", lambda e, hc=hc, ct=ct: e.matmul(ps[6][:, 0:64], lhsT=gT[:, hc, ct * 128:(ct + 1) * 128], rhs=w2vb[:, hc, :], start=(hc == 0), stop=(hc == 1)), reads=["w2vb", "gT"], writes=["ps6"])
                        P.add("act", lambda e, g=g, ct=ct: e.activation(out=vc1[:, ct, g, 0:64], in_=ps[6][:, 0:64], func=AF.Copy), reads=["ps6"], writes=["vc1"])
        A.release(mB1)
        P.fence()

        Sps = Ring([0, 1])

        def banded(qb, sources, o_of_u, o_slot, er, o_clear, mul_of_kt=None):
            P.add("dve", lambda e: e.memset(o_clear, 0.0), writes=[o_slot])
            items = []
            for si, src in enumerate(sources):
                dmax = src[6]
                for kt in range(max(0, 4 * qb - dmax), 4 * qb + 4):
                    u0 = max(0, kt - 4 * qb)
                    u1 = min(3, kt + dmax - 4 * qb)
                    if u0 <= u1:
                        items.append((si, kt, u0, u1))
            first = {}
            last = {}
            for idx, (si, kt, u0, u1) in enumerate(items):
                for u in range(u0, u1 + 1):
                    first.setdefault(u, idx)
                    last[u] = idx
            def front(idx):
                si, kt, u0, u1 = items[idx]
                qf, qs, kf, ks, vf, vs, dmax, mf = sources[si]
                n = (u1 - u0 + 1) * 128
                cq = qb * 512 + u0 * 128
                pi = Sps.next()
                P.add("pe", lambda e, pi=pi, kf=kf, kt=kt, qf=qf, cq=cq, n=n: e.matmul(ps[pi][:, 0:n], lhsT=kf(kt), rhs=qf(cq, n), start=True, stop=True), reads=list(qs) + list(ks), writes=["ps%d" % pi])
                en, ea = er.next()
                P.add("act", lambda e, pi=pi, ea=ea, n=n: e.activation(out=ea[:, 0:n], in_=ps[pi][:, 0:n], func=AF.Exp, scale=0.125), reads=["ps%d" % pi], writes=[en])
                if mul_of_kt is not None:
                    mn, ma = mul_of_kt(kt)
                    P.add("dve", lambda e, ea=ea, ma=ma, n=n, u0=u0: e.tensor_tensor(out=ea[:, 0:n], in0=ea[:, 0:n], in1=ma[:, u0 * 128:u0 * 128 + n], op=ALU.mult), reads=[en, mn], writes=[en])
                for u in range(u0, u1 + 1):
                    dl = 4 * qb + u - kt
                    mi = mf(dl)
                    lo = (u - u0) * 128
                    if mi is not None:
                        P.add("dve", lambda e, ea=ea, lo=lo, mi=mi: e.tensor_tensor(out=ea[:, lo:lo + 128], in0=ea[:, lo:lo + 128], in1=masks[:, mi, :], op=ALU.mult), reads=[en, "masks"], writes=[en])
                return en, ea

            def back(idx, en, ea):
                si, kt, u0, u1 = items[idx]
                qf, qs, kf, ks, vf, vs, dmax, mf = sources[si]
                for u in range(u0, u1 + 1):
                    lo = (u - u0) * 128
                    P.add("pe", lambda e, ea=ea, lo=lo, u=u, vf=vf, kt=kt, idx=idx: e.matmul(o_of_u(u), lhsT=ea[:, lo:lo + 128], rhs=vf(kt), start=False, stop=(last[u] == idx), skip_group_check=True), reads=[en] + list(vs), writes=[o_slot])

            pend = front(0) if items else None
            for idx in range(len(items)):
                nxt = front(idx + 1) if idx + 1 < len(items) else None
                back(idx, *pend)
                pend = nxt

        mC = A.mark()
        gat = A.alloc(NT * 24).rearrange("p (k c) -> p k c", k=NT)
        for i in range(4):
            P.dma(lambda e, i=i: e.dma_start(out=gat[:, 8 * i:8 * i + 8, :], in_=gates_s[1024 * i:1024 * (i + 1), :].rearrange("(k p) c -> p k c", p=128)), reads=["gates_s"], writes=["gat"])
        cmask = A.alloc(2 * S, BF16).rearrange("p (a t) -> p a t", a=2)
        P.dma(lambda e: e.dma_start(out=cmask, in_=cmask_d.rearrange("a p t -> p a t")), writes=["cmask"])
        selmul = A.alloc(NT * 64).rearrange("p (k c) -> p k c", k=NT)
        seladd = A.alloc(NT * 64).rearrange("p (k c) -> p k c", k=NT)
        for i in range(4):
            P.dma(lambda e, i=i: e.dma_start(out=selmul[:, 8 * i:8 * i + 8, :], in_=selmul_d[1024 * i:1024 * (i + 1), :].rearrange("(k p) c -> p k c", p=128)), writes=["selmul"])
            P.dma(lambda e, i=i: e.dma_start(out=seladd[:, 8 * i:8 * i + 8, :], in_=seladd_d[1024 * i:1024 * (i + 1), :].rearrange("(k p) c -> p k c", p=128)), writes=["seladd"])
        esel = A.alloc(S, BF16)
        P.dma(lambda e: e.dma_start(out=esel[0:64, :], in_=esel_d), writes=["esel"])
        Qn = A.alloc(2 * S, BF16).rearrange("p (c t) -> p c t", c=2)
        KS = A.alloc(S, BF16)
        KW = A.alloc(S, BF16)
        VS1 = A.alloc(NT * 65, BF16).rearrange("p (k c) -> p k c", k=NT)
        VW1 = A.alloc(NT * 65, BF16).rearrange("p (k c) -> p k c", k=NT)
        P.add("dve", lambda e: e.memset(VS1[:, :, 64:65], 1.0), writes=["VS1"])
        P.add("dve", lambda e: e.memset(VW1[:, :, 64:65], 1.0), writes=["VW1"])
        er = mkring("E", 3, 512, BF16)
        msbr = mkring("Msb", 2, 512, BF16)
        impacc = A.alloc(4 * 64).rearrange("p (u c) -> p u c", u=4)
        Y = A.alloc(4 * 256).rearrange("p (u c) -> p u c", u=4)
        Yb = A.alloc(4 * 256, BF16).rearrange("p (u c) -> p u c", u=4)
        rd = A.alloc(4)
        gd = A.alloc(4)
        sc = A.alloc(64)
        sct = A.alloc(64)
        m8 = A.alloc(16)
        selm = A.alloc(64, BF16)
        selT = A.alloc(512, BF16)
        yst = mkring("yst", 2, 2 * 512, BF16)

        def wmask(dl):
            return 0 if dl == 0 else (1 if dl == 4 else None)

        def smask(dl):
            return 0 if dl == 0 else None

        def finish_branch(o_views, o_slots, qb, g, j, br, first_branch):
            hh = 4 * g + j
            for u in range(4):
                P.add("dve", lambda e, u=u: e.tensor_scalar(out=rd[:, u:u + 1], in0=o_views[u][:, 64:65], scalar1=1e-30, scalar2=None, op0=ALU.max), reads=o_slots, writes=["rd"])
            P.add("dve", lambda e: e.reciprocal(out=rd, in_=rd), reads=["rd"], writes=["rd"])
            P.add("dve", lambda e: e.tensor_tensor(out=gd, in0=rd, in1=gat[:, 4 * qb:4 * qb + 4, hh * 3 + br], op=ALU.mult), reads=["rd", "gat"], writes=["gd"])
            for u in range(4):
                if first_branch:
                    P.add("dve", lambda e, u=u: e.tensor_scalar(out=Y[:, u, j * 64:(j + 1) * 64], in0=o_views[u][:, 0:64], scalar1=gd[:, u:u + 1], scalar2=None, op0=ALU.mult), reads=o_slots + ["gd"], writes=["Y"])
                else:
                    P.add("dve", lambda e, u=u: e.scalar_tensor_tensor(out=Y[:, u, j * 64:(j + 1) * 64], in0=o_views[u][:, 0:64], scalar=gd[:, u:u + 1], in1=Y[:, u, j * 64:(j + 1) * 64], op0=ALU.mult, op1=ALU.add), reads=o_slots + ["gd", "Y"], writes=["Y"])

        for g in range(2):
            for c in range(2):
                P.dma(lambda e, c=c, g=g: e.dma_start(out=Qn[:, c, :], in_=qkt_s[CID_Q + 2 * g + c]), reads=["qkt_s"], writes=["Qn"])
            P.dma(lambda e, g=g: e.dma_start(out=KS, in_=qkt_s[CID_KS + g]), reads=["qkt_s"], writes=["KS"])
            P.dma(lambda e, g=g: e.dma_start(out=KW, in_=qkt_s[CID_KW + g]), reads=["qkt_s"], writes=["KW"])
            for i in range(4):
                P.dma(lambda e, i=i, g=g: e.dma_start(out=VS1[:, 8 * i:8 * i + 8, 0:64], in_=vtm_s[1024 * i:1024 * (i + 1), g * 64:(g + 1) * 64].rearrange("(k p) c -> p k c", p=128)), reads=["vtm_s"], writes=["VS1"])
                P.dma(lambda e, i=i, g=g: e.dma_start(out=VW1[:, 8 * i:8 * i + 8, 0:64], in_=vtm_s[1024 * i:1024 * (i + 1), 128 + g * 64:128 + (g + 1) * 64].rearrange("(k p) c -> p k c", p=128)), reads=["vtm_s"], writes=["VW1"])
            for qb in range(NB):
                c0 = qb * 512
                cts = [0, 1] if qb >= 4 else [0]
                for j in range(4):
                    base = 64 * (j % 2)
                    cj = j // 2
                    oa = ps[2].rearrange("p (u c) -> p u c", u=2)
                    ob = ps[3].rearrange("p (u c) -> p u c", u=2)
                    ov_ = [oa[:, 0, 0:129], oa[:, 1, 0:129], ob[:, 0, 0:129], ob[:, 1, 0:129]]
                    osl = ["ps2", "ps2", "ps3", "ps3"]
                    P.add("dve", lambda e: e.memset(ps[2], 0.0), writes=["ps2"])
                    P.add("dve", lambda e: e.memset(ps[3], 0.0), writes=["ps3"])
                    for ct in cts:
                        pi = Sps.next()
                        P.add("pe", lambda e, pi=pi, ct=ct, base=base, cj=cj, g=g, c0=c0: e.matmul(ps[pi], lhsT=kcmp[base:base + 64, g, ct * 128:(ct + 1) * 128], rhs=Qn[base:base + 64, cj, c0:c0 + 512], start=True, stop=True), reads=["kcmp", "Qn"], writes=["ps%d" % pi])
                        en, ea = er.next()
                        P.add("act", lambda e, pi=pi, ea=ea: e.activation(out=ea, in_=ps[pi], func=AF.Exp, scale=0.125), reads=["ps%d" % pi], writes=[en])
                        P.add("dve", lambda e, ea=ea, ct=ct, c0=c0: e.tensor_tensor(out=ea, in0=ea, in1=cmask[:, ct, c0:c0 + 512], op=ALU.mult), reads=[en, "cmask"], writes=[en])
                        for u in range(4):
                            P.add("pe", lambda e, ea=ea, u=u, ct=ct, g=g, ov_=ov_, sp_=(ct == cts[-1]): e.matmul(ov_[u], lhsT=ea[:, u * 128:(u + 1) * 128], rhs=vc1[:, ct, g, :], start=False, stop=sp_, skip_group_check=True), reads=[en, "vc1"], writes=[osl[u]])
                    finish_branch(ov_, ["ps2", "ps3"], qb, g, j, 0, True)
                    for u in range(4):
                        if j == 0:
                            P.add("dve", lambda e, u=u: e.tensor_scalar(out=impacc[:, u, :], in0=ov_[u][:, 65:129], scalar1=rd[:, u:u + 1], scalar2=None, op0=ALU.mult), reads=["ps2", "ps3", "rd"], writes=["impacc"])
                        else:
                            P.add("dve", lambda e, u=u: e.scalar_tensor_tensor(out=impacc[:, u, :], in0=ov_[u][:, 65:129], scalar=rd[:, u:u + 1], in1=impacc[:, u, :], op0=ALU.mult, op1=ALU.add), reads=["ps2", "ps3", "rd", "impacc"], writes=["impacc"])
                for u in range(4):
                    tt = 4 * qb + u
                    P.add("dve", lambda e, u=u, tt=tt: e.tensor_tensor(out=sc, in0=impacc[:, u, :], in1=selmul[:, tt, :], op=ALU.mult), reads=["impacc", "selmul"], writes=["sc"])
                    P.add("dve", lambda e, tt=tt: e.tensor_tensor(out=sc, in0=sc, in1=seladd[:, tt, :], op=ALU.add), reads=["sc", "seladd"], writes=["sc"])
                    P.add("dve", lambda e: e.max(out=m8[:, 0:8], in_=sc), reads=["sc"], writes=["m8"])
                    P.add("dve", lambda e: e.match_replace(out=sct, in_to_replace=m8[:, 0:8], in_values=sc, imm_value=-1e30), reads=["sc", "m8"], writes=["sct"])
                    P.add("dve", lambda e: e.max(out=m8[:, 8:16], in_=sct), reads=["sct"], writes=["m8"])
                    P.add("dve", lambda e: e.tensor_scalar(out=selm, in0=sc, scalar1=m8[:, 15:16], scalar2=None, op0=ALU.is_ge), reads=["sc", "m8"], writes=["selm"])
                    P.add("pe", lambda e: e.transpose(out=psb[7][0:64, 0:128], in_=selm, identity=ident), reads=["selm", "ident"], writes=["ps7"])
                    P.add("act", lambda e, u=u: e.activation(out=selT[0:64, u * 128:(u + 1) * 128], in_=psb[7][0:64, 0:128], func=AF.Copy), reads=["ps7"], writes=["selT"])
                nkt = 4 * qb + 4
                for j in range(4):
                    base = 64 * (j % 2)
                    cj = j // 2
                    o5 = ps[5].rearrange("p (u c) -> p u c", u=4)
                    o6 = ps[6].rearrange("p (u c) -> p u c", u=4)
                    qf = lambda cq, n, base=base, cj=cj: Qn[base:base + 64, cj, cq:cq + n]

                    def mul_of_kt(kt):
                        mn, ma = msbr.next()
                        P.add("pe", lambda e, kt=kt: e.matmul(ps[4], lhsT=esel[0:64, kt * 128:(kt + 1) * 128], rhs=selT[0:64, :], start=True, stop=True), reads=["esel", "selT"], writes=["ps4"])
                        P.add("act", lambda e, ma=ma: e.activation(out=ma, in_=ps[4], func=AF.Copy), reads=["ps4"], writes=[mn])
                        return mn, ma

                    banded(qb, [(qf, ["Qn"], lambda kt, base=base: KS[base:base + 64, kt * 128:(kt + 1) * 128], ["KS"], lambda kt: VS1[:, kt, :], ["VS1"], 64, smask)],
                           lambda u: o5[:, u, 0:65], "ps5", er, ps[5], mul_of_kt=mul_of_kt)
                    finish_branch([o5[:, u, :] for u in range(4)], ["ps5"], qb, g, j, 1, False)
                    banded(qb, [(qf, ["Qn"], lambda kt, base=base: KW[base:base + 64, kt * 128:(kt + 1) * 128], ["KW"], lambda kt: VW1[:, kt, :], ["VW1"], 4, wmask)],
                           lambda u: o6[:, u, 0:65], "ps6", er, ps[6])
                    finish_branch([o6[:, u, :] for u in range(4)], ["ps6"], qb, g, j, 2, False)
                P.add("act", lambda e: e.activation(out=Yb, in_=Y, func=AF.Copy), reads=["Y"], writes=["Yb"])
                ysn, ys_ = yst.next()
                ys = ys_.rearrange("p (c t) -> p c t", c=2)
                for u in range(4):
                    for c in range(2):
                        P.add("pe", lambda e, u=u, c=c: e.transpose(out=psb[7][:, c * 128:(c + 1) * 128], in_=Yb[:, u, c * 128:(c + 1) * 128], identity=ident), reads=["Yb", "ident"], writes=["ps7"])
                    P.add("act", lambda e, u=u, ys=ys: e.activation(out=ys[:, :, u * 128:(u + 1) * 128], in_=psb[7][:, 0:256].rearrange("p (c t) -> p c t", c=2), func=AF.Copy), reads=["ps7"], writes=[ysn])
                P.dma(lambda e, ys=ys, g=g, c0=c0: e.dma_start(out=yt_s[2 * g:2 * g + 2, :, c0:c0 + 512].rearrange("c p t -> p c t"), in_=ys), reads=[ysn], writes=["yt_s"], queue="pool")
        A.release(mB)
        P.fence()
        if stop_after == "C":
            P.emit(final_slots=["yt_s"])
            return nc, P, A

        mD = A.mark()
        DQ = A.alloc(3 * S, BF16).rearrange("p (g t) -> p g t", g=3)
        DK = A.alloc(3 * S, BF16).rearrange("p (g t) -> p g t", g=3)
        DV = A.alloc(6 * NT * 65, BF16).rearrange("p (a k c) -> p a k c", a=6, k=NT)
        P.add("dve", lambda e: e.memset(DV[:, :, :, 64:65], 1.0), writes=["DV"])
        er = mkring("Ed", 3, 512, BF16)
        Yd = A.alloc(4 * 128).rearrange("p (u c) -> p u c", u=4)
        Ydb = A.alloc(4 * 128, BF16).rearrange("p (u c) -> p u c", u=4)
        rdd = A.alloc(4)
        ydst = mkring("ydst", 2, 512, BF16)
        ops_ring = Ring([5, 6])

        def dmask_fn(gi):
            d = DIL_PAT[gi][1]
            if d == 1:
                return lambda dl: 0 if dl == 0 else 2
            b = 3 if d == 4 else 6
            return lambda dl, b=b, d=d: (b + 1) if dl == 0 else ((b + 2) if dl == d else b)

        for pr in range(2):
            for gi in range(3):
                P.dma(lambda e, gi=gi, pr=pr: e.dma_start(out=DQ[:, gi, :], in_=qkt_s[CID_DIL + gi * 4 + pr]), reads=["qkt_s"], writes=["DQ"])
                P.dma(lambda e, gi=gi, pr=pr: e.dma_start(out=DK[:, gi, :], in_=qkt_s[CID_DIL + gi * 4 + 2 + pr]), reads=["qkt_s"], writes=["DK"])
                for hh in range(2):
                    col = 256 + (gi * 4 + pr * 2 + hh) * 64
                    for i in range(4):
                        P.dma(lambda e, i=i, gi=gi, hh=hh, col=col: e.dma_start(out=DV[:, gi * 2 + hh, 8 * i:8 * i + 8, 0:64], in_=vtm_s[1024 * i:1024 * (i + 1), col:col + 64].rearrange("(k p) c -> p k c", p=128)), reads=["vtm_s"], writes=["DV"])
            for qb in range(NB):
                c0 = qb * 512
                for hh in range(2):
                    base = 64 * hh
                    opi = ops_ring.next()
                    ov4 = ps[opi].rearrange("p (u c) -> p u c", u=4)
                    srcs = []
                    for gi in range(3):
                        srcs.append((lambda cq, n, gi=gi, base=base: DQ[base:base + 64, gi, cq:cq + n], ["DQ"],
                                     lambda kt, gi=gi, base=base: DK[base:base + 64, gi, kt * 128:(kt + 1) * 128], ["DK"],
                                     lambda kt, gi=gi, hh=hh: DV[:, gi * 2 + hh, kt, :], ["DV"], DIL_PAT[gi][1], dmask_fn(gi)))
                    banded(qb, srcs, lambda u, ov4=ov4: ov4[:, u, 0:65], "ps%d" % opi, er, ps[opi])
                    P.add("dve", lambda e, ov4=ov4: e.reciprocal(out=rdd, in_=ov4[:, :, 64]), reads=["ps%d" % opi], writes=["rdd"])
                    for u in range(4):
                        P.add("dve", lambda e, u=u, ov4=ov4, hh=hh: e.tensor_scalar(out=Ydb[:, u, hh * 64:(hh + 1) * 64], in0=ov4[:, u, 0:64], scalar1=rdd[:, u:u + 1], scalar2=None, op0=ALU.mult), reads=["ps%d" % opi, "rdd"], writes=["Ydb"])
                ysn, ys = ydst.next()
                for u in range(4):
                    P.add("pe", lambda e, u=u: e.transpose(out=psb[7][:, 0:128], in_=Ydb[:, u, :], identity=ident), reads=["Ydb", "ident"], writes=["ps7"])
                    P.add("act", lambda e, u=u, ys=ys: e.activation(out=ys[:, u * 128:(u + 1) * 128], in_=psb[7][:, 0:128], func=AF.Copy), reads=["ps7"], writes=[ysn])
                P.dma(lambda e, ys=ys, pr=pr, c0=c0: e.dma_start(out=yt_s[4 + pr, :, c0:c0 + 512], in_=ys), reads=[ysn], writes=["yt_s"], queue="pool")
        A.release(mD)
        P.fence()
        if stop_after == "D":
            P.emit(final_slots=["yt_s"])
            return nc, P, A

        mE = A.mark()
        wun = A.alloc(4 * D, BF16).rearrange("p (a b) -> p a b", a=4)
        wud = A.alloc(2 * D, BF16).rearrange("p (a b) -> p a b", a=2)
        wmg = A.alloc(8 * 2048, BF16).rearrange("p (a b) -> p a b", a=8)
        wo = A.alloc(8 * D, BF16).rearrange("p (a b) -> p a b", a=8)
        g1tE = A.alloc(8)
        P.dma(lambda e: e.dma_start(out=g1tE, in_=g1_d), writes=["g1tE"])
        mE1 = A.mark()
        wst = mkring("wstE", 2, 2048)
        for a in range(4):
            sn, sa = wst.next()
            P.dma(lambda e, sa=sa, a=a: e.dma_start(out=sa[:, 0:D], in_=wun_d[a * 128:(a + 1) * 128, :]), writes=[sn])
            P.add("dve", lambda e, sa=sa, a=a: e.tensor_copy(out=wun[:, a, :], in_=sa[:, 0:D]), reads=[sn], writes=["wun"])
        for a in range(2):
            sn, sa = wst.next()
            P.dma(lambda e, sa=sa, a=a: e.dma_start(out=sa[:, 0:D], in_=wud_d[a * 128:(a + 1) * 128, :]), writes=[sn])
            P.add("dve", lambda e, sa=sa, a=a: e.tensor_copy(out=wud[:, a, :], in_=sa[:, 0:D]), reads=[sn], writes=["wud"])
        for a in range(8):
            sn, sa = wst.next()
            P.dma(lambda e, sa=sa, a=a: e.dma_start(out=sa[:, 0:D], in_=wo_d[a * 128:(a + 1) * 128, :]), writes=[sn])
            P.add("dve", lambda e, sa=sa, a=a: e.tensor_copy(out=wo[:, a, :], in_=sa[:, 0:D]), reads=[sn], writes=["wo"])
        for a in range(8):
            sn, sa = wst.next()
            P.dma(lambda e, sa=sa, a=a: e.dma_start(out=sa, in_=wmg_d[a * 128:(a + 1) * 128, :]), writes=[sn])
            P.add("dve", lambda e, sa=sa, a=a: e.tensor_scalar(out=wmg[:, a, :], in0=sa, scalar1=g1tE[:, a:a + 1], scalar2=None, op0=ALU.mult), reads=[sn, "g1tE"], writes=["wmg"])
        A.release(mE1)
        P.fence()
        ytr = mkring("ytE", 2, 6 * 512, BF16)
        htr = mkring("htE", 2, 8 * 512, BF16)
        sg0 = A.alloc(512)
        sg1 = A.alloc(512)
        t1 = A.alloc(512)
        mT = A.alloc(8 * 512, BF16).rearrange("p (a b) -> p a b", a=8)
        xr = mkring("xtE", 2, 1024)
        x2r = mkring("x2E", 2, 1024)
        junkE = A.alloc(1024)
        ssr = mkring("ssE", 2, 1)
        hbr = mkring("hbE", 2, 1024, BF16)
        h2r = mkring("h2E", 2, 8 * 512, BF16)
        for b in range(NB):
            c0 = b * 512
            yn, ya_ = ytr.next()
            ya = ya_.rearrange("p (a b) -> p a b", a=6)
            hn, ha_ = htr.next()
            ha = ha_.rearrange("p (a b) -> p a b", a=8)
            P.dma(lambda e, ya=ya, c0=c0: e.dma_start(out=ya, in_=yt_s[:, :, c0:c0 + 512].rearrange("c p t -> p c t")), reads=["yt_s"], writes=[yn])
            P.dma(lambda e, ha=ha, c0=c0: e.dma_start(out=ha, in_=ht_s[:, :, c0:c0 + 512].rearrange("c p t -> p c t")), reads=["ht_s"], writes=[hn])
            for dc in range(8):
                dsl = slice(dc * 128, (dc + 1) * 128)
                for a in range(4):
                    P.add("pe", lambda e, a=a, dsl=dsl, ya=ya: e.matmul(ps[0], lhsT=wun[:, a, dsl], rhs=ya[:, a, :], start=(a == 0), stop=(a == 3)), reads=["wun", yn], writes=["ps0"])
                for a in range(2):
                    P.add("pe", lambda e, a=a, dsl=dsl, ya=ya: e.matmul(ps[1], lhsT=wud[:, a, dsl], rhs=ya[:, 4 + a, :], start=(a == 0), stop=(a == 1)), reads=["wud", yn], writes=["ps1"])
                for hf in range(2):
                    for a in range(8):
                        P.add("pe", lambda e, a=a, hf=hf, dc=dc, ha=ha: e.matmul(ps[2 + hf], lhsT=wmg[:, a, hf * 1024 + dc * 128:hf * 1024 + (dc + 1) * 128], rhs=ha[:, a, :], start=(a == 0), stop=(a == 7)), reads=["wmg", hn], writes=["ps%d" % (2 + hf)])
                P.add("act", lambda e: e.activation(out=sg0, in_=ps[2], func=AF.Sigmoid), reads=["ps2"], writes=["sg0"])
                P.add("act", lambda e: e.activation(out=sg1, in_=ps[3], func=AF.Sigmoid), reads=["ps3"], writes=["sg1"])
                P.add("dve", lambda e: e.tensor_tensor(out=t1, in0=sg0, in1=ps[0], op=ALU.mult), reads=["sg0", "ps0"], writes=["t1"])
                P.add("dve", lambda e: e.tensor_tensor(out=sg1, in0=sg1, in1=ps[1], op=ALU.mult), reads=["sg1", "ps1"], writes=["sg1"])
                P.add("dve", lambda e, dc=dc: e.tensor_tensor(out=mT[:, dc, :], in0=t1, in1=sg1, op=ALU.add), reads=["t1", "sg1"], writes=["mT"])
            h2n, h2_ = h2r.next()
            h2 = h2_.rearrange("p (a b) -> p a b", a=8)
            for u in range(4):
                t0 = c0 + u * 128
                for hf in range(2):
                    for a in range(8):
                        P.add("pe", lambda e, a=a, hf=hf, u=u: e.matmul(ps[4 + hf], lhsT=mT[:, a, u * 128:(u + 1) * 128], rhs=wo[:, a, hf * 512:(hf + 1) * 512], start=(a == 0), stop=(a == 7)), reads=["wo", "mT"], writes=["ps%d" % (4 + hf)])
                xn, xt = xr.next()
                P.dma(lambda e, xt=xt, t0=t0: e.dma_start(out=xt, in_=x_d[t0:t0 + 128, :]), writes=[xn])
                x2n, x2 = x2r.next()
                for hf in range(2):
                    P.add("dve", lambda e, hf=hf, xt=xt, x2=x2: e.tensor_tensor(out=x2[:, hf * 512:(hf + 1) * 512], in0=xt[:, hf * 512:(hf + 1) * 512], in1=ps[4 + hf], op=ALU.add), reads=[xn, "ps%d" % (4 + hf)], writes=[x2n])
                P.dma(lambda e, x2=x2, t0=t0: e.dma_start(out=x2_s[t0:t0 + 128, :], in_=x2), reads=[x2n], writes=["x2_s"], queue="pool")
                sn, ss = ssr.next()
                P.add("act", lambda e, x2=x2, ss=ss: e.activation(out=junkE, in_=x2, func=AF.Square, accum_out=ss), reads=[x2n], writes=["junkE", sn])
                P.add("act", lambda e, ss=ss: e.activation(out=ss, in_=ss, func=AF.Sqrt, scale=1.0 / D, bias=EPS), reads=[sn], writes=[sn])
                P.add("dve", lambda e, ss=ss: e.reciprocal(out=ss, in_=ss), reads=[sn], writes=[sn])
                hbn, hb = hbr.next()
                P.add("dve", lambda e, x2=x2, ss=ss, hb=hb: e.tensor_scalar(out=hb, in0=x2, scalar1=ss, scalar2=None, op0=ALU.mult), reads=[x2n, sn], writes=[hbn])
                for c in range(8):
                    P.add("pe", lambda e, c=c, hb=hb: e.transpose(out=psb[6][:, c * 128:(c + 1) * 128], in_=hb[:, c * 128:(c + 1) * 128], identity=ident), reads=[hbn, "ident"], writes=["ps6"])
                P.add("act", lambda e, u=u, h2=h2: e.activation(out=h2[:, :, u * 128:(u + 1) * 128], in_=psb[6].rearrange("p (a b) -> p a b", a=8), func=AF.Copy), reads=["ps6"], writes=[h2n])
            P.dma(lambda e, h2=h2, c0=c0: e.dma_start(out=h2t_s[:, :, c0:c0 + 512].rearrange("c p t -> p c t"), in_=h2), reads=[h2n], writes=["h2t_s"], queue="pool")
        A.release(mE)
        P.fence()
        if stop_after == "E":
            P.emit(final_slots=["x2_s", "h2t_s"])
            return nc, P, A

        wq = A.alloc(8 * D, BF16).rearrange("p (a b) -> p a b", a=8)
        g2t = A.alloc(8)
        skbd = A.alloc(256, BF16)
        P.dma(lambda e: e.dma_start(out=g2t, in_=g2_d), writes=["g2t"])
        mF1 = A.mark()
        wst = mkring("wstF", 2, 1024)
        for a in range(8):
            sn, sa = wst.next()
            P.dma(lambda e, sa=sa, a=a: e.dma_start(out=sa, in_=wq_d[a * 128:(a + 1) * 128, :]), writes=[sn])
            P.add("dve", lambda e, sa=sa, a=a: e.tensor_scalar(out=wq[:, a, :], in0=sa, scalar1=g2t[:, a:a + 1], scalar2=None, op0=ALU.mult), reads=[sn, "g2t"], writes=["wq"])
        sn, sa = wst.next()
        P.dma(lambda e, sa=sa: e.dma_start(out=sa[:, 0:256], in_=skbd_d), writes=[sn])
        P.add("dve", lambda e, sa=sa: e.tensor_copy(out=skbd, in_=sa[:, 0:256]), reads=[sn], writes=["skbd"])
        A.release(mF1)
        P.fence()
        hg = A.alloc(8 * 256, BF16).rearrange("p (a b) -> p a b", a=8)
        qtg = A.alloc(8 * 256, BF16).rearrange("p (a b) -> p a b", a=8)
        ssb = A.alloc(8 * 256).rearrange("p (h n) -> p h n", h=8)
        stmp = A.alloc(256)
        T16 = A.alloc(256).rearrange("p (a b) -> p a b", a=16)
        negm = A.alloc(16)
        ab = A.alloc(8 * 256).rearrange("p (h n) -> p h n", h=8)
        abt = A.alloc(256).rearrange("p (a b) -> p a b", a=16)
        Pc = A.alloc(8 * 256).rearrange("p (h n) -> p h n", h=8)
        P16 = A.alloc(128).rearrange("p (h n) -> p h n", h=8)
        Zs = A.alloc(8)
        a2 = A.alloc(8 * 128).rearrange("p (h n) -> p h n", h=8)
        a2t = A.alloc(128).rearrange("p (h n) -> p h n", h=8)
        pdr = mkring("Pd", 2, 2048)
        whr = mkring("Wh", 10, 2048, BF16)
        WT = A.alloc(128 * 256, BF16).rearrange("p (e t) -> p e t", e=128)
        usr = mkring("Us", 3, 1024, BF16)
        vsr = mkring("Vs", 3, 1024, BF16)
        ggr = mkring("Gg", 2, 256, BF16)
        gtr = mkring("GT", 2, 256, BF16)
        xr = mkring("xtG", 2, 1024)
        wps = Ring([2, 3])
        aps = Ring([0, 1])
        NG = S // 256
        for G in range(NG):
            g0 = G * 256
            P.dma(lambda e, g0=g0: e.dma_start(out=hg, in_=h2t_s[:, :, g0:g0 + 256].rearrange("c p t -> p c t")), reads=["h2t_s"], writes=["hg"])
            for h in range(8):
                pi = aps.next()
                for a in range(8):
                    P.add("pe", lambda e, pi=pi, a=a, h=h: e.matmul(ps[pi][:, 0:256], lhsT=wq[:, a, h * 128:(h + 1) * 128], rhs=hg[:, a, :], start=(a == 0), stop=(a == 7)), reads=["wq", "hg"], writes=["ps%d" % pi])
                P.add("act", lambda e, pi=pi, h=h: e.activation(out=qtg[:, h, :], in_=ps[pi][:, 0:256], func=AF.Copy), reads=["ps%d" % pi], writes=["qtg"])
            for w in range(2):
                for h in range(8):
                    pi = 4 + h // 2
                    P.add("pe", lambda e, pi=pi, h=h, w=w: e.matmul(ps[pi][:, (h % 2) * 256:(h % 2) * 256 + 256], lhsT=qtg[:, h, w * 128:(w + 1) * 128], rhs=skbd, start=True, stop=True), reads=["qtg", "skbd"], writes=["ps%d" % pi])
                for k in range(4):
                    P.add("act", lambda e, k=k: e.activation(out=ssb[:, 2 * k:2 * k + 2, :], in_=ps[4 + k].rearrange("p (a n) -> p a n", a=2), func=AF.Copy), reads=["ps%d" % (4 + k)], writes=["ssb"])
                for hc in range(16):
                    h, c = hc // 2, hc % 2
                    src = ssb[:, h, c * 128:(c + 1) * 128]
                    P.add("dve", lambda e, src=src, hc=hc: e.max(out=T16[:, hc, 0:8], in_=src), reads=["ssb"], writes=["T16"])
                    P.add("dve", lambda e, src=src, hc=hc: e.match_replace(out=stmp[:, 0:128], in_to_replace=T16[:, hc, 0:8], in_values=src, imm_value=-1e30), reads=["ssb", "T16"], writes=["stmp"])
                    P.add("dve", lambda e, hc=hc: e.max(out=T16[:, hc, 8:16], in_=stmp[:, 0:128]), reads=["stmp"], writes=["T16"])
                P.add("dve", lambda e: e.tensor_scalar(out=negm, in0=T16[:, :, 0], scalar1=-1.0, scalar2=None, op0=ALU.mult), reads=["T16"], writes=["negm"])
                for hc in range(16):
                    h, c = hc // 2, hc % 2
                    P.add("act", lambda e, h=h, c=c, hc=hc: e.activation(out=ab[:, h, c * 128:(c + 1) * 128], in_=ssb[:, h, c * 128:(c + 1) * 128], func=AF.Exp, bias=negm[:, hc:hc + 1]), reads=["ssb", "negm"], writes=["ab"])
                    P.add("act", lambda e, hc=hc: e.activation(out=abt[:, hc, :], in_=T16[:, hc, :], func=AF.Exp, bias=negm[:, hc:hc + 1]), reads=["T16", "negm"], writes=["abt"])
                abt4 = abt.rearrange("p (h c) n -> p h c n", c=2)

                def cand_top(at_ap, outname):
                    P.add("dve", lambda e: e.tensor_tensor(out=Pc.rearrange("p h (i j) -> p h i j", i=16), in0=at_ap.unsqueeze(3).to_broadcast([128, 8, 16, 16]), in1=abt4[:, :, 1, :].unsqueeze(2).to_broadcast([128, 8, 16, 16]), op=ALU.mult), reads=["abt", "a2t"], writes=["Pc"])
                    for h in range(8):
                        P.add("dve", lambda e, h=h: e.max(out=P16[:, h, 0:8], in_=Pc[:, h, :]), reads=["Pc"], writes=["P16"])
                        P.add("dve", lambda e, h=h: e.match_replace(out=stmp, in_to_replace=P16[:, h, 0:8], in_values=Pc[:, h, :], imm_value=-1.0), reads=["Pc", "P16"], writes=["stmp"])
                        P.add("dve", lambda e, h=h: e.max(out=P16[:, h, 8:16], in_=stmp), reads=["stmp"], writes=["P16"])

                cand_top(abt4[:, :, 0, :], "P16")
                P.add("dve", lambda e: e.reduce_sum(out=Zs, in_=P16, axis=AX.X), reads=["P16"], writes=["Zs"])
                P.add("dve", lambda e: e.reciprocal(out=Zs, in_=Zs), reads=["Zs"], writes=["Zs"])
                P.add("dve", lambda e: e.tensor_tensor(out=a2, in0=ab[:, :, 0:128], in1=Zs.unsqueeze(2).to_broadcast([128, 8, 128]), op=ALU.mult), reads=["ab", "Zs"], writes=["a2"])
                P.add("dve", lambda e: e.tensor_tensor(out=a2t, in0=abt4[:, :, 0, :], in1=Zs.unsqueeze(2).to_broadcast([128, 8, 16]), op=ALU.mult), reads=["abt", "Zs"], writes=["a2t"])
                cand_top(a2t, "P16")
                for eb in range(8):
                    whs = []
                    for h in range(8):
                        pn, pd = pdr.next()
                        P.add("dve", lambda e, pd=pd, h=h, eb=eb: e.tensor_tensor(out=pd.rearrange("p (i j) -> p i j", i=16), in0=a2[:, h, eb * 16:(eb + 1) * 16].unsqueeze(2).to_broadcast([128, 16, 128]), in1=ab[:, h, 128:256].unsqueeze(1).to_broadcast([128, 16, 128]), op=ALU.mult), reads=["a2", "ab"], writes=[pn])
                        wn, wh = whr.next()
                        P.add("dve", lambda e, pd=pd, wh=wh, h=h: e.scalar_tensor_tensor(out=wh, in0=pd, scalar=P16[:, h, 15:16], in1=pd, op0=ALU.is_ge, op1=ALU.mult), reads=[pn, "P16"], writes=[wn])
                        whs.append((wn, wh))
                    for q4 in range(4):
                        pi = wps.next()
                        for ci in range(4):
                            cc = q4 * 4 + ci
                            for h in range(8):
                                wn, wh = whs[h]
                                P.add("pe", lambda e, pi=pi, ci=ci, cc=cc, wh=wh, h=h: e.matmul(ps[pi][:, ci * 128:(ci + 1) * 128], lhsT=wh[:, cc * 128:(cc + 1) * 128], rhs=ident, start=(h == 0), stop=(h == 7)), reads=[wn, "ident"], writes=["ps%d" % pi])
                        e1 = eb * 16 + q4 * 4
                        P.add("act", lambda e, pi=pi, e1=e1, w=w: e.activation(out=WT[:, e1:e1 + 4, w * 128:(w + 1) * 128], in_=ps[pi].rearrange("p (a t) -> p a t", a=4), func=AF.Copy), reads=["ps%d" % pi], writes=["WT"])
            for e1 in range(128):
                un, us_ = usr.next()
                us = us_.rearrange("p (a b) -> p a b", a=8)
                vn, vs = vsr.next()
                P.dma(lambda e, us_=us_, e1=e1: e.dma_start(out=us_, in_=utb_s[e1]), reads=["utb_s"], writes=[un])
                P.dma(lambda e, vs=vs, e1=e1: e.dma_start(out=vs, in_=vb_s[e1]), reads=["vb_s"], writes=[vn])
                pi = aps.next()
                for a in range(8):
                    P.add("pe", lambda e, pi=pi, a=a, us=us: e.matmul(ps[pi][:, 0:256], lhsT=us[:, a, :], rhs=hg[:, a, :], start=(a == 0), stop=(a == 7)), reads=[un, "hg"], writes=["ps%d" % pi])
                gn, gg = ggr.next()
                P.add("act", lambda e, pi=pi, gg=gg: e.activation(out=gg, in_=ps[pi][:, 0:256], func=AF.Gelu_apprx_tanh), reads=["ps%d" % pi], writes=[gn])
                tn, gt = gtr.next()
                P.add("dve", lambda e, gg=gg, gt=gt, e1=e1: e.tensor_tensor(out=gt, in0=gg, in1=WT[:, e1, :], op=ALU.mult), reads=[gn, "WT"], writes=[tn])
                for w in range(2):
                    for hf in range(2):
                        pj = 4 + w * 2 + hf
                        P.add("pe", lambda e, pj=pj, w=w, hf=hf, gt=gt, vs=vs, e1=e1: e.matmul(ps[pj], lhsT=gt[:, w * 128:(w + 1) * 128], rhs=vs[:, hf * 512:(hf + 1) * 512], start=(e1 == 0), stop=(e1 == 127)), reads=[tn, vn], writes=["ps%d" % pj])
            for w in range(2):
                t0 = g0 + w * 128
                xn, xt = xr.next()
                P.dma(lambda e, xt=xt, t0=t0: e.dma_start(out=xt, in_=x2_s[t0:t0 + 128, :]), reads=["x2_s"], writes=[xn])
                for hf in range(2):
                    pj = 4 + w * 2 + hf
                    P.add("dve", lambda e, xt=xt, hf=hf, pj=pj: e.tensor_tensor(out=xt[:, hf * 512:(hf + 1) * 512], in0=xt[:, hf * 512:(hf + 1) * 512], in1=ps[pj], op=ALU.add), reads=[xn, "ps%d" % pj], writes=[xn])
                P.dma(lambda e, xt=xt, t0=t0: e.dma_start(out=out_d[t0:t0 + 128, :], in_=xt), reads=[xn], writes=["out"], queue="pool")
        P.emit(final_slots=["out"])
    return nc, P, A


def _consts():
    bf = ml_dtypes.bfloat16
    c = {}
    c["ident"] = np.eye(128, dtype=np.float32).astype(bf)
    bd = np.zeros((128, 128), np.float32)
    bd[:64, :64] = 1
    bd[64:, 64:] = 1
    c["bdones"] = bd.astype(bf)
    Rm = np.zeros((128, 128), np.float32)
    for hb in (0, 64):
        for i in range(8):
            Rm[hb + i, hb + i + 8] = -1.0
            Rm[hb + i + 8, hb + i] = 1.0
    c["rotT"] = np.ascontiguousarray(Rm.T).astype(bf)
    half = 8
    inv = (500000.0 ** (-(np.arange(half, dtype=np.float32) * 2.0 / 16))).astype(np.float32)

    def tables(pos):
        ang = pos.astype(np.float32)[None, :] * inv[:, None]
        cs = np.ones((128, pos.shape[0]), np.float32)
        sn = np.zeros((128, pos.shape[0]), np.float32)
        for hb in (0, 64):
            cs[hb:hb + 8] = np.cos(ang)
            cs[hb + 8:hb + 16] = np.cos(ang)
            sn[hb:hb + 8] = np.sin(ang)
            sn[hb + 8:hb + 16] = np.sin(ang)
        return cs, sn

    c["cosT"], c["sinT"] = tables(np.arange(S))
    pc = np.arange(256) * 16 + 31
    c["cosC"], c["sinC"] = tables(pc)
    t = np.arange(S)
    cm = np.zeros((2, 128, S), np.float32)
    for ct in range(2):
        cidx = ct * 128 + np.arange(128)
        cm[ct] = ((cidx[:, None] * 16 + 31 <= t[None, :]) & (cidx[:, None] < 255)).astype(np.float32)
    c["cmask"] = cm.astype(bf)
    k = np.arange(128)[:, None]
    q = np.arange(128)[None, :]
    ms = np.zeros((128, 9, 128), np.float32)
    ms[:, 0] = k <= q
    ms[:, 1] = k > q
    ms[:, 2] = k >= q
    for b, d in ((3, 4), (6, 16)):
        res = ((q - k) % d) == 0
        ms[:, b] = res
        ms[:, b + 1] = res & (k <= q)
        ms[:, b + 2] = res & (k >= q)
    c["masks"] = ms.astype(bf)
    n_cmp = 255
    s0 = np.arange(n_cmp) * 16
    s1 = s0 + 32
    b0 = np.arange(64) * 64
    b1 = b0 + 64
    ovm = np.clip(np.minimum(s1[:, None], b1[None, :]) - np.maximum(s0[:, None], b0[None, :]), 0, None) / 32.0
    ovp = np.zeros((256, 64), np.float32)
    ovp[:255] = ovm
    c["ov"] = ovp.reshape(2, 128, 64).astype(bf)
    blk = np.arange(64)[None, :]
    cur = (t // 64)[:, None]
    forced = (blk == 0) | (blk == cur) | (blk == cur - 1)
    causal = blk * 64 <= t[:, None]
    c["selmul"] = (causal & ~forced).astype(np.float32)
    c["seladd"] = np.where(forced, 1e3, np.where(causal, 0.0, -1.0)).astype(np.float32)
    es = np.zeros((64, S), np.float32)
    es[(t // 64), t] = 1.0
    c["esel"] = es.astype(bf)
    return c


def _layout_weights(inp):
    w_in = np.asarray(inp["w_in"], np.float32)
    o = {}
    cols = []
    for cch in range(4):
        cols.append(np.arange(cch * 128, (cch + 1) * 128))
    cols.append(np.arange(512, 640))
    cols.append(np.arange(640, 768))
    for base in (768, 1024):
        for g in range(2):
            cc = base + g * 64 + np.arange(64)
            cols.append(np.concatenate([cc, cc]))
    for g in range(3):
        for r in range(2):
            for pr in range(2):
                cols.append(1304 + g * 768 + r * 256 + pr * 128 + np.arange(128))
    fm_cols = np.concatenate(cols)
    assert fm_cols.shape[0] == NFM * 128
    o["wfm"] = np.ascontiguousarray(w_in[:, fm_cols])
    tm_cols = np.concatenate([np.arange(896, 1024), np.arange(1152, 1280)] +
                             [1304 + g * 768 + 512 + np.arange(256) for g in range(3)] + [np.arange(1280, 1304)])
    assert tm_cols.shape[0] == NTM
    o["wtm"] = np.ascontiguousarray(w_in[:, tm_cols])
    o["wmg"] = np.ascontiguousarray(w_in[:, 3608:5656])
    o["g1"] = np.ascontiguousarray(np.asarray(inp["norm1_g"], np.float32).reshape(8, 128).T)
    o["g2"] = np.ascontiguousarray(np.asarray(inp["norm2_g"], np.float32).reshape(8, 128).T)
    gains = np.ones((128, NFM + 1), np.float32)
    qn = np.asarray(inp["nsa_q_norm"], np.float32)
    kn = np.asarray(inp["nsa_k_norm"], np.float32)
    dq = np.asarray(inp["dil_q_norm"], np.float32)
    dk = np.asarray(inp["dil_k_norm"], np.float32)
    for cch in range(4):
        gains[:, cch] = np.tile(qn, 2)
    for g in range(2):
        gains[:, CID_KS + g] = np.tile(kn[1], 2)
        gains[:, CID_KW + g] = np.tile(kn[2], 2)
    for g in range(3):
        for pr in range(2):
            gains[:, CID_DIL + g * 4 + pr] = np.tile(dq[g], 2)
            gains[:, CID_DIL + g * 4 + 2 + pr] = np.tile(dk[g], 2)
    gains[:, NFM] = np.tile(kn[0], 2)
    o["gains"] = gains
    o["w1k"] = np.asarray(inp["cmp_w1_k"], np.float32)
    o["w1v"] = np.asarray(inp["cmp_w1_v"], np.float32)
    o["w2k"] = np.asarray(inp["cmp_w2_k"], np.float32)
    o["w2v"] = np.asarray(inp["cmp_w2_v"], np.float32)
    o["pek"] = np.ascontiguousarray(np.asarray(inp["cmp_pe_k"], np.float32).reshape(16, 128).T)
    o["pev"] = np.ascontiguousarray(np.asarray(inp["cmp_pe_v"], np.float32).reshape(16, 128).T)
    o["wun"] = np.asarray(inp["w_up_nsa"], np.float32)
    o["wud"] = np.asarray(inp["w_up_dil"], np.float32)
    o["wo"] = np.asarray(inp["w_o"], np.float32)
    o["wq"] = np.asarray(inp["peer_wq"], np.float32)
    sk = np.asarray(inp["peer_subkeys"], np.float32)
    skbd = np.zeros((128, 256), np.float32)
    skbd[0:64, 0:128] = sk[0].T
    skbd[64:128, 128:256] = sk[1].T
    o["skbd"] = skbd
    u = np.asarray(inp["peer_u"], np.float32)
    o["ut"] = np.ascontiguousarray(u.reshape(128, 128, 8, 128).transpose(0, 3, 2, 1)).reshape(128, 128, 1024)
    o["pv"] = np.asarray(inp["peer_v"], np.float32).reshape(128, 128, 1024)
    return o


_CACHE = {}


def kernel(**inputs):
    if "nc" not in _CACHE:
        _CACHE["nc"] = build_program()[0]
        _CACHE["consts"] = _consts()
    nc = _CACHE["nc"]
    shared = dict(_CACHE["consts"])
    shared.update(_layout_weights(inputs))
    x = np.asarray(inputs["x"], np.float32)
    in_maps = []
    for b in range(8):
        m = dict(shared)
        m["x"] = np.ascontiguousarray(x[b])
        in_maps.append(m)
    res = run_bass_kernel_spmd(nc, in_maps, core_ids=list(range(8)))
    return np.stack([np.asarray(r["out"], np.float32) for r in res.results], axis=0)
```

```python
import contextlib
import math
import numpy as np
import ml_dtypes
import concourse.bass as bass
import concourse.mybir as mybir
from concourse.bass_utils import run_bass_kernel_spmd

F32 = mybir.dt.float32
BF16 = mybir.dt.bfloat16
ALU = mybir.AluOpType
AF = mybir.ActivationFunctionType
AX = mybir.AxisListType

S = 4096
D = 1024
NT = S // 128
NB = S // 512
EPS = 1e-6


class Slot:
    __slots__ = ("name", "writers", "readers", "dcount")

    def __init__(self, name):
        self.name = name
        self.writers = {}
        self.readers = {}
        self.dcount = 0


class Op:
    __slots__ = ("eng", "fn", "deps", "signal", "value", "key", "is_dma")

    def __init__(self, eng, fn, key, is_dma=False, value=None):
        self.eng = eng
        self.fn = fn
        self.deps = []
        self.signal = False
        self.value = value
        self.key = key
        self.is_dma = is_dma


class Prog:
    COMPUTE = ("pe", "act", "dve", "pool")

    def __init__(self, nc):
        self.nc = nc
        self.ops = {e: [] for e in ("pe", "act", "dve", "pool", "sp")}
        self.slots = {}
        self.last = {}
        self.fence_deps = {e: [] for e in self.ops}
        self.n_ops = 0

    def slot(self, name):
        s = self.slots.get(name)
        if s is None:
            s = self.slots[name] = Slot(name)
        return s

    def _track(self, op, reads, writes):
        deps = op.deps
        fd = self.fence_deps[op.eng]
        if fd:
            deps.extend(fd)
            self.fence_deps[op.eng] = []
        for r in reads:
            s = self.slot(r)
            deps.extend(s.writers.values())
            s.readers[op.key] = op
        for w in writes:
            s = self.slot(w)
            deps.extend(s.readers.values())
            deps.extend(s.writers.values())
            if any(o is not op for o in s.readers.values()):
                s.writers = {op.key: op}
                s.readers = {}
            else:
                s.readers = {}
                s.writers[op.key] = op
        op.deps = [d for d in deps if d is not op and (d.key != op.key or (not op.is_dma and op.eng != "pe"))]
        self.last[op.key] = op
        self.n_ops += 1

    def add(self, eng, fn, reads=(), writes=()):
        op = Op(eng, fn, eng)
        self.ops[eng].append(op)
        self._track(op, reads, writes)
        return op

    def dma(self, fn, reads=(), writes=(), queue="sp"):
        assert len(writes) == 1
        s = self.slot(writes[0])
        s.dcount += 1
        op = Op(queue, fn, ("d", s.name), is_dma=True, value=16 * s.dcount)
        self.ops[queue].append(op)
        self._track(op, reads, writes)
        return op

    def fence(self):
        allops = list(self.last.values())
        for e in self.fence_deps:
            self.fence_deps[e] = list(allops)

    def emit(self, final_slots=()):
        nc = self.nc
        fin = Op("sp", None, "fin")
        for name in final_slots:
            fin.deps.extend(self.slot(name).writers.values())
        self.ops["sp"].append(fin)
        for e, lst in self.ops.items():
            for op in lst:
                for d in op.deps:
                    if not d.is_dma:
                        d.signal = True
        for e in self.COMPUTE:
            c = 0
            for op in self.ops[e]:
                if op.signal and not op.is_dma:
                    c += 1
                    op.value = c
        keys = list(self.COMPUTE)
        for lst in self.ops.values():
            for op in lst:
                if op.is_dma and op.key not in keys:
                    keys.append(op.key)
        self.n_sems = len(keys)
        with contextlib.ExitStack() as st:
            sems = {}
            for i, k in enumerate(keys):
                sems[k] = st.enter_context(nc.semaphore("s%d" % i))
            block = st.enter_context(nc.Block())

            def run(eng_name, eng):
                waited = {}
                for op in self.ops[eng_name]:
                    need = {}
                    for d in op.deps:
                        v = d.value
                        if v > need.get(d.key, 0):
                            need[d.key] = v
                    for k, v in need.items():
                        if waited.get(k, 0) < v:
                            eng.wait_ge(sems[k], v)
                            waited[k] = v
                    if op.fn is None:
                        continue
                    ins = op.fn(eng)
                    if op.is_dma:
                        ins.then_inc(sems[op.key], 16)
                    elif op.signal:
                        ins.then_inc(sems[op.key], 1)

            @block.sync
            def _(eng):
                run("sp", eng)

            @block.tensor
            def _(eng):
                run("pe", eng)

            @block.scalar
            def _(eng):
                run("act", eng)

            @block.vector
            def _(eng):
                run("dve", eng)

            @block.gpsimd
            def _(eng):
                run("pool", eng)


class Arena:
    def __init__(self, t, total_f32):
        self.t = t
        self.total = total_f32
        self.off = 0
        self.peak = 0

    def mark(self):
        return self.off

    def release(self, m):
        self.off = m

    def alloc(self, cols, dtype=F32):
        n32 = cols if dtype == F32 else (cols + 1) // 2
        a = self.off
        self.off += n32
        self.peak = max(self.peak, self.off)
        assert self.off <= self.total, ("SBUF arena overflow", self.off, self.total)
        v = self.t[:, a:a + n32]
        if dtype != F32:
            v = v.bitcast(dtype)[:, 0:cols]
        return v


class Ring:
    def __init__(self, items):
        self.items = list(items)
        self.i = 0

    def next(self):
        r = self.items[self.i % len(self.items)]
        self.i += 1
        return r


CID_Q = 0
CID_KC = 4
CID_VC = 5
CID_KS = 6
CID_KW = 8
CID_DIL = 10
NFM = 22
NTM = 1048
DIL_PAT = ((128, 1), (512, 4), (2048, 16))


def build_program(debug=False, stop_after=None):
    nc = bass.Bass("TRN2", target_bir_lowering=False)

    def din(name, shape, dt=F32):
        return nc.dram_tensor(name, list(shape), dt, kind="ExternalInput").ap()

    skind = "ExternalOutput" if debug else "Internal"

    def dscr(name, shape, dt):
        return nc.dram_tensor(name, list(shape), dt, kind=skind).ap()

    x_d = din("x", [S, D])
    wfm_d = din("wfm", [D, NFM * 128])
    wtm_d = din("wtm", [D, NTM])
    wmg_d = din("wmg", [D, 2048])
    g1_d = din("g1", [128, 8])
    g2_d = din("g2", [128, 8])
    gains_d = din("gains", [128, NFM + 1])
    w1k_d = din("w1k", [2048, 256])
    w1v_d = din("w1v", [2048, 256])
    w2k_d = din("w2k", [256, 64])
    w2v_d = din("w2v", [256, 64])
    pek_d = din("pek", [128, 16])
    pev_d = din("pev", [128, 16])
    wun_d = din("wun", [512, D])
    wud_d = din("wud", [256, D])
    wo_d = din("wo", [D, D])
    wq_d = din("wq", [D, D])
    skbd_d = din("skbd", [128, 256])
    ut_d = din("ut", [128, 128, 1024])
    v_d = din("pv", [128, 128, 1024])
    ident_d = din("ident", [128, 128], BF16)
    bd_d = din("bdones", [128, 128], BF16)
    rot_d = din("rotT", [128, 128], BF16)
    cos_d = din("cosT", [128, S])
    sin_d = din("sinT", [128, S])
    cosc_d = din("cosC", [128, 256])
    sinc_d = din("sinC", [128, 256])
    cmask_d = din("cmask", [2, 128, S], BF16)
    masks_d = din("masks", [128, 9, 128], BF16)
    ov_d = din("ov", [2, 128, 64], BF16)
    selmul_d = din("selmul", [S, 64])
    seladd_d = din("seladd", [S, 64])
    esel_d = din("esel", [64, S], BF16)
    out_d = nc.dram_tensor("out", [S, D], F32, kind="ExternalOutput").ap()

    qkt_s = dscr("qkt_s", [NFM, 128, S], BF16)
    vtm_s = dscr("vtm_s", [S, 1024], BF16)
    gates_s = dscr("gates_s", [S, 24], F32)
    ht_s = dscr("ht_s", [8, 128, S], BF16)
    yt_s = dscr("yt_s", [6, 128, S], BF16)
    x2_s = dscr("x2_s", [S, D], F32)
    h2t_s = dscr("h2t_s", [8, 128, S], BF16)
    utb_s = dscr("utb_s", [128, 128, 1024], BF16)
    vb_s = dscr("vb_s", [128, 128, 1024], BF16)

    TOT = 53100
    with contextlib.ExitStack() as st:
        at = st.enter_context(nc.sbuf_tensor("arena", [128, TOT], F32))
        pst = [st.enter_context(nc.psum_tensor("ps%d" % i, [128, 512], F32)) for i in range(8)]
        ps = [t.ap() for t in pst]
        psb = [t.ap().bitcast(BF16) for t in pst]
        A = Arena(at, TOT)
        P = Prog(nc)
        uid = [0]

        def nm(prefix):
            uid[0] += 1
            return "%s_%d" % (prefix, uid[0])

        def mkring(prefix, n, cols, dtype=F32):
            return Ring([(nm(prefix), A.alloc(cols, dtype)) for _ in range(n)])

        ident = A.alloc(128, BF16)
        bdones = A.alloc(128, BF16)
        rotT = A.alloc(128, BF16)
        masks = A.alloc(9 * 128, BF16).rearrange("p (a b) -> p a b", a=9)
        P.dma(lambda e: e.dma_start(out=ident, in_=ident_d), writes=["ident"])
        P.dma(lambda e: e.dma_start(out=bdones, in_=bd_d), writes=["bdones"])
        P.dma(lambda e: e.dma_start(out=rotT, in_=rot_d), writes=["rotT"])
        P.dma(lambda e: e.dma_start(out=masks, in_=masks_d), writes=["masks"])
        gains = A.alloc(NFM + 1)
        P.dma(lambda e: e.dma_start(out=gains, in_=gains_d), writes=["gains"])
        persist_mark = A.mark()

        def norm_rope(zps_name, zps, n, gain_ap, cos_ap, sin_ap, cos_slots, out_name, out_ap, R):
            sqn, sq = R["sq"].next()
            P.add("act", lambda e: e.activation(out=sq[:, 0:n], in_=zps, func=AF.Square), reads=[zps_name], writes=[sqn])
            P.add("pe", lambda e: e.matmul(ps[3][:, 0:n], lhsT=bdones, rhs=sq[:, 0:n], start=True, stop=True), reads=[sqn, "bdones"], writes=["ps3"])
            rsn, rs = R["rs"].next()
            P.add("act", lambda e: e.activation(out=rs[:, 0:n], in_=ps[3][:, 0:n], func=AF.Sqrt, scale=1.0 / 64, bias=EPS), reads=["ps3"], writes=[rsn])
            P.add("dve", lambda e: e.reciprocal(out=rs[:, 0:n], in_=rs[:, 0:n]), reads=[rsn], writes=[rsn])
            znn, zn = R["zn"].next()
            P.add("dve", lambda e: e.scalar_tensor_tensor(out=zn[:, 0:n], in0=zps, scalar=gain_ap, in1=rs[:, 0:n], op0=ALU.mult, op1=ALU.mult), reads=[zps_name, rsn, "gains"], writes=[znn])
            zbn, zb = R["zb"].next()
            P.add("act", lambda e: e.activation(out=zb[:, 0:n], in_=zn[:, 0:n], func=AF.Copy), reads=[znn], writes=[zbn])
            P.add("pe", lambda e: e.matmul(ps[4][:, 0:n], lhsT=rotT, rhs=zb[:, 0:n], start=True, stop=True), reads=[zbn, "rotT"], writes=["ps4"])
            P.add("dve", lambda e: e.tensor_tensor(out=zn[:, 0:n], in0=zn[:, 0:n], in1=cos_ap, op=ALU.mult), reads=[znn] + cos_slots, writes=[znn])
            t2n, t2 = R["t2"].next()
            P.add("dve", lambda e: e.tensor_tensor(out=t2[:, 0:n], in0=ps[4][:, 0:n], in1=sin_ap, op=ALU.mult), reads=["ps4"] + cos_slots, writes=[t2n])
            P.add("dve", lambda e: e.tensor_tensor(out=out_ap, in0=zn[:, 0:n], in1=t2[:, 0:n], op=ALU.add), reads=[znn, t2n], writes=[out_name])

        m0 = A.mark()
        stg = mkring("pstg", 2, 4096)
        stb = mkring("pstb", 2, 4096, BF16)
        k = 0
        for src, dst in ((ut_d, utb_s), (v_d, vb_s)):
            for i in range(32):
                sn, sa = stg.next()
                bn, ba = stb.next()
                P.dma(lambda e, sa=sa, src=src, i=i: e.dma_start(out=sa.rearrange("p (a b) -> p a b", a=4), in_=src[4 * i:4 * i + 4].rearrange("a p c -> p a c")), writes=[sn])
                eng = ("dve", "act", "pool")[k % 3]
                k += 1
                if eng == "act":
                    P.add("act", lambda e, sa=sa, ba=ba: e.activation(out=ba, in_=sa, func=AF.Copy), reads=[sn], writes=[bn])
                else:
                    P.add(eng, lambda e, sa=sa, ba=ba: e.tensor_copy(out=ba, in_=sa), reads=[sn], writes=[bn])
                P.dma(lambda e, ba=ba, dst=dst, i=i: e.dma_start(out=dst[4 * i:4 * i + 4].rearrange("a p c -> p a c"), in_=ba.rearrange("p (a b) -> p a b", a=4)), reads=[bn], writes=["utb_s" if dst is utb_s else "vb_s"], queue="pool")
        A.release(m0)
        P.fence()

        m0 = A.mark()
        wfm = A.alloc(8 * NFM * 128, BF16).rearrange("p (a b) -> p a b", a=8)
        wtm = A.alloc(8 * NTM, BF16).rearrange("p (a b) -> p a b", a=8)
        g1t = A.alloc(8)
        cosT = A.alloc(S)
        sinT = A.alloc(S)
        P.dma(lambda e: e.dma_start(out=g1t, in_=g1_d), writes=["g1t"])
        P.dma(lambda e: e.dma_start(out=cosT, in_=cos_d), writes=["cosT"])
        P.dma(lambda e: e.dma_start(out=sinT, in_=sin_d), writes=["sinT"])
        m1 = A.mark()
        wst = mkring("wst", 2, NFM * 128)
        for kc in range(8):
            sn, sa = wst.next()
            P.dma(lambda e, sa=sa, kc=kc: e.dma_start(out=sa, in_=wfm_d[kc * 128:(kc + 1) * 128, :]), writes=[sn])
            P.add("dve", lambda e, sa=sa, kc=kc: e.tensor_scalar(out=wfm[:, kc, :], in0=sa, scalar1=g1t[:, kc:kc + 1], scalar2=None, op0=ALU.mult), reads=[sn, "g1t"], writes=["wfm"])
        for kc in range(8):
            sn, sa = wst.next()
            P.dma(lambda e, sa=sa, kc=kc: e.dma_start(out=sa[:, 0:NTM], in_=wtm_d[kc * 128:(kc + 1) * 128, :]), writes=[sn])
            P.add("dve", lambda e, sa=sa, kc=kc: e.tensor_scalar(out=wtm[:, kc, :], in0=sa[:, 0:NTM], scalar1=g1t[:, kc:kc + 1], scalar2=None, op0=ALU.mult), reads=[sn, "g1t"], writes=["wtm"])
        A.release(m1)
        P.fence()
        xr = mkring("xt", 2, 1024)
        junk = A.alloc(1024)
        ssr = mkring("ss", 2, 1)
        hbr = mkring("hb", 2, 1024, BF16)
        hTr = mkring("hTb", 2, 8 * 512, BF16)
        R = {"sq": mkring("sq", 2, 512, BF16), "rs": mkring("rs", 2, 512), "zn": mkring("zn", 2, 512),
             "zb": mkring("zb", 2, 512, BF16), "t2": mkring("t2", 2, 512)}
        fmo = mkring("fmo", 3, 512, BF16)
        tmo = mkring("tmo", 2, 1024, BF16)
        gto = mkring("gto", 2, 24)
        fmps = Ring([1, 2])
        for b in range(NB):
            hTn, hTb_ = hTr.next()
            hTb = hTb_.rearrange("p (a b) -> p a b", a=8)
            c0 = b * 512
            for u in range(4):
                t0 = c0 + u * 128
                xn, xt = xr.next()
                P.dma(lambda e, xt=xt, t0=t0: e.dma_start(out=xt, in_=x_d[t0:t0 + 128, :]), writes=[xn])
                sn, ss = ssr.next()
                P.add("act", lambda e, xt=xt, ss=ss: e.activation(out=junk, in_=xt, func=AF.Square, accum_out=ss), reads=[xn], writes=["junk", sn])
                P.add("act", lambda e, ss=ss: e.activation(out=ss, in_=ss, func=AF.Sqrt, scale=1.0 / D, bias=EPS), reads=[sn], writes=[sn])
                P.add("dve", lambda e, ss=ss: e.reciprocal(out=ss, in_=ss), reads=[sn], writes=[sn])
                hn_, hb = hbr.next()
                P.add("dve", lambda e, xt=xt, ss=ss, hb=hb: e.tensor_scalar(out=hb, in0=xt, scalar1=ss, scalar2=None, op0=ALU.mult), reads=[xn, sn], writes=[hn_])
                for c in range(8):
                    P.add("pe", lambda e, c=c, hb=hb: e.transpose(out=psb[0][:, c * 128:(c + 1) * 128], in_=hb[:, c * 128:(c + 1) * 128], identity=ident), reads=[hn_, "ident"], writes=["ps0"])
                P.add("act", lambda e, u=u, hTb=hTb: e.activation(out=hTb[:, :, u * 128:(u + 1) * 128], in_=psb[0].rearrange("p (a b) -> p a b", a=8), func=AF.Copy), reads=["ps0"], writes=[hTn])
            P.dma(lambda e, hTb=hTb, c0=c0: e.dma_start(out=ht_s[:, :, c0:c0 + 512].rearrange("c p t -> p c t"), in_=hTb), reads=[hTn], writes=["ht_s"], queue="pool")
            for cid in range(NFM):
                pi = fmps.next()
                for kc in range(8):
                    P.add("pe", lambda e, pi=pi, kc=kc, cid=cid, hTb=hTb: e.matmul(ps[pi], lhsT=wfm[:, kc, cid * 128:(cid + 1) * 128], rhs=hTb[:, kc, :], start=(kc == 0), stop=(kc == 7)), reads=["wfm", hTn], writes=["ps%d" % pi])
                on, oa = fmo.next()
                if cid in (CID_KC, CID_VC):
                    P.add("act", lambda e, pi=pi, oa=oa: e.activation(out=oa, in_=ps[pi], func=AF.Copy), reads=["ps%d" % pi], writes=[on])
                else:
                    norm_rope("ps%d" % pi, ps[pi], 512, gains[:, cid:cid + 1], cosT[:, c0:c0 + 512], sinT[:, c0:c0 + 512], ["cosT", "sinT"], on, oa, R)
                P.dma(lambda e, oa=oa, cid=cid, c0=c0: e.dma_start(out=qkt_s[cid, :, c0:c0 + 512], in_=oa), reads=[on], writes=["qkt_s"], queue="pool")
            for u in range(4):
                t0 = c0 + u * 128
                for r, (a0, a1) in enumerate(((0, 512), (512, 1024), (1024, NTM))):
                    for kc in range(8):
                        P.add("pe", lambda e, r=r, kc=kc, u=u, a0=a0, a1=a1, hTb=hTb: e.matmul(ps[5 + r][:, 0:a1 - a0], lhsT=hTb[:, kc, u * 128:(u + 1) * 128], rhs=wtm[:, kc, a0:a1], start=(kc == 0), stop=(kc == 7)), reads=["wtm", hTn], writes=["ps%d" % (5 + r)])
                tn, ta = tmo.next()
                P.add("act", lambda e, ta=ta: e.activation(out=ta[:, 0:512], in_=ps[5], func=AF.Copy), reads=["ps5"], writes=[tn])
                P.add("dve", lambda e, ta=ta: e.tensor_copy(out=ta[:, 512:1024], in_=ps[6]), reads=["ps6"], writes=[tn])
                P.dma(lambda e, ta=ta, t0=t0: e.dma_start(out=vtm_s[t0:t0 + 128, :], in_=ta), reads=[tn], writes=["vtm_s"], queue="pool")
                gn, ga = gto.next()
                P.add("act", lambda e, ga=ga: e.activation(out=ga, in_=ps[7][:, 0:24], func=AF.Sigmoid), reads=["ps7"], writes=[gn])
                P.dma(lambda e, ga=ga, t0=t0: e.dma_start(out=gates_s[t0:t0 + 128, :], in_=ga), reads=[gn], writes=["gates_s"], queue="pool")
        A.release(m0)
        P.fence()
        if stop_after == "A":
            P.emit(final_slots=["qkt_s", "vtm_s", "gates_s", "ht_s", "utb_s", "vb_s"])
            return nc, P, A

        mB = A.mark()
        kcmp = A.alloc(2 * 256, BF16).rearrange("p (g c) -> p g c", g=2)
        vc1 = A.alloc(2 * 2 * 129, BF16).rearrange("p (t g c) -> p t g c", t=2, g=2)
        P.add("dve", lambda e: e.memset(kcmp, 0.0), writes=["kcmp"])
        P.add("dve", lambda e: e.memset(vc1, 0.0), writes=["vc1"])
        P.add("dve", lambda e: e.memset(vc1[:, 0, :, 64:65], 1.0), writes=["vc1"])
        P.add("dve", lambda e: e.memset(vc1[0:127, 1, :, 64:65], 1.0), writes=["vc1"])
        for ct in range(2):
            for g in range(2):
                P.dma(lambda e, ct=ct, g=g: e.dma_start(out=vc1[:, ct, g, 65:129], in_=ov_d[ct]), writes=["vc1"])
        mB1 = A.mark()
        x2c = A.alloc(2 * 2 * S, BF16).rearrange("p (k g t) -> p k g t", k=2, g=2)
        P.add("pool", lambda e: e.memset(x2c[:, :, :, S - 1:S], 0.0), writes=["x2c"])
        for kv in range(2):
            for g in range(2):
                P.dma(lambda e, kv=kv, g=g: e.dma_start(out=x2c[0:64, kv, g, :], in_=qkt_s[CID_KC + kv, g * 64:(g + 1) * 64, :]), reads=["qkt_s"], writes=["x2c"])
                P.dma(lambda e, kv=kv, g=g: e.dma_start(out=x2c[64:128, kv, g, 0:S - 1], in_=qkt_s[CID_KC + kv, g * 64:(g + 1) * 64, 1:S]), reads=["qkt_s"], writes=["x2c"])
        w1s = A.alloc(16 * 256).rearrange("p (a h) -> p a h", a=16)
        w1b = A.alloc(2 * 16 * 256, BF16).rearrange("p (k a h) -> p k a h", k=2, a=16)
        pes = A.alloc(32)
        peb = A.alloc(32, BF16)
        w2s = A.alloc(2 * 2 * 64).rearrange("p (k c d) -> p k c d", k=2, c=2)
        w2kd = A.alloc(2 * 128, BF16).rearrange("p (c d) -> p c d", c=2)
        w2vb = A.alloc(2 * 64, BF16).rearrange("p (c d) -> p c d", c=2)
        cosC = A.alloc(256)
        sinC = A.alloc(256)
        P.dma(lambda e: e.dma_start(out=cosC, in_=cosc_d), writes=["cosC"])
        P.dma(lambda e: e.dma_start(out=sinC, in_=sinc_d), writes=["sinC"])
        P.dma(lambda e: e.dma_start(out=pes[:, 0:16], in_=pek_d), writes=["pes"])
        P.dma(lambda e: e.dma_start(out=pes[:, 16:32], in_=pev_d), writes=["pes"])
        P.add("dve", lambda e: e.tensor_copy(out=peb, in_=pes), reads=["pes"], writes=["peb"])
        for kv, (w1d, w2d) in enumerate(((w1k_d, w2k_d), (w1v_d, w2v_d))):
            P.dma(lambda e, w1d=w1d: e.dma_start(out=w1s, in_=w1d.rearrange("(a p) h -> p a h", p=128)), writes=["w1s"])
            P.add("dve", lambda e, kv=kv: e.tensor_copy(out=w1b[:, kv], in_=w1s), reads=["w1s"], writes=["w1b"])
            P.dma(lambda e, kv=kv, w2d=w2d: e.dma_start(out=w2s[:, kv], in_=w2d.rearrange("(c p) d -> p c d", p=128)), writes=["w2s"])
        P.add("dve", lambda e: e.tensor_copy(out=w2kd[:, :, 0:64], in_=w2s[:, 0]), reads=["w2s"], writes=["w2kd"])
        P.add("dve", lambda e: e.tensor_copy(out=w2kd[:, :, 64:128], in_=w2s[:, 0]), reads=["w2s"], writes=["w2kd"])
        P.add("dve", lambda e: e.tensor_copy(out=w2vb, in_=w2s[:, 1]), reads=["w2s"], writes=["w2vb"])
        biasT = A.alloc(4)
        gT = A.alloc(2 * 256, BF16).rearrange("p (c n) -> p c n", c=2)
        RB = {"sq": mkring("sqB", 1, 256, BF16), "rs": mkring("rsB", 1, 256), "zn": mkring("znB", 1, 256),
              "zb": mkring("zbB", 1, 256, BF16), "t2": mkring("t2B", 1, 256)}
        for kv in range(2):
            for hc in range(2):
                for a in range(16):
                    P.add("pe", lambda e, kv=kv, hc=hc, a=a: e.matmul(ps[2][:, 0:1], lhsT=w1b[:, kv, a, hc * 128:(hc + 1) * 128], rhs=peb[:, kv * 16 + a:kv * 16 + a + 1], start=(a == 0), stop=(a == 15)), reads=["w1b", "peb"], writes=["ps2"])
                P.add("dve", lambda e, kv=kv, hc=hc: e.tensor_copy(out=biasT[:, kv * 2 + hc:kv * 2 + hc + 1], in_=ps[2][:, 0:1]), reads=["ps2"], writes=["biasT"])
        for kv in range(2):
            for g in range(2):
                P.add("dve", lambda e: e.memset(gT, 0.0), writes=["gT"])
                for hc in range(2):
                    pi = hc
                    for a in range(16):
                        P.add("pe", lambda e, kv=kv, g=g, hc=hc, a=a, pi=pi: e.matmul(ps[pi][:, 0:255], lhsT=w1b[:, kv, a, hc * 128:(hc + 1) * 128], rhs=x2c[:, kv, g, 2 * a:2 * a + 16 * 254 + 1:16], start=(a == 0), stop=(a == 15)), reads=["w1b", "x2c"], writes=["ps%d" % pi])
                    P.add("act", lambda e, kv=kv, hc=hc, pi=pi: e.activation(out=gT[:, hc, 0:255], in_=ps[pi][:, 0:255], func=AF.Gelu_apprx_tanh, bias=biasT[:, kv * 2 + hc:kv * 2 + hc + 1]), reads=["ps%d" % pi, "biasT"], writes=["gT"])
                if kv == 0:
                    for hc in range(2):
                        P.add("pe", lambda e, hc=hc: e.matmul(ps[5][:, 0:256], lhsT=w2kd[:, hc, :], rhs=gT[:, hc, :], start=(hc == 0), stop=(hc == 1)), reads=["w2kd", "gT"], writes=["ps5"])
                    norm_rope("ps5", ps[5][:, 0:256], 256, gains[:, NFM:NFM + 1], cosC, sinC, ["cosC", "sinC"], "kcmp", kcmp[:, g, :], RB)
                    P.add("dve", lambda e, g=g: e.memset(kcmp[:, g, 255:256], 0.0), writes=["kcmp"])
                else:
                    for ct in range(2):
                        for hc in range(2):
                            P.add("pe", lambda e, hc=hc, ct=ct: e.matmul(ps[6][:, 0:64], lhsT=gT[:, hc, ct * 128:(ct + 1) * 128], rhs=w2vb[:, hc, :], start=(hc == 0), stop=(hc == 1)), reads=["w2vb", "gT"], writes=["ps6"])
                        P.add("act", lambda e, g=g, ct=ct: e.activation(out=vc1[:, ct, g, 0:64], in_=ps[6][:, 0:64], func=AF.Copy), reads=["ps6"], writes=["vc1"])
        A.release(mB1)
        P.fence()

        Sps = Ring([0, 1])

        def banded(qb, sources, o_of_u, o_slot, er, o_clear, mul_of_kt=None):
            P.add("dve", lambda e: e.memset(o_clear, 0.0), writes=[o_slot])
            items = []
            for si, src in enumerate(sources):
                dmax = src[6]
                for kt in range(max(0, 4 * qb - dmax), 4 * qb + 4):
                    u0 = max(0, kt - 4 * qb)
                    u1 = min(3, kt + dmax - 4 * qb)
                    if u0 <= u1:
                        items.append((si, kt, u0, u1))
            first = {}
            last = {}
            for idx, (si, kt, u0, u1) in enumerate(items):
                for u in range(u0, u1 + 1):
                    first.setdefault(u, idx)
                    last[u] = idx
            def front(idx):
                si, kt, u0, u1 = items[idx]
                qf, qs, kf, ks, vf, vs, dmax, mf = sources[si]
                n = (u1 - u0 + 1) * 128
                cq = qb * 512 + u0 * 128
                pi = Sps.next()
                P.add("pe", lambda e, pi=pi, kf=kf, kt=kt, qf=qf, cq=cq, n=n: e.matmul(ps[pi][:, 0:n], lhsT=kf(kt), rhs=qf(cq, n), start=True, stop=True), reads=list(qs) + list(ks), writes=["ps%d" % pi])
                en, ea = er.next()
                P.add("act", lambda e, pi=pi, ea=ea, n=n: e.activation(out=ea[:, 0:n], in_=ps[pi][:, 0:n], func=AF.Exp, scale=0.125), reads=["ps%d" % pi], writes=[en])
                if mul_of_kt is not None:
                    mn, ma = mul_of_kt(kt)
                    P.add("dve", lambda e, ea=ea, ma=ma, n=n, u0=u0: e.tensor_tensor(out=ea[:, 0:n], in0=ea[:, 0:n], in1=ma[:, u0 * 128:u0 * 128 + n], op=ALU.mult), reads=[en, mn], writes=[en])
                for u in range(u0, u1 + 1):
                    dl = 4 * qb + u - kt
                    mi = mf(dl)
                    lo = (u - u0) * 128
                    if mi is not None:
                        P.add("dve", lambda e, ea=ea, lo=lo, mi=mi: e.tensor_tensor(out=ea[:, lo:lo + 128], in0=ea[:, lo:lo + 128], in1=masks[:, mi, :], op=ALU.mult), reads=[en, "masks"], writes=[en])
                return en, ea

            def back(idx, en, ea):
                si, kt, u0, u1 = items[idx]
                qf, qs, kf, ks, vf, vs, dmax, mf = sources[si]
                for u in range(u0, u1 + 1):
                    lo = (u - u0) * 128
                    P.add("pe", lambda e, ea=ea, lo=lo, u=u, vf=vf, kt=kt, idx=idx: e.matmul(o_of_u(u), lhsT=ea[:, lo:lo + 128], rhs=vf(kt), start=False, stop=(last[u] == idx), skip_group_check=True), reads=[en] + list(vs), writes=[o_slot])

            pend = front(0) if items else None
            for idx in range(len(items)):
                nxt = front(idx + 1) if idx + 1 < len(items) else None
                back(idx, *pend)
                pend = nxt

        mC = A.mark()
        gat = A.alloc(NT * 24).rearrange("p (k c) -> p k c", k=NT)
        for i in range(4):
            P.dma(lambda e, i=i: e.dma_start(out=gat[:, 8 * i:8 * i + 8, :], in_=gates_s[1024 * i:1024 * (i + 1), :].rearrange("(k p) c -> p k c", p=128)), reads=["gates_s"], writes=["gat"])
        cmask = A.alloc(2 * S, BF16).rearrange("p (a t) -> p a t", a=2)
        P.dma(lambda e: e.dma_start(out=cmask, in_=cmask_d.rearrange("a p t -> p a t")), writes=["cmask"])
        selmul = A.alloc(NT * 64).rearrange("p (k c) -> p k c", k=NT)
        seladd = A.alloc(NT * 64).rearrange("p (k c) -> p k c", k=NT)
        for i in range(4):
            P.dma(lambda e, i=i: e.dma_start(out=selmul[:, 8 * i:8 * i + 8, :], in_=selmul_d[1024 * i:1024 * (i + 1), :].rearrange("(k p) c -> p k c", p=128)), writes=["selmul"])
            P.dma(lambda e, i=i: e.dma_start(out=seladd[:, 8 * i:8 * i + 8, :], in_=seladd_d[1024 * i:1024 * (i + 1), :].rearrange("(k p) c -> p k c", p=128)), writes=["seladd"])
        esel = A.alloc(S, BF16)
        P.dma(lambda e: e.dma_start(out=esel[0:64, :], in_=esel_d), writes=["esel"])
        Qn = A.alloc(2 * S, BF16).rearrange("p (c t) -> p c t", c=2)
        KS = A.alloc(S, BF16)
        KW = A.alloc(S, BF16)
        VS1 = A.alloc(NT * 65, BF16).rearrange("p (k c) -> p k c", k=NT)
        VW1 = A.alloc(NT * 65, BF16).rearrange("p (k c) -> p k c", k=NT)
        P.add("dve", lambda e: e.memset(VS1[:, :, 64:65], 1.0), writes=["VS1"])
        P.add("dve", lambda e: e.memset(VW1[:, :, 64:65], 1.0), writes=["VW1"])
        er = mkring("E", 3, 512, BF16)
        msbr = mkring("Msb", 2, 512, BF16)
        impacc = A.alloc(4 * 64).rearrange("p (u c) -> p u c", u=4)
        Y = A.alloc(4 * 256).rearrange("p (u c) -> p u c", u=4)
        Yb = A.alloc(4 * 256, BF16).rearrange("p (u c) -> p u c", u=4)
        rd = A.alloc(4)
        gd = A.alloc(4)
        sc = A.alloc(64)
        sct = A.alloc(64)
        m8 = A.alloc(16)
        selm = A.alloc(64, BF16)
        selT = A.alloc(512, BF16)
        yst = mkring("yst", 2, 2 * 512, BF16)

        def wmask(dl):
            return 0 if dl == 0 else (1 if dl == 4 else None)

        def smask(dl):
            return 0 if dl == 0 else None

        def finish_branch(o_views, o_slots, qb, g, j, br, first_branch):
            hh = 4 * g + j
            for u in range(4):
                P.add("dve", lambda e, u=u: e.tensor_scalar(out=rd[:, u:u + 1], in0=o_views[u][:, 64:65], scalar1=1e-30, scalar2=None, op0=ALU.max), reads=o_slots, writes=["rd"])
            P.add("dve", lambda e: e.reciprocal(out=rd, in_=rd), reads=["rd"], writes=["rd"])
            P.add("dve", lambda e: e.tensor_tensor(out=gd, in0=rd, in1=gat[:, 4 * qb:4 * qb + 4, hh * 3 + br], op=ALU.mult), reads=["rd", "gat"], writes=["gd"])
            for u in range(4):
                if first_branch:
                    P.add("dve", lambda e, u=u: e.tensor_scalar(out=Y[:, u, j * 64:(j + 1) * 64], in0=o_views[u][:, 0:64], scalar1=gd[:, u:u + 1], scalar2=None, op0=ALU.mult), reads=o_slots + ["gd"], writes=["Y"])
                else:
                    P.add("dve", lambda e, u=u: e.scalar_tensor_tensor(out=Y[:, u, j * 64:(j + 1) * 64], in0=o_views[u][:, 0:64], scalar=gd[:, u:u + 1], in1=Y[:, u, j * 64:(j + 1) * 64], op0=ALU.mult, op1=ALU.add), reads=o_slots + ["gd", "Y"], writes=["Y"])

        for g in range(2):
            for c in range(2):
                P.dma(lambda e, c=c, g=g: e.dma_start(out=Qn[:, c, :], in_=qkt_s[CID_Q + 2 * g + c]), reads=["qkt_s"], writes=["Qn"])
            P.dma(lambda e, g=g: e.dma_start(out=KS, in_=qkt_s[CID_KS + g]), reads=["qkt_s"], writes=["KS"])
            P.dma(lambda e, g=g: e.dma_start(out=KW, in_=qkt_s[CID_KW + g]), reads=["qkt_s"], writes=["KW"])
            for i in range(4):
                P.dma(lambda e, i=i, g=g: e.dma_start(out=VS1[:, 8 * i:8 * i + 8, 0:64], in_=vtm_s[1024 * i:1024 * (i + 1), g * 64:(g + 1) * 64].rearrange("(k p) c -> p k c", p=128)), reads=["vtm_s"], writes=["VS1"])
                P.dma(lambda e, i=i, g=g: e.dma_start(out=VW1[:, 8 * i:8 * i + 8, 0:64], in_=vtm_s[1024 * i:1024 * (i + 1), 128 + g * 64:128 + (g + 1) * 64].rearrange("(k p) c -> p k c", p=128)), reads=["vtm_s"], writes=["VW1"])
            for qb in range(NB):
                c0 = qb * 512
                cts = [0, 1] if qb >= 4 else [0]
                for j in range(4):
                    base = 64 * (j % 2)
                    cj = j // 2
                    oa = ps[2].rearrange("p (u c) -> p u c", u=2)
                    ob = ps[3].rearrange("p (u c) -> p u c", u=2)
                    ov_ = [oa[:, 0, 0:129], oa[:, 1, 0:129], ob[:, 0, 0:129], ob[:, 1, 0:129]]
                    osl = ["ps2", "ps2", "ps3", "ps3"]
                    P.add("dve", lambda e: e.memset(ps[2], 0.0), writes=["ps2"])
                    P.add("dve", lambda e: e.memset(ps[3], 0.0), writes=["ps3"])
                    for ct in cts:
                        pi = Sps.next()
                        P.add("pe", lambda e, pi=pi, ct=ct, base=base, cj=cj, g=g, c0=c0: e.matmul(ps[pi], lhsT=kcmp[base:base + 64, g, ct * 128:(ct + 1) * 128], rhs=Qn[base:base + 64, cj, c0:c0 + 512], start=True, stop=True), reads=["kcmp", "Qn"], writes=["ps%d" % pi])
                        en, ea = er.next()
                        P.add("act", lambda e, pi=pi, ea=ea: e.activation(out=ea, in_=ps[pi], func=AF.Exp, scale=0.125), reads=["ps%d" % pi], writes=[en])
                        P.add("dve", lambda e, ea=ea, ct=ct, c0=c0: e.tensor_tensor(out=ea, in0=ea, in1=cmask[:, ct, c0:c0 + 512], op=ALU.mult), reads=[en, "cmask"], writes=[en])
                        for u in range(4):
                            P.add("pe", lambda e, ea=ea, u=u, ct=ct, g=g, ov_=ov_, sp_=(ct == cts[-1]): e.matmul(ov_[u], lhsT=ea[:, u * 128:(u + 1) * 128], rhs=vc1[:, ct, g, :], start=False, stop=sp_, skip_group_check=True), reads=[en, "vc1"], writes=[osl[u]])
                    finish_branch(ov_, ["ps2", "ps3"], qb, g, j, 0, True)
                    for u in range(4):
                        if j == 0:
                            P.add("dve", lambda e, u=u: e.tensor_scalar(out=impacc[:, u, :], in0=ov_[u][:, 65:129], scalar1=rd[:, u:u + 1], scalar2=None, op0=ALU.mult), reads=["ps2", "ps3", "rd"], writes=["impacc"])
                        else:
                            P.add("dve", lambda e, u=u: e.scalar_tensor_tensor(out=impacc[:, u, :], in0=ov_[u][:, 65:129], scalar=rd[:, u:u + 1], in1=impacc[:, u, :], op0=ALU.mult, op1=ALU.add), reads=["ps2", "ps3", "rd", "impacc"], writes=["impacc"])
                for u in range(4):
                    tt = 4 * qb + u
                    P.add("dve", lambda e, u=u, tt=tt: e.tensor_tensor(out=sc, in0=impacc[:, u, :], in1=selmul[:, tt, :], op=ALU.mult), reads=["impacc", "selmul"], writes=["sc"])
                    P.add("dve", lambda e, tt=tt: e.tensor_tensor(out=sc, in0=sc, in1=seladd[:, tt, :], op=ALU.add), reads=["sc", "seladd"], writes=["sc"])
                    P.add("dve", lambda e: e.max(out=m8[:, 0:8], in_=sc), reads=["sc"], writes=["m8"])
                    P.add("dve", lambda e: e.match_replace(out=sct, in_to_replace=m8[:, 0:8], in_values=sc, imm_value=-1e30), reads=["sc", "m8"], writes=["sct"])
                    P.add("dve", lambda e: e.max(out=m8[:, 8:16], in_=sct), reads=["sct"], writes=["m8"])
                    P.add("dve", lambda e: e.tensor_scalar(out=selm, in0=sc, scalar1=m8[:, 15:16], scalar2=None, op0=ALU.is_ge), reads=["sc", "m8"], writes=["selm"])
                    P.add("pe", lambda e: e.transpose(out=psb[7][0:64, 0:128], in_=selm, identity=ident), reads=["selm", "ident"], writes=["ps7"])
                    P.add("act", lambda e, u=u: e.activation(out=selT[0:64, u * 128:(u + 1) * 128], in_=psb[7][0:64, 0:128], func=AF.Copy), reads=["ps7"], writes=["selT"])
                nkt = 4 * qb + 4
                for j in range(4):
                    base = 64 * (j % 2)
                    cj = j // 2
                    o5 = ps[5].rearrange("p (u c) -> p u c", u=4)
                    o6 = ps[6].rearrange("p (u c) -> p u c", u=4)
                    qf = lambda cq, n, base=base, cj=cj: Qn[base:base + 64, cj, cq:cq + n]

                    def mul_of_kt(kt):
                        mn, ma = msbr.next()
                        P.add("pe", lambda e, kt=kt: e.matmul(ps[4], lhsT=esel[0:64, kt * 128:(kt + 1) * 128], rhs=selT[0:64, :], start=True, stop=True), reads=["esel", "selT"], writes=["ps4"])
                        P.add("act", lambda e, ma=ma: e.activation(out=ma, in_=ps[4], func=AF.Copy), reads=["ps4"], writes=[mn])
                        return mn, ma

                    banded(qb, [(qf, ["Qn"], lambda kt, base=base: KS[base:base + 64, kt * 128:(kt + 1) * 128], ["KS"], lambda kt: VS1[:, kt, :], ["VS1"], 64, smask)],
                           lambda u: o5[:, u, 0:65], "ps5", er, ps[5], mul_of_kt=mul_of_kt)
                    finish_branch([o5[:, u, :] for u in range(4)], ["ps5"], qb, g, j, 1, False)
                    banded(qb, [(qf, ["Qn"], lambda kt, base=base: KW[base:base + 64, kt * 128:(kt + 1) * 128], ["KW"], lambda kt: VW1[:, kt, :], ["VW1"], 4, wmask)],
                           lambda u: o6[:, u, 0:65], "ps6", er, ps[6])
                    finish_branch([o6[:, u, :] for u in range(4)], ["ps6"], qb, g, j, 2, False)
                P.add("act", lambda e: e.activation(out=Yb, in_=Y, func=AF.Copy), reads=["Y"], writes=["Yb"])
                ysn, ys_ = yst.next()
                ys = ys_.rearrange("p (c t) -> p c t", c=2)
                for u in range(4):
                    for c in range(2):
                        P.add("pe", lambda e, u=u, c=c: e.transpose(out=psb[7][:, c * 128:(c + 1) * 128], in_=Yb[:, u, c * 128:(c + 1) * 128], identity=ident), reads=["Yb", "ident"], writes=["ps7"])
                    P.add("act", lambda e, u=u, ys=ys: e.activation(out=ys[:, :, u * 128:(u + 1) * 128], in_=psb[7][:, 0:256].rearrange("p (c t) -> p c t", c=2), func=AF.Copy), reads=["ps7"], writes=[ysn])
                P.dma(lambda e, ys=ys, g=g, c0=c0: e.dma_start(out=yt_s[2 * g:2 * g + 2, :, c0:c0 + 512].rearrange("c p t -> p c t"), in_=ys), reads=[ysn], writes=["yt_s"], queue="pool")
        A.release(mB)
        P.fence()
        if stop_after == "C":
            P.emit(final_slots=["yt_s"])
            return nc, P, A

        mD = A.mark()
        DQ = A.alloc(3 * S, BF16).rearrange("p (g t) -> p g t", g=3)
        DK = A.alloc(3 * S, BF16).rearrange("p (g t) -> p g t", g=3)
        DV = A.alloc(6 * NT * 65, BF16).rearrange("p (a k c) -> p a k c", a=6, k=NT)
        P.add("dve", lambda e: e.memset(DV[:, :, :, 64:65], 1.0), writes=["DV"])
        er = mkring("Ed", 3, 512, BF16)
        Yd = A.alloc(4 * 128).rearrange("p (u c) -> p u c", u=4)
        Ydb = A.alloc(4 * 128, BF16).rearrange("p (u c) -> p u c", u=4)
        rdd = A.alloc(4)
        ydst = mkring("ydst", 2, 512, BF16)
        ops_ring = Ring([5, 6])

        def dmask_fn(gi):
            d = DIL_PAT[gi][1]
            if d == 1:
                return lambda dl: 0 if dl == 0 else 2
            b = 3 if d == 4 else 6
            return lambda dl, b=b, d=d: (b + 1) if dl == 0 else ((b + 2) if dl == d else b)

        for pr in range(2):
            for gi in range(3):
                P.dma(lambda e, gi=gi, pr=pr: e.dma_start(out=DQ[:, gi, :], in_=qkt_s[CID_DIL + gi * 4 + pr]), reads=["qkt_s"], writes=["DQ"])
                P.dma(lambda e, gi=gi, pr=pr: e.dma_start(out=DK[:, gi, :], in_=qkt_s[CID_DIL + gi * 4 + 2 + pr]), reads=["qkt_s"], writes=["DK"])
                for hh in range(2):
                    col = 256 + (gi * 4 + pr * 2 + hh) * 64
                    for i in range(4):
                        P.dma(lambda e, i=i, gi=gi, hh=hh, col=col: e.dma_start(out=DV[:, gi * 2 + hh, 8 * i:8 * i + 8, 0:64], in_=vtm_s[1024 * i:1024 * (i + 1), col:col + 64].rearrange("(k p) c -> p k c", p=128)), reads=["vtm_s"], writes=["DV"])
            for qb in range(NB):
                c0 = qb * 512
                for hh in range(2):
                    base = 64 * hh
                    opi = ops_ring.next()
                    ov4 = ps[opi].rearrange("p (u c) -> p u c", u=4)
                    srcs = []
                    for gi in range(3):
                        srcs.append((lambda cq, n, gi=gi, base=base: DQ[base:base + 64, gi, cq:cq + n], ["DQ"],
                                     lambda kt, gi=gi, base=base: DK[base:base + 64, gi, kt * 128:(kt + 1) * 128], ["DK"],
                                     lambda kt, gi=gi, hh=hh: DV[:, gi * 2 + hh, kt, :], ["DV"], DIL_PAT[gi][1], dmask_fn(gi)))
                    banded(qb, srcs, lambda u, ov4=ov4: ov4[:, u, 0:65], "ps%d" % opi, er, ps[opi])
                    P.add("dve", lambda e, ov4=ov4: e.reciprocal(out=rdd, in_=ov4[:, :, 64]), reads=["ps%d" % opi], writes=["rdd"])
                    for u in range(4):
                        P.add("dve", lambda e, u=u, ov4=ov4, hh=hh: e.tensor_scalar(out=Ydb[:, u, hh * 64:(hh + 1) * 64], in0=ov4[:, u, 0:64], scalar1=rdd[:, u:u + 1], scalar2=None, op0=ALU.mult), reads=["ps%d" % opi, "rdd"], writes=["Ydb"])
                ysn, ys = ydst.next()
                for u in range(4):
                    P.add("pe", lambda e, u=u: e.transpose(out=psb[7][:, 0:128], in_=Ydb[:, u, :], identity=ident), reads=["Ydb", "ident"], writes=["ps7"])
                    P.add("act", lambda e, u=u, ys=ys: e.activation(out=ys[:, u * 128:(u + 1) * 128], in_=psb[7][:, 0:128], func=AF.Copy), reads=["ps7"], writes=[ysn])
                P.dma(lambda e, ys=ys, pr=pr, c0=c0: e.dma_start(out=yt_s[4 + pr, :, c0:c0 + 512], in_=ys), reads=[ysn], writes=["yt_s"], queue="pool")
        A.release(mD)
        P.fence()
        if stop_after == "D":
            P.emit(final_slots=["yt_s"])
            return nc, P, A

        mE = A.mark()
        wun = A.alloc(4 * D, BF16).rearrange("p (a b) -> p a b", a=4)
        wud = A.alloc(2 * D, BF16).rearrange("p (a b) -> p a b", a=2)
        wmg = A.alloc(8 * 2048, BF16).rearrange("p (a b) -> p a b", a=8)
        wo = A.alloc(8 * D, BF16).rearrange("p (a b) -> p a b", a=8)
        g1tE = A.alloc(8)
        P.dma(lambda e: e.dma_start(out=g1tE, in_=g1_d), writes=["g1tE"])
        mE1 = A.mark()
        wst = mkring("wstE", 2, 2048)
        for a in range(4):
            sn, sa = wst.next()
            P.dma(lambda e, sa=sa, a=a: e.dma_start(out=sa[:, 0:D], in_=wun_d[a * 128:(a + 1) * 128, :]), writes=[sn])
            P.add("dve", lambda e, sa=sa, a=a: e.tensor_copy(out=wun[:, a, :], in_=sa[:, 0:D]), reads=[sn], writes=["wun"])
        for a in range(2):
            sn, sa = wst.next()
            P.dma(lambda e, sa=sa, a=a: e.dma_start(out=sa[:, 0:D], in_=wud_d[a * 128:(a + 1) * 128, :]), writes=[sn])
            P.add("dve", lambda e, sa=sa, a=a: e.tensor_copy(out=wud[:, a, :], in_=sa[:, 0:D]), reads=[sn], writes=["wud"])
        for a in range(8):
            sn, sa = wst.next()
            P.dma(lambda e, sa=sa, a=a: e.dma_start(out=sa[:, 0:D], in_=wo_d[a * 128:(a + 1) * 128, :]), writes=[sn])
            P.add("dve", lambda e, sa=sa, a=a: e.tensor_copy(out=wo[:, a, :], in_=sa[:, 0:D]), reads=[sn], writes=["wo"])
        for a in range(8):
            sn, sa = wst.next()
            P.dma(lambda e, sa=sa, a=a: e.dma_start(out=sa, in_=wmg_d[a * 128:(a + 1) * 128, :]), writes=[sn])
            P.add("dve", lambda e, sa=sa, a=a: e.tensor_scalar(out=wmg[:, a, :], in0=sa, scalar1=g1tE[:, a:a + 1], scalar2=None, op0=ALU.mult), reads=[sn, "g1tE"], writes=["wmg"])
        A.release(mE1)
        P.fence()
        ytr = mkring("ytE", 2, 6 * 512, BF16)
        htr = mkring("htE", 2, 8 * 512, BF16)
        sg0 = A.alloc(512)
        sg1 = A.alloc(512)
        t1 = A.alloc(512)
        mT = A.alloc(8 * 512, BF16).rearrange("p (a b) -> p a b", a=8)
        xr = mkring("xtE", 2, 1024)
        x2r = mkring("x2E", 2, 1024)
        junkE = A.alloc(1024)
        ssr = mkring("ssE", 2, 1)
        hbr = mkring("hbE", 2, 1024, BF16)
        h2r = mkring("h2E", 2, 8 * 512, BF16)
        for b in range(NB):
            c0 = b * 512
            yn, ya_ = ytr.next()
            ya = ya_.rearrange("p (a b) -> p a b", a=6)
            hn, ha_ = htr.next()
            ha = ha_.rearrange("p (a b) -> p a b", a=8)
            P.dma(lambda e, ya=ya, c0=c0: e.dma_start(out=ya, in_=yt_s[:, :, c0:c0 + 512].rearrange("c p t -> p c t")), reads=["yt_s"], writes=[yn])
            P.dma(lambda e, ha=ha, c0=c0: e.dma_start(out=ha, in_=ht_s[:, :, c0:c0 + 512].rearrange("c p t -> p c t")), reads=["ht_s"], writes=[hn])
            for dc in range(8):
                dsl = slice(dc * 128, (dc + 1) * 128)
                for a in range(4):
                    P.add("pe", lambda e, a=a, dsl=dsl, ya=ya: e.matmul(ps[0], lhsT=wun[:, a, dsl], rhs=ya[:, a, :], start=(a == 0), stop=(a == 3)), reads=["wun", yn], writes=["ps0"])
                for a in range(2):
                    P.add("pe", lambda e, a=a, dsl=dsl, ya=ya: e.matmul(ps[1], lhsT=wud[:, a, dsl], rhs=ya[:, 4 + a, :], start=(a == 0), stop=(a == 1)), reads=["wud", yn], writes=["ps1"])
                for hf in range(2):
                    for a in range(8):
                        P.add("pe", lambda e, a=a, hf=hf, dc=dc, ha=ha: e.matmul(ps[2 + hf], lhsT=wmg[:, a, hf * 1024 + dc * 128:hf * 1024 + (dc + 1) * 128], rhs=ha[:, a, :], start=(a == 0), stop=(a == 7)), reads=["wmg", hn], writes=["ps%d" % (2 + hf)])
                P.add("act", lambda e: e.activation(out=sg0, in_=ps[2], func=AF.Sigmoid), reads=["ps2"], writes=["sg0"])
                P.add("act", lambda e: e.activation(out=sg1, in_=ps[3], func=AF.Sigmoid), reads=["ps3"], writes=["sg1"])
                P.add("dve", lambda e: e.tensor_tensor(out=t1, in0=sg0, in1=ps[0], op=ALU.mult), reads=["sg0", "ps0"], writes=["t1"])
                P.add("dve", lambda e: e.tensor_tensor(out=sg1, in0=sg1, in1=ps[1], op=ALU.mult), reads=["sg1", "ps1"], writes=["sg1"])
                P.add("dve", lambda e, dc=dc: e.tensor_tensor(out=mT[:, dc, :], in0=t1, in1=sg1, op=ALU.add), reads=["t1", "sg1"], writes=["mT"])
            h2n, h2_ = h2r.next()
            h2 = h2_.rearrange("p (a b) -> p a b", a=8)
            for u in range(4):
                t0 = c0 + u * 128
                for hf in range(2):
                    for a in range(8):
                        P.add("pe", lambda e, a=a, hf=hf, u=u: e.matmul(ps[4 + hf], lhsT=mT[:, a, u * 128:(u + 1) * 128], rhs=wo[:, a, hf * 512:(hf + 1) * 512], start=(a == 0), stop=(a == 7)), reads=["wo", "mT"], writes=["ps%d" % (4 + hf)])
                xn, xt = xr.next()
                P.dma(lambda e, xt=xt, t0=t0: e.dma_start(out=xt, in_=x_d[t0:t0 + 128, :]), writes=[xn])
                x2n, x2 = x2r.next()
                for hf in range(2):
                    P.add("dve", lambda e, hf=hf, xt=xt, x2=x2: e.tensor_tensor(out=x2[:, hf * 512:(hf + 1) * 512], in0=xt[:, hf * 512:(hf + 1) * 512], in1=ps[4 + hf], op=ALU.add), reads=[xn, "ps%d" % (4 + hf)], writes=[x2n])
                P.dma(lambda e, x2=x2, t0=t0: e.dma_start(out=x2_s[t0:t0 + 128, :], in_=x2), reads=[x2n], writes=["x2_s"], queue="pool")
                sn, ss = ssr.next()
                P.add("act", lambda e, x2=x2, ss=ss: e.activation(out=junkE, in_=x2, func=AF.Square, accum_out=ss), reads=[x2n], writes=["junkE", sn])
                P.add("act", lambda e, ss=ss: e.activation(out=ss, in_=ss, func=AF.Sqrt, scale=1.0 / D, bias=EPS), reads=[sn], writes=[sn])
                P.add("dve", lambda e, ss=ss: e.reciprocal(out=ss, in_=ss), reads=[sn], writes=[sn])
                hbn, hb = hbr.next()
                P.add("dve", lambda e, x2=x2, ss=ss, hb=hb: e.tensor_scalar(out=hb, in0=x2, scalar1=ss, scalar2=None, op0=ALU.mult), reads=[x2n, sn], writes=[hbn])
                for c in range(8):
                    P.add("pe", lambda e, c=c, hb=hb: e.transpose(out=psb[6][:, c * 128:(c + 1) * 128], in_=hb[:, c * 128:(c + 1) * 128], identity=ident), reads=[hbn, "ident"], writes=["ps6"])
                P.add("act", lambda e, u=u, h2=h2: e.activation(out=h2[:, :, u * 128:(u + 1) * 128], in_=psb[6].rearrange("p (a b) -> p a b", a=8), func=AF.Copy), reads=["ps6"], writes=[h2n])
            P.dma(lambda e, h2=h2, c0=c0: e.dma_start(out=h2t_s[:, :, c0:c0 + 512].rearrange("c p t -> p c t"), in_=h2), reads=[h2n], writes=["h2t_s"], queue="pool")
        A.release(mE)
        P.fence()
        if stop_after == "E":
            P.emit(final_slots=["x2_s", "h2t_s"])
            return nc, P, A

        wq = A.alloc(8 * D, BF16).rearrange("p (a b) -> p a b", a=8)
        g2t = A.alloc(8)
        skbd = A.alloc(256, BF16)
        P.dma(lambda e: e.dma_start(out=g2t, in_=g2_d), writes=["g2t"])
        mF1 = A.mark()
        wst = mkring("wstF", 2, 1024)
        for a in range(8):
            sn, sa = wst.next()
            P.dma(lambda e, sa=sa, a=a: e.dma_start(out=sa, in_=wq_d[a * 128:(a + 1) * 128, :]), writes=[sn])
            P.add("dve", lambda e, sa=sa, a=a: e.tensor_scalar(out=wq[:, a, :], in0=sa, scalar1=g2t[:, a:a + 1], scalar2=None, op0=ALU.mult), reads=[sn, "g2t"], writes=["wq"])
        sn, sa = wst.next()
        P.dma(lambda e, sa=sa: e.dma_start(out=sa[:, 0:256], in_=skbd_d), writes=[sn])
        P.add("dve", lambda e, sa=sa: e.tensor_copy(out=skbd, in_=sa[:, 0:256]), reads=[sn], writes=["skbd"])
        A.release(mF1)
        P.fence()
        hg = A.alloc(8 * 256, BF16).rearrange("p (a b) -> p a b", a=8)
        qtg = A.alloc(8 * 256, BF16).rearrange("p (a b) -> p a b", a=8)
        ssb = A.alloc(8 * 256).rearrange("p (h n) -> p h n", h=8)
        stmp = A.alloc(256)
        T16 = A.alloc(256).rearrange("p (a b) -> p a b", a=16)
        negm = A.alloc(16)
        ab = A.alloc(8 * 256).rearrange("p (h n) -> p h n", h=8)
        abt = A.alloc(256).rearrange("p (a b) -> p a b", a=16)
        Pc = A.alloc(8 * 256).rearrange("p (h n) -> p h n", h=8)
        P16 = A.alloc(128).rearrange("p (h n) -> p h n", h=8)
        Zs = A.alloc(8)
        a2 = A.alloc(8 * 128).rearrange("p (h n) -> p h n", h=8)
        a2t = A.alloc(128).rearrange("p (h n) -> p h n", h=8)
        pdr = mkring("Pd", 2, 1024)
        whr = mkring("Wh", 16, 1024, BF16)
        WT = A.alloc(128 * 256, BF16).rearrange("p (e t) -> p e t", e=128)
        usr = mkring("Us", 4, 1024, BF16)
        vsr = mkring("Vs", 4, 1024, BF16)
        ggr = mkring("Gg", 2, 256, BF16)
        gtr = mkring("GT", 2, 256, BF16)
        xr = mkring("xtG", 2, 1024)
        wps = Ring([2, 3])
        aps = Ring([0, 1])
        NG = S // 256
        for G in range(NG):
            g0 = G * 256
            P.dma(lambda e, g0=g0: e.dma_start(out=hg, in_=h2t_s[:, :, g0:g0 + 256].rearrange("c p t -> p c t")), reads=["h2t_s"], writes=["hg"])
            for h in range(8):
                pi = aps.next()
                for a in range(8):
                    P.add("pe", lambda e, pi=pi, a=a, h=h: e.matmul(ps[pi][:, 0:256], lhsT=wq[:, a, h * 128:(h + 1) * 128], rhs=hg[:, a, :], start=(a == 0), stop=(a == 7)), reads=["wq", "hg"], writes=["ps%d" % pi])
                P.add("act", lambda e, pi=pi, h=h: e.activation(out=qtg[:, h, :], in_=ps[pi][:, 0:256], func=AF.Copy), reads=["ps%d" % pi], writes=["qtg"])
            for w in range(2):
                for h in range(8):
                    pi = 4 + h // 2
                    P.add("pe", lambda e, pi=pi, h=h, w=w: e.matmul(ps[pi][:, (h % 2) * 256:(h % 2) * 256 + 256], lhsT=qtg[:, h, w * 128:(w + 1) * 128], rhs=skbd, start=True, stop=True), reads=["qtg", "skbd"], writes=["ps%d" % pi])
                for k in range(4):
                    P.add("act", lambda e, k=k: e.activation(out=ssb[:, 2 * k:2 * k + 2, :], in_=ps[4 + k].rearrange("p (a n) -> p a n", a=2), func=AF.Copy), reads=["ps%d" % (4 + k)], writes=["ssb"])
                for hc in range(16):
                    h, c = hc // 2, hc % 2
                    src = ssb[:, h, c * 128:(c + 1) * 128]
                    P.add("dve", lambda e, src=src, hc=hc: e.max(out=T16[:, hc, 0:8], in_=src), reads=["ssb"], writes=["T16"])
                    P.add("dve", lambda e, src=src, hc=hc: e.match_replace(out=stmp[:, 0:128], in_to_replace=T16[:, hc, 0:8], in_values=src, imm_value=-1e30), reads=["ssb", "T16"], writes=["stmp"])
                    P.add("dve", lambda e, hc=hc: e.max(out=T16[:, hc, 8:16], in_=stmp[:, 0:128]), reads=["stmp"], writes=["T16"])
                P.add("dve", lambda e: e.tensor_scalar(out=negm, in0=T16[:, :, 0], scalar1=-1.0, scalar2=None, op0=ALU.mult), reads=["T16"], writes=["negm"])
                for hc in range(16):
                    h, c = hc // 2, hc % 2
                    P.add("act", lambda e, h=h, c=c, hc=hc: e.activation(out=ab[:, h, c * 128:(c + 1) * 128], in_=ssb[:, h, c * 128:(c + 1) * 128], func=AF.Exp, bias=negm[:, hc:hc + 1]), reads=["ssb", "negm"], writes=["ab"])
                    P.add("act", lambda e, hc=hc: e.activation(out=abt[:, hc, :], in_=T16[:, hc, :], func=AF.Exp, bias=negm[:, hc:hc + 1]), reads=["T16", "negm"], writes=["abt"])
                abt4 = abt.rearrange("p (h c) n -> p h c n", c=2)

                def cand_top(at_ap, outname):
                    P.add("dve", lambda e: e.tensor_tensor(out=Pc.rearrange("p h (i j) -> p h i j", i=16), in0=at_ap.unsqueeze(3).to_broadcast([128, 8, 16, 16]), in1=abt4[:, :, 1, :].unsqueeze(2).to_broadcast([128, 8, 16, 16]), op=ALU.mult), reads=["abt", "a2t"], writes=["Pc"])
                    for h in range(8):
                        P.add("dve", lambda e, h=h: e.max(out=P16[:, h, 0:8], in_=Pc[:, h, :]), reads=["Pc"], writes=["P16"])
                        P.add("dve", lambda e, h=h: e.match_replace(out=stmp, in_to_replace=P16[:, h, 0:8], in_values=Pc[:, h, :], imm_value=-1.0), reads=["Pc", "P16"], writes=["stmp"])
                        P.add("dve", lambda e, h=h: e.max(out=P16[:, h, 8:16], in_=stmp), reads=["stmp"], writes=["P16"])

                cand_top(abt4[:, :, 0, :], "P16")
                P.add("dve", lambda e: e.reduce_sum(out=Zs, in_=P16, axis=AX.X), reads=["P16"], writes=["Zs"])
                P.add("dve", lambda e: e.reciprocal(out=Zs, in_=Zs), reads=["Zs"], writes=["Zs"])
                P.add("dve", lambda e: e.tensor_tensor(out=a2, in0=ab[:, :, 0:128], in1=Zs.unsqueeze(2).to_broadcast([128, 8, 128]), op=ALU.mult), reads=["ab", "Zs"], writes=["a2"])
                P.add("dve", lambda e: e.tensor_tensor(out=a2t, in0=abt4[:, :, 0, :], in1=Zs.unsqueeze(2).to_broadcast([128, 8, 16]), op=ALU.mult), reads=["abt", "Zs"], writes=["a2t"])
                cand_top(a2t, "P16")
                for eb in range(16):
                    whs = []
                    for h in range(8):
                        pn, pd = pdr.next()
                        P.add("dve", lambda e, pd=pd, h=h, eb=eb: e.tensor_tensor(out=pd.rearrange("p (i j) -> p i j", i=8), in0=a2[:, h, eb * 8:(eb + 1) * 8].unsqueeze(2).to_broadcast([128, 8, 128]), in1=ab[:, h, 128:256].unsqueeze(1).to_broadcast([128, 8, 128]), op=ALU.mult), reads=["a2", "ab"], writes=[pn])
                        wn, wh = whr.next()
                        P.add("dve", lambda e, pd=pd, wh=wh, h=h: e.scalar_tensor_tensor(out=wh, in0=pd, scalar=P16[:, h, 15:16], in1=pd, op0=ALU.is_ge, op1=ALU.mult), reads=[pn, "P16"], writes=[wn])
                        whs.append((wn, wh))
                    for q4 in range(2):
                        pi = wps.next()
                        for ci in range(4):
                            cc = q4 * 4 + ci
                            for h in range(8):
                                wn, wh = whs[h]
                                P.add("pe", lambda e, pi=pi, ci=ci, cc=cc, wh=wh, h=h: e.matmul(ps[pi][:, ci * 128:(ci + 1) * 128], lhsT=wh[:, cc * 128:(cc + 1) * 128], rhs=ident, start=(h == 0), stop=(h == 7)), reads=[wn, "ident"], writes=["ps%d" % pi])
                        e1 = eb * 8 + q4 * 4
                        P.add("act", lambda e, pi=pi, e1=e1, w=w: e.activation(out=WT[:, e1:e1 + 4, w * 128:(w + 1) * 128], in_=ps[pi].rearrange("p (a t) -> p a t", a=4), func=AF.Copy), reads=["ps%d" % pi], writes=["WT"])
            def gfront(e1):
                un, us_ = usr.next()
                us = us_.rearrange("p (a b) -> p a b", a=8)
                vn, vs = vsr.next()
                P.dma(lambda e, us_=us_, e1=e1: e.dma_start(out=us_, in_=utb_s[e1]), reads=["utb_s"], writes=[un])
                P.dma(lambda e, vs=vs, e1=e1: e.dma_start(out=vs, in_=vb_s[e1]), reads=["vb_s"], writes=[vn])
                pi = aps.next()
                for a in range(8):
                    P.add("pe", lambda e, pi=pi, a=a, us=us: e.matmul(ps[pi][:, 0:256], lhsT=us[:, a, :], rhs=hg[:, a, :], start=(a == 0), stop=(a == 7)), reads=[un, "hg"], writes=["ps%d" % pi])
                gn, gg = ggr.next()
                P.add("act", lambda e, pi=pi, gg=gg: e.activation(out=gg, in_=ps[pi][:, 0:256], func=AF.Gelu_apprx_tanh), reads=["ps%d" % pi], writes=[gn])
                tn, gt = gtr.next()
                P.add("dve", lambda e, gg=gg, gt=gt, e1=e1: e.tensor_tensor(out=gt, in0=gg, in1=WT[:, e1, :], op=ALU.mult), reads=[gn, "WT"], writes=[tn])
                return tn, gt, vn, vs

            def gback(e1, tn, gt, vn, vs):
                for w in range(2):
                    for hf in range(2):
                        pj = 4 + w * 2 + hf
                        P.add("pe", lambda e, pj=pj, w=w, hf=hf, gt=gt, vs=vs, e1=e1: e.matmul(ps[pj], lhsT=gt[:, w * 128:(w + 1) * 128], rhs=vs[:, hf * 512:(hf + 1) * 512], start=(e1 == 0), stop=(e1 == 127)), reads=[tn, vn], writes=["ps%d" % pj])

            pend = gfront(0)
            for e1 in range(128):
                nxt = gfront(e1 + 1) if e1 + 1 < 128 else None
                gback(e1, *pend)
                pend = nxt
            for w in range(2):
                t0 = g0 + w * 128
                xn, xt = xr.next()
                P.dma(lambda e, xt=xt, t0=t0: e.dma_start(out=xt, in_=x2_s[t0:t0 + 128, :]), reads=["x2_s"], writes=[xn])
                for hf in range(2):
                    pj = 4 + w * 2 + hf
                    P.add("dve", lambda e, xt=xt, hf=hf, pj=pj: e.tensor_tensor(out=xt[:, hf * 512:(hf + 1) * 512], in0=xt[:, hf * 512:(hf + 1) * 512], in1=ps[pj], op=ALU.add), reads=[xn, "ps%d" % pj], writes=[xn])
                P.dma(lambda e, xt=xt, t0=t0: e.dma_start(out=out_d[t0:t0 + 128, :], in_=xt), reads=[xn], writes=["out"], queue="pool")
        P.emit(final_slots=["out"])
    return nc, P, A


def _consts():
    bf = ml_dtypes.bfloat16
    c = {}
    c["ident"] = np.eye(128, dtype=np.float32).astype(bf)
    bd = np.zeros((128, 128), np.float32)
    bd[:64, :64] = 1
    bd[64:, 64:] = 1
    c["bdones"] = bd.astype(bf)
    Rm = np.zeros((128, 128), np.float32)
    for hb in (0, 64):
        for i in range(8):
            Rm[hb + i, hb + i + 8] = -1.0
            Rm[hb + i + 8, hb + i] = 1.0
    c["rotT"] = np.ascontiguousarray(Rm.T).astype(bf)
    half = 8
    inv = (500000.0 ** (-(np.arange(half, dtype=np.float32) * 2.0 / 16))).astype(np.float32)

    def tables(pos):
        ang = pos.astype(np.float32)[None, :] * inv[:, None]
        cs = np.ones((128, pos.shape[0]), np.float32)
        sn = np.zeros((128, pos.shape[0]), np.float32)
        for hb in (0, 64):
            cs[hb:hb + 8] = np.cos(ang)
            cs[hb + 8:hb + 16] = np.cos(ang)
            sn[hb:hb + 8] = np.sin(ang)
            sn[hb + 8:hb + 16] = np.sin(ang)
        return cs, sn

    c["cosT"], c["sinT"] = tables(np.arange(S))
    pc = np.arange(256) * 16 + 31
    c["cosC"], c["sinC"] = tables(pc)
    t = np.arange(S)
    cm = np.zeros((2, 128, S), np.float32)
    for ct in range(2):
        cidx = ct * 128 + np.arange(128)
        cm[ct] = ((cidx[:, None] * 16 + 31 <= t[None, :]) & (cidx[:, None] < 255)).astype(np.float32)
    c["cmask"] = cm.astype(bf)
    k = np.arange(128)[:, None]
    q = np.arange(128)[None, :]
    ms = np.zeros((128, 9, 128), np.float32)
    ms[:, 0] = k <= q
    ms[:, 1] = k > q
    ms[:, 2] = k >= q
    for b, d in ((3, 4), (6, 16)):
        res = ((q - k) % d) == 0
        ms[:, b] = res
        ms[:, b + 1] = res & (k <= q)
        ms[:, b + 2] = res & (k >= q)
    c["masks"] = ms.astype(bf)
    n_cmp = 255
    s0 = np.arange(n_cmp) * 16
    s1 = s0 + 32
    b0 = np.arange(64) * 64
    b1 = b0 + 64
    ovm = np.clip(np.minimum(s1[:, None], b1[None, :]) - np.maximum(s0[:, None], b0[None, :]), 0, None) / 32.0
    ovp = np.zeros((256, 64), np.float32)
    ovp[:255] = ovm
    c["ov"] = ovp.reshape(2, 128, 64).astype(bf)
    blk = np.arange(64)[None, :]
    cur = (t // 64)[:, None]
    forced = (blk == 0) | (blk == cur) | (blk == cur - 1)
    causal = blk * 64 <= t[:, None]
    c["selmul"] = (causal & ~forced).astype(np.float32)
    c["seladd"] = np.where(forced, 1e3, np.where(causal, 0.0, -1.0)).astype(np.float32)
    es = np.zeros((64, S), np.float32)
    es[(t // 64), t] = 1.0
    c["esel"] = es.astype(bf)
    return c


def _layout_weights(inp):
    w_in = np.asarray(inp["w_in"], np.float32)
    o = {}
    cols = []
    for cch in range(4):
        cols.append(np.arange(cch * 128, (cch + 1) * 128))
    cols.append(np.arange(512, 640))
    cols.append(np.arange(640, 768))
    for base in (768, 1024):
        for g in range(2):
            cc = base + g * 64 + np.arange(64)
            cols.append(np.concatenate([cc, cc]))
    for g in range(3):
        for r in range(2):
            for pr in range(2):
                cols.append(1304 + g * 768 + r * 256 + pr * 128 + np.arange(128))
    fm_cols = np.concatenate(cols)
    assert fm_cols.shape[0] == NFM * 128
    o["wfm"] = np.ascontiguousarray(w_in[:, fm_cols])
    tm_cols = np.concatenate([np.arange(896, 1024), np.arange(1152, 1280)] +
                             [1304 + g * 768 + 512 + np.arange(256) for g in range(3)] + [np.arange(1280, 1304)])
    assert tm_cols.shape[0] == NTM
    o["wtm"] = np.ascontiguousarray(w_in[:, tm_cols])
    o["wmg"] = np.ascontiguousarray(w_in[:, 3608:5656])
    o["g1"] = np.ascontiguousarray(np.asarray(inp["norm1_g"], np.float32).reshape(8, 128).T)
    o["g2"] = np.ascontiguousarray(np.asarray(inp["norm2_g"], np.float32).reshape(8, 128).T)
    gains = np.ones((128, NFM + 1), np.float32)
    qn = np.asarray(inp["nsa_q_norm"], np.float32)
    kn = np.asarray(inp["nsa_k_norm"], np.float32)
    dq = np.asarray(inp["dil_q_norm"], np.float32)
    dk = np.asarray(inp["dil_k_norm"], np.float32)
    for cch in range(4):
        gains[:, cch] = np.tile(qn, 2)
    for g in range(2):
        gains[:, CID_KS + g] = np.tile(kn[1], 2)
        gains[:, CID_KW + g] = np.tile(kn[2], 2)
    for g in range(3):
        for pr in range(2):
            gains[:, CID_DIL + g * 4 + pr] = np.tile(dq[g], 2)
            gains[:, CID_DIL + g * 4 + 2 + pr] = np.tile(dk[g], 2)
    gains[:, NFM] = np.tile(kn[0], 2)
    o["gains"] = gains
    o["w1k"] = np.asarray(inp["cmp_w1_k"], np.float32)
    o["w1v"] = np.asarray(inp["cmp_w1_v"], np.float32)
    o["w2k"] = np.asarray(inp["cmp_w2_k"], np.float32)
    o["w2v"] = np.asarray(inp["cmp_w2_v"], np.float32)
    o["pek"] = np.ascontiguousarray(np.asarray(inp["cmp_pe_k"], np.float32).reshape(16, 128).T)
    o["pev"] = np.ascontiguousarray(np.asarray(inp["cmp_pe_v"], np.float32).reshape(16, 128).T)
    o["wun"] = np.asarray(inp["w_up_nsa"], np.float32)
    o["wud"] = np.asarray(inp["w_up_dil"], np.float32)
    o["wo"] = np.asarray(inp["w_o"], np.float32)
    o["wq"] = np.asarray(inp["peer_wq"], np.float32)
    sk = np.asarray(inp["peer_subkeys"], np.float32)
    skbd = np.zeros((128, 256), np.float32)
    skbd[0:64, 0:128] = sk[0].T
    skbd[64:128, 128:256] = sk[1].T
    o["skbd"] = skbd
    u = np.asarray(inp["peer_u"], np.float32)
    o["ut"] = np.ascontiguousarray(u.reshape(128, 128, 8, 128).transpose(0, 3, 2, 1)).reshape(128, 128, 1024)
    o["pv"] = np.asarray(inp["peer_v"], np.float32).reshape(128, 128, 1024)
    return o


_CACHE = {}


def kernel(**inputs):
    if "nc" not in _CACHE:
        _CACHE["nc"] = build_program()[0]
        _CACHE["consts"] = _consts()
    nc = _CACHE["nc"]
    shared = dict(_CACHE["consts"])
    shared.update(_layout_weights(inputs))
    x = np.asarray(inputs["x"], np.float32)
    in_maps = []
    for b in range(8):
        m = dict(shared)
        m["x"] = np.ascontiguousarray(x[b])
        in_maps.append(m)
    res = run_bass_kernel_spmd(nc, in_maps, core_ids=list(range(8)))
    return np.stack([np.asarray(r["out"], np.float32) for r in res.results], axis=0)
```

```python
import contextlib
import math
import numpy as np
import ml_dtypes
import concourse.bass as bass
import concourse.mybir as mybir
from concourse.bass_utils import run_bass_kernel_spmd

F32 = mybir.dt.float32
BF16 = mybir.dt.bfloat16
ALU = mybir.AluOpType
AF = mybir.ActivationFunctionType
AX = mybir.AxisListType

S = 4096
D = 1024
NT = S // 128
NB = S // 512
EPS = 1e-6


class Slot:
    __slots__ = ("name", "writers", "readers", "dcount")

    def __init__(self, name):
        self.name = name
        self.writers = {}
        self.readers = {}
        self.dcount = 0


class Op:
    __slots__ = ("eng", "fn", "deps", "signal", "value", "key", "is_dma")

    def __init__(self, eng, fn, key, is_dma=False, value=None):
        self.eng = eng
        self.fn = fn
        self.deps = []
        self.signal = False
        self.value = value
        self.key = key
        self.is_dma = is_dma


class Prog:
    COMPUTE = ("pe", "act", "dve", "pool")

    def __init__(self, nc):
        self.nc = nc
        self.ops = {e: [] for e in ("pe", "act", "dve", "pool", "sp")}
        self.slots = {}
        self.last = {}
        self.fence_deps = {e: [] for e in self.ops}
        self.n_ops = 0

    def slot(self, name):
        s = self.slots.get(name)
        if s is None:
            s = self.slots[name] = Slot(name)
        return s

    def _track(self, op, reads, writes):
        deps = op.deps
        fd = self.fence_deps[op.eng]
        if fd:
            deps.extend(fd)
            self.fence_deps[op.eng] = []
        for r in reads:
            s = self.slot(r)
            deps.extend(s.writers.values())
            s.readers[op.key] = op
        for w in writes:
            s = self.slot(w)
            deps.extend(s.readers.values())
            deps.extend(s.writers.values())
            if any(o is not op for o in s.readers.values()):
                s.writers = {op.key: op}
                s.readers = {}
            else:
                s.readers = {}
                s.writers[op.key] = op
        op.deps = [d for d in deps if d is not op and (d.key != op.key or (not op.is_dma and op.eng != "pe"))]
        self.last[op.key] = op
        self.n_ops += 1

    def add(self, eng, fn, reads=(), writes=()):
        op = Op(eng, fn, eng)
        self.ops[eng].append(op)
        self._track(op, reads, writes)
        return op

    def dma(self, fn, reads=(), writes=(), queue="sp"):
        assert len(writes) == 1
        s = self.slot(writes[0])
        s.dcount += 1
        op = Op(queue, fn, ("d", s.name), is_dma=True, value=16 * s.dcount)
        self.ops[queue].append(op)
        self._track(op, reads, writes)
        return op

    def fence(self):
        allops = list(self.last.values())
        for e in self.fence_deps:
            self.fence_deps[e] = list(allops)

    def emit(self, final_slots=()):
        nc = self.nc
        fin = Op("sp", None, "fin")
        for name in final_slots:
            fin.deps.extend(self.slot(name).writers.values())
        self.ops["sp"].append(fin)
        for e, lst in self.ops.items():
            for op in lst:
                for d in op.deps:
                    if not d.is_dma:
                        d.signal = True
        for e in self.COMPUTE:
            c = 0
            for op in self.ops[e]:
                if op.signal and not op.is_dma:
                    c += 1
                    op.value = c
        keys = list(self.COMPUTE)
        for lst in self.ops.values():
            for op in lst:
                if op.is_dma and op.key not in keys:
                    keys.append(op.key)
        self.n_sems = len(keys)
        with contextlib.ExitStack() as st:
            sems = {}
            for i, k in enumerate(keys):
                sems[k] = st.enter_context(nc.semaphore("s%d" % i))
            block = st.enter_context(nc.Block())

            def run(eng_name, eng):
                waited = {}
                for op in self.ops[eng_name]:
                    need = {}
                    for d in op.deps:
                        v = d.value
                        if v > need.get(d.key, 0):
                            need[d.key] = v
                    for k, v in need.items():
                        if waited.get(k, 0) < v:
                            eng.wait_ge(sems[k], v)
                            waited[k] = v
                    if op.fn is None:
                        continue
                    ins = op.fn(eng)
                    if op.is_dma:
                        ins.then_inc(sems[op.key], 16)
                    elif op.signal:
                        ins.then_inc(sems[op.key], 1)

            @block.sync
            def _(eng):
                run("sp", eng)

            @block.tensor
            def _(eng):
                run("pe", eng)

            @block.scalar
            def _(eng):
                run("act", eng)

            @block.vector
            def _(eng):
                run("dve", eng)

            @block.gpsimd
            def _(eng):
                run("pool", eng)


class Arena:
    def __init__(self, t, total_f32):
        self.t = t
        self.total = total_f32
        self.off = 0
        self.peak = 0

    def mark(self):
        return self.off

    def release(self, m):
        self.off = m

    def alloc(self, cols, dtype=F32):
        n32 = cols if dtype == F32 else (cols + 1) // 2
        a = self.off
        self.off += n32
        self.peak = max(self.peak, self.off)
        assert self.off <= self.total, ("SBUF arena overflow", self.off, self.total)
        v = self.t[:, a:a + n32]
        if dtype != F32:
            v = v.bitcast(dtype)[:, 0:cols]
        return v


class Ring:
    def __init__(self, items):
        self.items = list(items)
        self.i = 0

    def next(self):
        r = self.items[self.i % len(self.items)]
        self.i += 1
        return r


CID_Q = 0
CID_KC = 4
CID_VC = 5
CID_KS = 6
CID_KW = 8
CID_DIL = 10
NFM = 22
NTM = 1048
DIL_PAT = ((128, 1), (512, 4), (2048, 16))


def build_program(debug=False, stop_after=None):
    nc = bass.Bass("TRN2", target_bir_lowering=False)

    def din(name, shape, dt=F32):
        return nc.dram_tensor(name, list(shape), dt, kind="ExternalInput").ap()

    skind = "ExternalOutput" if debug else "Internal"

    def dscr(name, shape, dt):
        return nc.dram_tensor(name, list(shape), dt, kind=skind).ap()

    x_d = din("x", [S, D])
    wfm_d = din("wfm", [D, NFM * 128])
    wtm_d = din("wtm", [D, NTM])
    wmg_d = din("wmg", [D, 2048])
    g1_d = din("g1", [128, 8])
    g2_d = din("g2", [128, 8])
    gains_d = din("gains", [128, NFM + 1])
    w1k_d = din("w1k", [2048, 256])
    w1v_d = din("w1v", [2048, 256])
    w2k_d = din("w2k", [256, 64])
    w2v_d = din("w2v", [256, 64])
    pek_d = din("pek", [128, 16])
    pev_d = din("pev", [128, 16])
    wun_d = din("wun", [512, D])
    wud_d = din("wud", [256, D])
    wo_d = din("wo", [D, D])
    wq_d = din("wq", [D, D])
    skbd_d = din("skbd", [128, 256])
    ut_d = din("ut", [128, 128, 1024])
    v_d = din("pv", [128, 128, 1024])
    ident_d = din("ident", [128, 128], BF16)
    bd_d = din("bdones", [128, 128], BF16)
    rot_d = din("rotT", [128, 128], BF16)
    cos_d = din("cosT", [128, S])
    sin_d = din("sinT", [128, S])
    cosc_d = din("cosC", [128, 256])
    sinc_d = din("sinC", [128, 256])
    cmask_d = din("cmask", [2, 128, S], BF16)
    masks_d = din("masks", [128, 9, 128], BF16)
    ov_d = din("ov", [2, 128, 64], BF16)
    selmul_d = din("selmul", [S, 64])
    seladd_d = din("seladd", [S, 64])
    esel_d = din("esel", [64, S], BF16)
    out_d = nc.dram_tensor("out", [S, D], F32, kind="ExternalOutput").ap()

    qkt_s = dscr("qkt_s", [NFM, 128, S], BF16)
    vtm_s = dscr("vtm_s", [S, 1024], BF16)
    gates_s = dscr("gates_s", [S, 24], F32)
    ht_s = dscr("ht_s", [8, 128, S], BF16)
    yt_s = dscr("yt_s", [6, 128, S], BF16)
    x2_s = dscr("x2_s", [S, D], F32)
    h2t_s = dscr("h2t_s", [8, 128, S], BF16)
    utb_s = dscr("utb_s", [128, 128, 1024], BF16)
    vb_s = dscr("vb_s", [128, 128, 1024], BF16)

    TOT = 53100
    with contextlib.ExitStack() as st:
        at = st.enter_context(nc.sbuf_tensor("arena", [128, TOT], F32))
        pst = [st.enter_context(nc.psum_tensor("ps%d" % i, [128, 512], F32)) for i in range(8)]
        ps = [t.ap() for t in pst]
        psb = [t.ap().bitcast(BF16) for t in pst]
        A = Arena(at, TOT)
        P = Prog(nc)
        uid = [0]

        def nm(prefix):
            uid[0] += 1
            return "%s_%d" % (prefix, uid[0])

        def mkring(prefix, n, cols, dtype=F32):
            return Ring([(nm(prefix), A.alloc(cols, dtype)) for _ in range(n)])

        ident = A.alloc(128, BF16)
        bdones = A.alloc(128, BF16)
        rotT = A.alloc(128, BF16)
        masks = A.alloc(9 * 128, BF16).rearrange("p (a b) -> p a b", a=9)
        P.dma(lambda e: e.dma_start(out=ident, in_=ident_d), writes=["ident"])
        P.dma(lambda e: e.dma_start(out=bdones, in_=bd_d), writes=["bdones"])
        P.dma(lambda e: e.dma_start(out=rotT, in_=rot_d), writes=["rotT"])
        P.dma(lambda e: e.dma_start(out=masks, in_=masks_d), writes=["masks"])
        gains = A.alloc(NFM + 1)
        P.dma(lambda e: e.dma_start(out=gains, in_=gains_d), writes=["gains"])
        persist_mark = A.mark()

        def norm_rope(zps_name, zps, n, gain_ap, cos_ap, sin_ap, cos_slots, out_name, out_ap, R):
            sqn, sq = R["sq"].next()
            P.add("act", lambda e: e.activation(out=sq[:, 0:n], in_=zps, func=AF.Square), reads=[zps_name], writes=[sqn])
            P.add("pe", lambda e: e.matmul(ps[3][:, 0:n], lhsT=bdones, rhs=sq[:, 0:n], start=True, stop=True), reads=[sqn, "bdones"], writes=["ps3"])
            rsn, rs = R["rs"].next()
            P.add("act", lambda e: e.activation(out=rs[:, 0:n], in_=ps[3][:, 0:n], func=AF.Sqrt, scale=1.0 / 64, bias=EPS), reads=["ps3"], writes=[rsn])
            P.add("dve", lambda e: e.reciprocal(out=rs[:, 0:n], in_=rs[:, 0:n]), reads=[rsn], writes=[rsn])
            znn, zn = R["zn"].next()
            P.add("dve", lambda e: e.scalar_tensor_tensor(out=zn[:, 0:n], in0=zps, scalar=gain_ap, in1=rs[:, 0:n], op0=ALU.mult, op1=ALU.mult), reads=[zps_name, rsn, "gains"], writes=[znn])
            zbn, zb = R["zb"].next()
            P.add("act", lambda e: e.activation(out=zb[:, 0:n], in_=zn[:, 0:n], func=AF.Copy), reads=[znn], writes=[zbn])
            P.add("pe", lambda e: e.matmul(ps[4][:, 0:n], lhsT=rotT, rhs=zb[:, 0:n], start=True, stop=True), reads=[zbn, "rotT"], writes=["ps4"])
            P.add("dve", lambda e: e.tensor_tensor(out=zn[:, 0:n], in0=zn[:, 0:n], in1=cos_ap, op=ALU.mult), reads=[znn] + cos_slots, writes=[znn])
            t2n, t2 = R["t2"].next()
            P.add("dve", lambda e: e.tensor_tensor(out=t2[:, 0:n], in0=ps[4][:, 0:n], in1=sin_ap, op=ALU.mult), reads=["ps4"] + cos_slots, writes=[t2n])
            P.add("dve", lambda e: e.tensor_tensor(out=out_ap, in0=zn[:, 0:n], in1=t2[:, 0:n], op=ALU.add), reads=[znn, t2n], writes=[out_name])

        m0 = A.mark()
        stg = mkring("pstg", 2, 4096)
        stb = mkring("pstb", 2, 4096, BF16)
        k = 0
        for src, dst in ((ut_d, utb_s), (v_d, vb_s)):
            for i in range(32):
                sn, sa = stg.next()
                bn, ba = stb.next()
                P.dma(lambda e, sa=sa, src=src, i=i: e.dma_start(out=sa.rearrange("p (a b) -> p a b", a=4), in_=src[4 * i:4 * i + 4].rearrange("a p c -> p a c")), writes=[sn])
                eng = ("dve", "act", "pool")[k % 3]
                k += 1
                if eng == "act":
                    P.add("act", lambda e, sa=sa, ba=ba: e.activation(out=ba, in_=sa, func=AF.Copy), reads=[sn], writes=[bn])
                else:
                    P.add(eng, lambda e, sa=sa, ba=ba: e.tensor_copy(out=ba, in_=sa), reads=[sn], writes=[bn])
                P.dma(lambda e, ba=ba, dst=dst, i=i: e.dma_start(out=dst[4 * i:4 * i + 4].rearrange("a p c -> p a c"), in_=ba.rearrange("p (a b) -> p a b", a=4)), reads=[bn], writes=["utb_s" if dst is utb_s else "vb_s"], queue="pool")
        A.release(m0)
        P.fence()

        m0 = A.mark()
        wfm = A.alloc(8 * NFM * 128, BF16).rearrange("p (a b) -> p a b", a=8)
        wtm = A.alloc(8 * NTM, BF16).rearrange("p (a b) -> p a b", a=8)
        g1t = A.alloc(8)
        cosT = A.alloc(S)
        sinT = A.alloc(S)
        P.dma(lambda e: e.dma_start(out=g1t, in_=g1_d), writes=["g1t"])
        P.dma(lambda e: e.dma_start(out=cosT, in_=cos_d), writes=["cosT"])
        P.dma(lambda e: e.dma_start(out=sinT, in_=sin_d), writes=["sinT"])
        m1 = A.mark()
        wst = mkring("wst", 2, NFM * 128)
        for kc in range(8):
            sn, sa = wst.next()
            P.dma(lambda e, sa=sa, kc=kc: e.dma_start(out=sa, in_=wfm_d[kc * 128:(kc + 1) * 128, :]), writes=[sn])
            P.add("dve", lambda e, sa=sa, kc=kc: e.tensor_scalar(out=wfm[:, kc, :], in0=sa, scalar1=g1t[:, kc:kc + 1], scalar2=None, op0=ALU.mult), reads=[sn, "g1t"], writes=["wfm"])
        for kc in range(8):
            sn, sa = wst.next()
            P.dma(lambda e, sa=sa, kc=kc: e.dma_start(out=sa[:, 0:NTM], in_=wtm_d[kc * 128:(kc + 1) * 128, :]), writes=[sn])
            P.add("dve", lambda e, sa=sa, kc=kc: e.tensor_scalar(out=wtm[:, kc, :], in0=sa[:, 0:NTM], scalar1=g1t[:, kc:kc + 1], scalar2=None, op0=ALU.mult), reads=[sn, "g1t"], writes=["wtm"])
        A.release(m1)
        P.fence()
        xr = mkring("xt", 2, 1024)
        junk = A.alloc(1024)
        ssr = mkring("ss", 2, 1)
        hbr = mkring("hb", 2, 1024, BF16)
        hTr = mkring("hTb", 2, 8 * 512, BF16)
        R = {"sq": mkring("sq", 2, 512, BF16), "rs": mkring("rs", 2, 512), "zn": mkring("zn", 2, 512),
             "zb": mkring("zb", 2, 512, BF16), "t2": mkring("t2", 2, 512)}
        fmo = mkring("fmo", 3, 512, BF16)
        tmo = mkring("tmo", 2, 1024, BF16)
        gto = mkring("gto", 2, 24)
        fmps = Ring([1, 2])
        for b in range(NB):
            hTn, hTb_ = hTr.next()
            hTb = hTb_.rearrange("p (a b) -> p a b", a=8)
            c0 = b * 512
            for u in range(4):
                t0 = c0 + u * 128
                xn, xt = xr.next()
                P.dma(lambda e, xt=xt, t0=t0: e.dma_start(out=xt, in_=x_d[t0:t0 + 128, :]), writes=[xn])
                sn, ss = ssr.next()
                P.add("act", lambda e, xt=xt, ss=ss: e.activation(out=junk, in_=xt, func=AF.Square, accum_out=ss), reads=[xn], writes=["junk", sn])
                P.add("act", lambda e, ss=ss: e.activation(out=ss, in_=ss, func=AF.Sqrt, scale=1.0 / D, bias=EPS), reads=[sn], writes=[sn])
                P.add("dve", lambda e, ss=ss: e.reciprocal(out=ss, in_=ss), reads=[sn], writes=[sn])
                hn_, hb = hbr.next()
                P.add("dve", lambda e, xt=xt, ss=ss, hb=hb: e.tensor_scalar(out=hb, in0=xt, scalar1=ss, scalar2=None, op0=ALU.mult), reads=[xn, sn], writes=[hn_])
                for c in range(8):
                    P.add("pe", lambda e, c=c, hb=hb: e.transpose(out=psb[0][:, c * 128:(c + 1) * 128], in_=hb[:, c * 128:(c + 1) * 128], identity=ident), reads=[hn_, "ident"], writes=["ps0"])
                P.add("act", lambda e, u=u, hTb=hTb: e.activation(out=hTb[:, :, u * 128:(u + 1) * 128], in_=psb[0].rearrange("p (a b) -> p a b", a=8), func=AF.Copy), reads=["ps0"], writes=[hTn])
            P.dma(lambda e, hTb=hTb, c0=c0: e.dma_start(out=ht_s[:, :, c0:c0 + 512].rearrange("c p t -> p c t"), in_=hTb), reads=[hTn], writes=["ht_s"], queue="pool")
            for cid in range(NFM):
                pi = fmps.next()
                for kc in range(8):
                    P.add("pe", lambda e, pi=pi, kc=kc, cid=cid, hTb=hTb: e.matmul(ps[pi], lhsT=wfm[:, kc, cid * 128:(cid + 1) * 128], rhs=hTb[:, kc, :], start=(kc == 0), stop=(kc == 7)), reads=["wfm", hTn], writes=["ps%d" % pi])
                on, oa = fmo.next()
                if cid in (CID_KC, CID_VC):
                    P.add("act", lambda e, pi=pi, oa=oa: e.activation(out=oa, in_=ps[pi], func=AF.Copy), reads=["ps%d" % pi], writes=[on])
                else:
                    norm_rope("ps%d" % pi, ps[pi], 512, gains[:, cid:cid + 1], cosT[:, c0:c0 + 512], sinT[:, c0:c0 + 512], ["cosT", "sinT"], on, oa, R)
                P.dma(lambda e, oa=oa, cid=cid, c0=c0: e.dma_start(out=qkt_s[cid, :, c0:c0 + 512], in_=oa), reads=[on], writes=["qkt_s"], queue="pool")
            for u in range(4):
                t0 = c0 + u * 128
                for r, (a0, a1) in enumerate(((0, 512), (512, 1024), (1024, NTM))):
                    for kc in range(8):
                        P.add("pe", lambda e, r=r, kc=kc, u=u, a0=a0, a1=a1, hTb=hTb: e.matmul(ps[5 + r][:, 0:a1 - a0], lhsT=hTb[:, kc, u * 128:(u + 1) * 128], rhs=wtm[:, kc, a0:a1], start=(kc == 0), stop=(kc == 7)), reads=["wtm", hTn], writes=["ps%d" % (5 + r)])
                tn, ta = tmo.next()
                P.add("act", lambda e, ta=ta: e.activation(out=ta[:, 0:512], in_=ps[5], func=AF.Copy), reads=["ps5"], writes=[tn])
                P.add("dve", lambda e, ta=ta: e.tensor_copy(out=ta[:, 512:1024], in_=ps[6]), reads=["ps6"], writes=[tn])
                P.dma(lambda e, ta=ta, t0=t0: e.dma_start(out=vtm_s[t0:t0 + 128, :], in_=ta), reads=[tn], writes=["vtm_s"], queue="pool")
                gn, ga = gto.next()
                P.add("act", lambda e, ga=ga: e.activation(out=ga, in_=ps[7][:, 0:24], func=AF.Sigmoid), reads=["ps7"], writes=[gn])
                P.dma(lambda e, ga=ga, t0=t0: e.dma_start(out=gates_s[t0:t0 + 128, :], in_=ga), reads=[gn], writes=["gates_s"], queue="pool")
        A.release(m0)
        P.fence()
        if stop_after == "A":
            P.emit(final_slots=["qkt_s", "vtm_s", "gates_s", "ht_s", "utb_s", "vb_s"])
            return nc, P, A

        mB = A.mark()
        kcmp = A.alloc(2 * 256, BF16).rearrange("p (g c) -> p g c", g=2)
        vc1 = A.alloc(2 * 2 * 129, BF16).rearrange("p (t g c) -> p t g c", t=2, g=2)
        P.add("dve", lambda e: e.memset(kcmp, 0.0), writes=["kcmp"])
        P.add("dve", lambda e: e.memset(vc1, 0.0), writes=["vc1"])
        P.add("dve", lambda e: e.memset(vc1[:, 0, :, 64:65], 1.0), writes=["vc1"])
        P.add("dve", lambda e: e.memset(vc1[0:127, 1, :, 64:65], 1.0), writes=["vc1"])
        for ct in range(2):
            for g in range(2):
                P.dma(lambda e, ct=ct, g=g: e.dma_start(out=vc1[:, ct, g, 65:129], in_=ov_d[ct]), writes=["vc1"])
        mB1 = A.mark()
        x2c = A.alloc(2 * 2 * S, BF16).rearrange("p (k g t) -> p k g t", k=2, g=2)
        P.add("pool", lambda e: e.memset(x2c[:, :, :, S - 1:S], 0.0), writes=["x2c"])
        for kv in range(2):
            for g in range(2):
                P.dma(lambda e, kv=kv, g=g: e.dma_start(out=x2c[0:64, kv, g, :], in_=qkt_s[CID_KC + kv, g * 64:(g + 1) * 64, :]), reads=["qkt_s"], writes=["x2c"])
                P.dma(lambda e, kv=kv, g=g: e.dma_start(out=x2c[64:128, kv, g, 0:S - 1], in_=qkt_s[CID_KC + kv, g * 64:(g + 1) * 64, 1:S]), reads=["qkt_s"], writes=["x2c"])
        w1s = A.alloc(16 * 256).rearrange("p (a h) -> p a h", a=16)
        w1b = A.alloc(2 * 16 * 256, BF16).rearrange("p (k a h) -> p k a h", k=2, a=16)
        pes = A.alloc(32)
        peb = A.alloc(32, BF16)
        w2s = A.alloc(2 * 2 * 64).rearrange("p (k c d) -> p k c d", k=2, c=2)
        w2kd = A.alloc(2 * 128, BF16).rearrange("p (c d) -> p c d", c=2)
        w2vb = A.alloc(2 * 64, BF16).rearrange("p (c d) -> p c d", c=2)
        cosC = A.alloc(256)
        sinC = A.alloc(256)
        P.dma(lambda e: e.dma_start(out=cosC, in_=cosc_d), writes=["cosC"])
        P.dma(lambda e: e.dma_start(out=sinC, in_=sinc_d), writes=["sinC"])
        P.dma(lambda e: e.dma_start(out=pes[:, 0:16], in_=pek_d), writes=["pes"])
        P.dma(lambda e: e.dma_start(out=pes[:, 16:32], in_=pev_d), writes=["pes"])
        P.add("dve", lambda e: e.tensor_copy(out=peb, in_=pes), reads=["pes"], writes=["peb"])
        for kv, (w1d, w2d) in enumerate(((w1k_d, w2k_d), (w1v_d, w2v_d))):
            P.dma(lambda e, w1d=w1d: e.dma_start(out=w1s, in_=w1d.rearrange("(a p) h -> p a h", p=128)), writes=["w1s"])
            P.add("dve", lambda e, kv=kv: e.tensor_copy(out=w1b[:, kv], in_=w1s), reads=["w1s"], writes=["w1b"])
            P.dma(lambda e, kv=kv, w2d=w2d: e.dma_start(out=w2s[:, kv], in_=w2d.rearrange("(c p) d -> p c d", p=128)), writes=["w2s"])
        P.add("dve", lambda e: e.tensor_copy(out=w2kd[:, :, 0:64], in_=w2s[:, 0]), reads=["w2s"], writes=["w2kd"])
        P.add("dve", lambda e: e.tensor_copy(out=w2kd[:, :, 64:128], in_=w2s[:, 0]), reads=["w2s"], writes=["w2kd"])
        P.add("dve", lambda e: e.tensor_copy(out=w2vb, in_=w2s[:, 1]), reads=["w2s"], writes=["w2vb"])
        biasT = A.alloc(4)
        gT = A.alloc(2 * 256, BF16).rearrange("p (c n) -> p c n", c=2)
        RB = {"sq": mkring("sqB", 1, 256, BF16), "rs": mkring("rsB", 1, 256), "zn": mkring("znB", 1, 256),
              "zb": mkring("zbB", 1, 256, BF16), "t2": mkring("t2B", 1, 256)}
        for kv in range(2):
            for hc in range(2):
                for a in range(16):
                    P.add("pe", lambda e, kv=kv, hc=hc, a=a: e.matmul(ps[2][:, 0:1], lhsT=w1b[:, kv, a, hc * 128:(hc + 1) * 128], rhs=peb[:, kv * 16 + a:kv * 16 + a + 1], start=(a == 0), stop=(a == 15)), reads=["w1b", "peb"], writes=["ps2"])
                P.add("dve", lambda e, kv=kv, hc=hc: e.tensor_copy(out=biasT[:, kv * 2 + hc:kv * 2 + hc + 1], in_=ps[2][:, 0:1]), reads=["ps2"], writes=["biasT"])
        for kv in range(2):
            for g in range(2):
                P.add("dve", lambda e: e.memset(gT, 0.0), writes=["gT"])
                for hc in range(2):
                    pi = hc
                    for a in range(16):
                        P.add("pe", lambda e, kv=kv, g=g, hc=hc, a=a, pi=pi: e.matmul(ps[pi][:, 0:255], lhsT=w1b[:, kv, a, hc * 128:(hc + 1) * 128], rhs=x2c[:, kv, g, 2 * a:2 * a + 16 * 254 + 1:16], start=(a == 0), stop=(a == 15)), reads=["w1b", "x2c"], writes=["ps%d" % pi])
                    P.add("act", lambda e, kv=kv, hc=hc, pi=pi: e.activation(out=gT[:, hc, 0:255], in_=ps[pi][:, 0:255], func=AF.Gelu_apprx_tanh, bias=biasT[:, kv * 2 + hc:kv * 2 + hc + 1]), reads=["ps%d" % pi, "biasT"], writes=["gT"])
                if kv == 0:
                    for hc in range(2):
                        P.add("pe", lambda e, hc=hc: e.matmul(ps[5][:, 0:256], lhsT=w2kd[:, hc, :], rhs=gT[:, hc, :], start=(hc == 0), stop=(hc == 1)), reads=["w2kd", "gT"], writes=["ps5"])
                    norm_rope("ps5", ps[5][:, 0:256], 256, gains[:, NFM:NFM + 1], cosC, sinC, ["cosC", "sinC"], "kcmp", kcmp[:, g, :], RB)
                    P.add("dve", lambda e, g=g: e.memset(kcmp[:, g, 255:256], 0.0), writes=["kcmp"])
                else:
                    for ct in range(2):
                        for hc in range(2):
                            P.add("pe", lambda e, hc=hc, ct=ct: e.matmul(ps[6][:, 0:64], lhsT=gT[:, hc, ct * 128:(ct + 1) * 128], rhs=w2vb[:, hc, :], start=(hc == 0), stop=(hc == 1)), reads=["w2vb", "gT"], writes=["ps6"])
                        P.add("act", lambda e, g=g, ct=ct: e.activation(out=vc1[:, ct, g, 0:64], in_=ps[6][:, 0:64], func=AF.Copy), reads=["ps6"], writes=["vc1"])
        A.release(mB1)
        P.fence()

        Sps = Ring([0, 1])

        def banded(qb, sources, o_of_u, o_slot, er, o_clear, mul_of_kt=None, sring=None, look=1):
            P.add("dve", lambda e: e.memset(o_clear, 0.0), writes=[o_slot])
            items = []
            for si, src in enumerate(sources):
                dmax = src[6]
                for kt in range(max(0, 4 * qb - dmax), 4 * qb + 4):
                    u0 = max(0, kt - 4 * qb)
                    u1 = min(3, kt + dmax - 4 * qb)
                    if u0 <= u1:
                        items.append((si, kt, u0, u1))
            first = {}
            last = {}
            for idx, (si, kt, u0, u1) in enumerate(items):
                for u in range(u0, u1 + 1):
                    first.setdefault(u, idx)
                    last[u] = idx
            def front(idx):
                si, kt, u0, u1 = items[idx]
                qf, qs, kf, ks, vf, vs, dmax, mf = sources[si]
                n = (u1 - u0 + 1) * 128
                cq = qb * 512 + u0 * 128
                pi = (sring or Sps).next()
                P.add("pe", lambda e, pi=pi, kf=kf, kt=kt, qf=qf, cq=cq, n=n: e.matmul(ps[pi][:, 0:n], lhsT=kf(kt), rhs=qf(cq, n), start=True, stop=True), reads=list(qs) + list(ks), writes=["ps%d" % pi])
                en, ea = er.next()
                P.add("act", lambda e, pi=pi, ea=ea, n=n: e.activation(out=ea[:, 0:n], in_=ps[pi][:, 0:n], func=AF.Exp, scale=0.125), reads=["ps%d" % pi], writes=[en])
                if mul_of_kt is not None:
                    mn, ma = mul_of_kt(kt)
                    P.add("dve", lambda e, ea=ea, ma=ma, n=n, u0=u0: e.tensor_tensor(out=ea[:, 0:n], in0=ea[:, 0:n], in1=ma[:, u0 * 128:u0 * 128 + n], op=ALU.mult), reads=[en, mn], writes=[en])
                for u in range(u0, u1 + 1):
                    dl = 4 * qb + u - kt
                    mi = mf(dl)
                    lo = (u - u0) * 128
                    if mi is not None:
                        P.add("dve", lambda e, ea=ea, lo=lo, mi=mi: e.tensor_tensor(out=ea[:, lo:lo + 128], in0=ea[:, lo:lo + 128], in1=masks[:, mi, :], op=ALU.mult), reads=[en, "masks"], writes=[en])
                return en, ea

            def back(idx, en, ea):
                si, kt, u0, u1 = items[idx]
                qf, qs, kf, ks, vf, vs, dmax, mf = sources[si]
                for u in range(u0, u1 + 1):
                    lo = (u - u0) * 128
                    P.add("pe", lambda e, ea=ea, lo=lo, u=u, vf=vf, kt=kt, idx=idx: e.matmul(o_of_u(u), lhsT=ea[:, lo:lo + 128], rhs=vf(kt), start=False, stop=(last[u] == idx), skip_group_check=True), reads=[en] + list(vs), writes=[o_slot])

            pend = []
            nf = 0
            for idx in range(len(items)):
                while nf < len(items) and nf <= idx + look:
                    pend.append(front(nf))
                    nf += 1
                back(idx, *pend.pop(0))

        mC = A.mark()
        gat = A.alloc(NT * 24).rearrange("p (k c) -> p k c", k=NT)
        for i in range(4):
            P.dma(lambda e, i=i: e.dma_start(out=gat[:, 8 * i:8 * i + 8, :], in_=gates_s[1024 * i:1024 * (i + 1), :].rearrange("(k p) c -> p k c", p=128)), reads=["gates_s"], writes=["gat"])
        cmask = A.alloc(2 * S, BF16).rearrange("p (a t) -> p a t", a=2)
        P.dma(lambda e: e.dma_start(out=cmask, in_=cmask_d.rearrange("a p t -> p a t")), writes=["cmask"])
        selmul = A.alloc(NT * 64).rearrange("p (k c) -> p k c", k=NT)
        seladd = A.alloc(NT * 64).rearrange("p (k c) -> p k c", k=NT)
        for i in range(4):
            P.dma(lambda e, i=i: e.dma_start(out=selmul[:, 8 * i:8 * i + 8, :], in_=selmul_d[1024 * i:1024 * (i + 1), :].rearrange("(k p) c -> p k c", p=128)), writes=["selmul"])
            P.dma(lambda e, i=i: e.dma_start(out=seladd[:, 8 * i:8 * i + 8, :], in_=seladd_d[1024 * i:1024 * (i + 1), :].rearrange("(k p) c -> p k c", p=128)), writes=["seladd"])
        esel = A.alloc(S, BF16)
        P.dma(lambda e: e.dma_start(out=esel[0:64, :], in_=esel_d), writes=["esel"])
        Qn = A.alloc(2 * S, BF16).rearrange("p (c t) -> p c t", c=2)
        KS = A.alloc(S, BF16)
        KW = A.alloc(S, BF16)
        VS1 = A.alloc(NT * 65, BF16).rearrange("p (k c) -> p k c", k=NT)
        VW1 = A.alloc(NT * 65, BF16).rearrange("p (k c) -> p k c", k=NT)
        P.add("dve", lambda e: e.memset(VS1[:, :, 64:65], 1.0), writes=["VS1"])
        P.add("dve", lambda e: e.memset(VW1[:, :, 64:65], 1.0), writes=["VW1"])
        er = mkring("E", 3, 512, BF16)
        msbr = mkring("Msb", 2, 512, BF16)
        impacc = A.alloc(4 * 64).rearrange("p (u c) -> p u c", u=4)
        Y = A.alloc(4 * 256).rearrange("p (u c) -> p u c", u=4)
        Yb = A.alloc(4 * 256, BF16).rearrange("p (u c) -> p u c", u=4)
        rd = A.alloc(4)
        gd = A.alloc(4)
        sc = A.alloc(64)
        sct = A.alloc(64)
        m8 = A.alloc(16)
        selm = A.alloc(64, BF16)
        selT = A.alloc(512, BF16)
        yst = mkring("yst", 2, 2 * 512, BF16)

        def wmask(dl):
            return 0 if dl == 0 else (1 if dl == 4 else None)

        def smask(dl):
            return 0 if dl == 0 else None

        def finish_branch(o_views, o_slots, qb, g, j, br, first_branch):
            hh = 4 * g + j
            for u in range(4):
                P.add("dve", lambda e, u=u: e.tensor_scalar(out=rd[:, u:u + 1], in0=o_views[u][:, 64:65], scalar1=1e-30, scalar2=None, op0=ALU.max), reads=o_slots, writes=["rd"])
            P.add("dve", lambda e: e.reciprocal(out=rd, in_=rd), reads=["rd"], writes=["rd"])
            P.add("dve", lambda e: e.tensor_tensor(out=gd, in0=rd, in1=gat[:, 4 * qb:4 * qb + 4, hh * 3 + br], op=ALU.mult), reads=["rd", "gat"], writes=["gd"])
            for u in range(4):
                if first_branch:
                    P.add("dve", lambda e, u=u: e.tensor_scalar(out=Y[:, u, j * 64:(j + 1) * 64], in0=o_views[u][:, 0:64], scalar1=gd[:, u:u + 1], scalar2=None, op0=ALU.mult), reads=o_slots + ["gd"], writes=["Y"])
                else:
                    P.add("dve", lambda e, u=u: e.scalar_tensor_tensor(out=Y[:, u, j * 64:(j + 1) * 64], in0=o_views[u][:, 0:64], scalar=gd[:, u:u + 1], in1=Y[:, u, j * 64:(j + 1) * 64], op0=ALU.mult, op1=ALU.add), reads=o_slots + ["gd", "Y"], writes=["Y"])

        for g in range(2):
            for c in range(2):
                P.dma(lambda e, c=c, g=g: e.dma_start(out=Qn[:, c, :], in_=qkt_s[CID_Q + 2 * g + c]), reads=["qkt_s"], writes=["Qn"])
            P.dma(lambda e, g=g: e.dma_start(out=KS, in_=qkt_s[CID_KS + g]), reads=["qkt_s"], writes=["KS"])
            P.dma(lambda e, g=g: e.dma_start(out=KW, in_=qkt_s[CID_KW + g]), reads=["qkt_s"], writes=["KW"])
            for i in range(4):
                P.dma(lambda e, i=i, g=g: e.dma_start(out=VS1[:, 8 * i:8 * i + 8, 0:64], in_=vtm_s[1024 * i:1024 * (i + 1), g * 64:(g + 1) * 64].rearrange("(k p) c -> p k c", p=128)), reads=["vtm_s"], writes=["VS1"])
                P.dma(lambda e, i=i, g=g: e.dma_start(out=VW1[:, 8 * i:8 * i + 8, 0:64], in_=vtm_s[1024 * i:1024 * (i + 1), 128 + g * 64:128 + (g + 1) * 64].rearrange("(k p) c -> p k c", p=128)), reads=["vtm_s"], writes=["VW1"])
            for qb in range(NB):
                c0 = qb * 512
                cts = [0, 1] if qb >= 4 else [0]
                for j in range(4):
                    base = 64 * (j % 2)
                    cj = j // 2
                    oa = ps[2].rearrange("p (u c) -> p u c", u=2)
                    ob = ps[3].rearrange("p (u c) -> p u c", u=2)
                    ov_ = [oa[:, 0, 0:129], oa[:, 1, 0:129], ob[:, 0, 0:129], ob[:, 1, 0:129]]
                    osl = ["ps2", "ps2", "ps3", "ps3"]
                    P.add("dve", lambda e: e.memset(ps[2], 0.0), writes=["ps2"])
                    P.add("dve", lambda e: e.memset(ps[3], 0.0), writes=["ps3"])
                    for ct in cts:
                        pi = Sps.next()
                        P.add("pe", lambda e, pi=pi, ct=ct, base=base, cj=cj, g=g, c0=c0: e.matmul(ps[pi], lhsT=kcmp[base:base + 64, g, ct * 128:(ct + 1) * 128], rhs=Qn[base:base + 64, cj, c0:c0 + 512], start=True, stop=True), reads=["kcmp", "Qn"], writes=["ps%d" % pi])
                        en, ea = er.next()
                        P.add("act", lambda e, pi=pi, ea=ea: e.activation(out=ea, in_=ps[pi], func=AF.Exp, scale=0.125), reads=["ps%d" % pi], writes=[en])
                        P.add("dve", lambda e, ea=ea, ct=ct, c0=c0: e.tensor_tensor(out=ea, in0=ea, in1=cmask[:, ct, c0:c0 + 512], op=ALU.mult), reads=[en, "cmask"], writes=[en])
                        for u in range(4):
                            P.add("pe", lambda e, ea=ea, u=u, ct=ct, g=g, ov_=ov_, sp_=(ct == cts[-1]): e.matmul(ov_[u], lhsT=ea[:, u * 128:(u + 1) * 128], rhs=vc1[:, ct, g, :], start=False, stop=sp_, skip_group_check=True), reads=[en, "vc1"], writes=[osl[u]])
                    finish_branch(ov_, ["ps2", "ps3"], qb, g, j, 0, True)
                    for u in range(4):
                        if j == 0:
                            P.add("dve", lambda e, u=u: e.tensor_scalar(out=impacc[:, u, :], in0=ov_[u][:, 65:129], scalar1=rd[:, u:u + 1], scalar2=None, op0=ALU.mult), reads=["ps2", "ps3", "rd"], writes=["impacc"])
                        else:
                            P.add("dve", lambda e, u=u: e.scalar_tensor_tensor(out=impacc[:, u, :], in0=ov_[u][:, 65:129], scalar=rd[:, u:u + 1], in1=impacc[:, u, :], op0=ALU.mult, op1=ALU.add), reads=["ps2", "ps3", "rd", "impacc"], writes=["impacc"])
                for u in range(4):
                    tt = 4 * qb + u
                    P.add("dve", lambda e, u=u, tt=tt: e.tensor_tensor(out=sc, in0=impacc[:, u, :], in1=selmul[:, tt, :], op=ALU.mult), reads=["impacc", "selmul"], writes=["sc"])
                    P.add("dve", lambda e, tt=tt: e.tensor_tensor(out=sc, in0=sc, in1=seladd[:, tt, :], op=ALU.add), reads=["sc", "seladd"], writes=["sc"])
                    P.add("dve", lambda e: e.max(out=m8[:, 0:8], in_=sc), reads=["sc"], writes=["m8"])
                    P.add("dve", lambda e: e.match_replace(out=sct, in_to_replace=m8[:, 0:8], in_values=sc, imm_value=-1e30), reads=["sc", "m8"], writes=["sct"])
                    P.add("dve", lambda e: e.max(out=m8[:, 8:16], in_=sct), reads=["sct"], writes=["m8"])
                    P.add("dve", lambda e: e.tensor_scalar(out=selm, in0=sc, scalar1=m8[:, 15:16], scalar2=None, op0=ALU.is_ge), reads=["sc", "m8"], writes=["selm"])
                    P.add("pe", lambda e: e.transpose(out=psb[7][0:64, 0:128], in_=selm, identity=ident), reads=["selm", "ident"], writes=["ps7"])
                    P.add("act", lambda e, u=u: e.activation(out=selT[0:64, u * 128:(u + 1) * 128], in_=psb[7][0:64, 0:128], func=AF.Copy), reads=["ps7"], writes=["selT"])
                nkt = 4 * qb + 4
                for j in range(4):
                    base = 64 * (j % 2)
                    cj = j // 2
                    o5 = ps[5].rearrange("p (u c) -> p u c", u=4)
                    o6 = ps[6].rearrange("p (u c) -> p u c", u=4)
                    qf = lambda cq, n, base=base, cj=cj: Qn[base:base + 64, cj, cq:cq + n]

                    def mul_of_kt(kt):
                        mn, ma = msbr.next()
                        P.add("pe", lambda e, kt=kt: e.matmul(ps[4], lhsT=esel[0:64, kt * 128:(kt + 1) * 128], rhs=selT[0:64, :], start=True, stop=True), reads=["esel", "selT"], writes=["ps4"])
                        P.add("act", lambda e, ma=ma: e.activation(out=ma, in_=ps[4], func=AF.Copy), reads=["ps4"], writes=[mn])
                        return mn, ma

                    banded(qb, [(qf, ["Qn"], lambda kt, base=base: KS[base:base + 64, kt * 128:(kt + 1) * 128], ["KS"], lambda kt: VS1[:, kt, :], ["VS1"], 64, smask)],
                           lambda u: o5[:, u, 0:65], "ps5", er, ps[5], mul_of_kt=mul_of_kt)
                    finish_branch([o5[:, u, :] for u in range(4)], ["ps5"], qb, g, j, 1, False)
                    banded(qb, [(qf, ["Qn"], lambda kt, base=base: KW[base:base + 64, kt * 128:(kt + 1) * 128], ["KW"], lambda kt: VW1[:, kt, :], ["VW1"], 4, wmask)],
                           lambda u: o6[:, u, 0:65], "ps6", er, ps[6])
                    finish_branch([o6[:, u, :] for u in range(4)], ["ps6"], qb, g, j, 2, False)
                P.add("act", lambda e: e.activation(out=Yb, in_=Y, func=AF.Copy), reads=["Y"], writes=["Yb"])
                ysn, ys_ = yst.next()
                ys = ys_.rearrange("p (c t) -> p c t", c=2)
                for u in range(4):
                    for c in range(2):
                        P.add("pe", lambda e, u=u, c=c: e.transpose(out=psb[7][:, c * 128:(c + 1) * 128], in_=Yb[:, u, c * 128:(c + 1) * 128], identity=ident), reads=["Yb", "ident"], writes=["ps7"])
                    P.add("act", lambda e, u=u, ys=ys: e.activation(out=ys[:, :, u * 128:(u + 1) * 128], in_=psb[7][:, 0:256].rearrange("p (c t) -> p c t", c=2), func=AF.Copy), reads=["ps7"], writes=[ysn])
                P.dma(lambda e, ys=ys, g=g, c0=c0: e.dma_start(out=yt_s[2 * g:2 * g + 2, :, c0:c0 + 512].rearrange("c p t -> p c t"), in_=ys), reads=[ysn], writes=["yt_s"], queue="pool")
        A.release(mB)
        P.fence()
        if stop_after == "C":
            P.emit(final_slots=["yt_s"])
            return nc, P, A

        mD = A.mark()
        DQ = A.alloc(3 * S, BF16).rearrange("p (g t) -> p g t", g=3)
        DK = A.alloc(3 * S, BF16).rearrange("p (g t) -> p g t", g=3)
        DV = A.alloc(6 * NT * 65, BF16).rearrange("p (a k c) -> p a k c", a=6, k=NT)
        P.add("dve", lambda e: e.memset(DV[:, :, :, 64:65], 1.0), writes=["DV"])
        er = mkring("Ed", 5, 512, BF16)
        SpsD = Ring([0, 1, 2, 3])
        Yd = A.alloc(4 * 128).rearrange("p (u c) -> p u c", u=4)
        Ydb = A.alloc(4 * 128, BF16).rearrange("p (u c) -> p u c", u=4)
        rdd = A.alloc(4)
        ydst = mkring("ydst", 2, 512, BF16)
        ops_ring = Ring([5, 6])

        def dmask_fn(gi):
            d = DIL_PAT[gi][1]
            if d == 1:
                return lambda dl: 0 if dl == 0 else 2
            b = 3 if d == 4 else 6
            return lambda dl, b=b, d=d: (b + 1) if dl == 0 else ((b + 2) if dl == d else b)

        for pr in range(2):
            for gi in range(3):
                P.dma(lambda e, gi=gi, pr=pr: e.dma_start(out=DQ[:, gi, :], in_=qkt_s[CID_DIL + gi * 4 + pr]), reads=["qkt_s"], writes=["DQ"])
                P.dma(lambda e, gi=gi, pr=pr: e.dma_start(out=DK[:, gi, :], in_=qkt_s[CID_DIL + gi * 4 + 2 + pr]), reads=["qkt_s"], writes=["DK"])
                for hh in range(2):
                    col = 256 + (gi * 4 + pr * 2 + hh) * 64
                    for i in range(4):
                        P.dma(lambda e, i=i, gi=gi, hh=hh, col=col: e.dma_start(out=DV[:, gi * 2 + hh, 8 * i:8 * i + 8, 0:64], in_=vtm_s[1024 * i:1024 * (i + 1), col:col + 64].rearrange("(k p) c -> p k c", p=128)), reads=["vtm_s"], writes=["DV"])
            for qb in range(NB):
                c0 = qb * 512
                for hh in range(2):
                    base = 64 * hh
                    opi = ops_ring.next()
                    ov4 = ps[opi].rearrange("p (u c) -> p u c", u=4)
                    srcs = []
                    for gi in range(3):
                        srcs.append((lambda cq, n, gi=gi, base=base: DQ[base:base + 64, gi, cq:cq + n], ["DQ"],
                                     lambda kt, gi=gi, base=base: DK[base:base + 64, gi, kt * 128:(kt + 1) * 128], ["DK"],
                                     lambda kt, gi=gi, hh=hh: DV[:, gi * 2 + hh, kt, :], ["DV"], DIL_PAT[gi][1], dmask_fn(gi)))
                    banded(qb, srcs, lambda u, ov4=ov4: ov4[:, u, 0:65], "ps%d" % opi, er, ps[opi], sring=SpsD, look=3)
                    P.add("dve", lambda e, ov4=ov4: e.reciprocal(out=rdd, in_=ov4[:, :, 64]), reads=["ps%d" % opi], writes=["rdd"])
                    for u in range(4):
                        P.add("dve", lambda e, u=u, ov4=ov4, hh=hh: e.tensor_scalar(out=Ydb[:, u, hh * 64:(hh + 1) * 64], in0=ov4[:, u, 0:64], scalar1=rdd[:, u:u + 1], scalar2=None, op0=ALU.mult), reads=["ps%d" % opi, "rdd"], writes=["Ydb"])
                ysn, ys = ydst.next()
                for u in range(4):
                    P.add("pe", lambda e, u=u: e.transpose(out=psb[7][:, 0:128], in_=Ydb[:, u, :], identity=ident), reads=["Ydb", "ident"], writes=["ps7"])
                    P.add("act", lambda e, u=u, ys=ys: e.activation(out=ys[:, u * 128:(u + 1) * 128], in_=psb[7][:, 0:128], func=AF.Copy), reads=["ps7"], writes=[ysn])
                P.dma(lambda e, ys=ys, pr=pr, c0=c0: e.dma_start(out=yt_s[4 + pr, :, c0:c0 + 512], in_=ys), reads=[ysn], writes=["yt_s"], queue="pool")
        A.release(mD)
        P.fence()
        if stop_after == "D":
            P.emit(final_slots=["yt_s"])
            return nc, P, A

        mE = A.mark()
        wun = A.alloc(4 * D, BF16).rearrange("p (a b) -> p a b", a=4)
        wud = A.alloc(2 * D, BF16).rearrange("p (a b) -> p a b", a=2)
        wmg = A.alloc(8 * 2048, BF16).rearrange("p (a b) -> p a b", a=8)
        wo = A.alloc(8 * D, BF16).rearrange("p (a b) -> p a b", a=8)
        g1tE = A.alloc(8)
        P.dma(lambda e: e.dma_start(out=g1tE, in_=g1_d), writes=["g1tE"])
        mE1 = A.mark()
        wst = mkring("wstE", 2, 2048)
        for a in range(4):
            sn, sa = wst.next()
            P.dma(lambda e, sa=sa, a=a: e.dma_start(out=sa[:, 0:D], in_=wun_d[a * 128:(a + 1) * 128, :]), writes=[sn])
            P.add("dve", lambda e, sa=sa, a=a: e.tensor_copy(out=wun[:, a, :], in_=sa[:, 0:D]), reads=[sn], writes=["wun"])
        for a in range(2):
            sn, sa = wst.next()
            P.dma(lambda e, sa=sa, a=a: e.dma_start(out=sa[:, 0:D], in_=wud_d[a * 128:(a + 1) * 128, :]), writes=[sn])
            P.add("dve", lambda e, sa=sa, a=a: e.tensor_copy(out=wud[:, a, :], in_=sa[:, 0:D]), reads=[sn], writes=["wud"])
        for a in range(8):
            sn, sa = wst.next()
            P.dma(lambda e, sa=sa, a=a: e.dma_start(out=sa[:, 0:D], in_=wo_d[a * 128:(a + 1) * 128, :]), writes=[sn])
            P.add("dve", lambda e, sa=sa, a=a: e.tensor_copy(out=wo[:, a, :], in_=sa[:, 0:D]), reads=[sn], writes=["wo"])
        for a in range(8):
            sn, sa = wst.next()
            P.dma(lambda e, sa=sa, a=a: e.dma_start(out=sa, in_=wmg_d[a * 128:(a + 1) * 128, :]), writes=[sn])
            P.add("dve", lambda e, sa=sa, a=a: e.tensor_scalar(out=wmg[:, a, :], in0=sa, scalar1=g1tE[:, a:a + 1], scalar2=None, op0=ALU.mult), reads=[sn, "g1tE"], writes=["wmg"])
        A.release(mE1)
        P.fence()
        ytr = mkring("ytE", 2, 6 * 512, BF16)
        htr = mkring("htE", 2, 8 * 512, BF16)
        sg0 = A.alloc(512)
        sg1 = A.alloc(512)
        t1 = A.alloc(512)
        mT = A.alloc(8 * 512, BF16).rearrange("p (a b) -> p a b", a=8)
        xr = mkring("xtE", 2, 1024)
        x2r = mkring("x2E", 2, 1024)
        junkE = A.alloc(1024)
        ssr = mkring("ssE", 2, 1)
        hbr = mkring("hbE", 2, 1024, BF16)
        h2r = mkring("h2E", 2, 8 * 512, BF16)
        for b in range(NB):
            c0 = b * 512
            yn, ya_ = ytr.next()
            ya = ya_.rearrange("p (a b) -> p a b", a=6)
            hn, ha_ = htr.next()
            ha = ha_.rearrange("p (a b) -> p a b", a=8)
            P.dma(lambda e, ya=ya, c0=c0: e.dma_start(out=ya, in_=yt_s[:, :, c0:c0 + 512].rearrange("c p t -> p c t")), reads=["yt_s"], writes=[yn])
            P.dma(lambda e, ha=ha, c0=c0: e.dma_start(out=ha, in_=ht_s[:, :, c0:c0 + 512].rearrange("c p t -> p c t")), reads=["ht_s"], writes=[hn])
            for dc in range(8):
                dsl = slice(dc * 128, (dc + 1) * 128)
                for a in range(4):
                    P.add("pe", lambda e, a=a, dsl=dsl, ya=ya: e.matmul(ps[0], lhsT=wun[:, a, dsl], rhs=ya[:, a, :], start=(a == 0), stop=(a == 3)), reads=["wun", yn], writes=["ps0"])
                for a in range(2):
                    P.add("pe", lambda e, a=a, dsl=dsl, ya=ya: e.matmul(ps[1], lhsT=wud[:, a, dsl], rhs=ya[:, 4 + a, :], start=(a == 0), stop=(a == 1)), reads=["wud", yn], writes=["ps1"])
                for hf in range(2):
                    for a in range(8):
                        P.add("pe", lambda e, a=a, hf=hf, dc=dc, ha=ha: e.matmul(ps[2 + hf], lhsT=wmg[:, a, hf * 1024 + dc * 128:hf * 1024 + (dc + 1) * 128], rhs=ha[:, a, :], start=(a == 0), stop=(a == 7)), reads=["wmg", hn], writes=["ps%d" % (2 + hf)])
                P.add("act", lambda e: e.activation(out=sg0, in_=ps[2], func=AF.Sigmoid), reads=["ps2"], writes=["sg0"])
                P.add("act", lambda e: e.activation(out=sg1, in_=ps[3], func=AF.Sigmoid), reads=["ps3"], writes=["sg1"])
                P.add("dve", lambda e: e.tensor_tensor(out=t1, in0=sg0, in1=ps[0], op=ALU.mult), reads=["sg0", "ps0"], writes=["t1"])
                P.add("dve", lambda e: e.tensor_tensor(out=sg1, in0=sg1, in1=ps[1], op=ALU.mult), reads=["sg1", "ps1"], writes=["sg1"])
                P.add("dve", lambda e, dc=dc: e.tensor_tensor(out=mT[:, dc, :], in0=t1, in1=sg1, op=ALU.add), reads=["t1", "sg1"], writes=["mT"])
            h2n, h2_ = h2r.next()
            h2 = h2_.rearrange("p (a b) -> p a b", a=8)
            for u in range(4):
                t0 = c0 + u * 128
                for hf in range(2):
                    for a in range(8):
                        P.add("pe", lambda e, a=a, hf=hf, u=u: e.matmul(ps[4 + hf], lhsT=mT[:, a, u * 128:(u + 1) * 128], rhs=wo[:, a, hf * 512:(hf + 1) * 512], start=(a == 0), stop=(a == 7)), reads=["wo", "mT"], writes=["ps%d" % (4 + hf)])
                xn, xt = xr.next()
                P.dma(lambda e, xt=xt, t0=t0: e.dma_start(out=xt, in_=x_d[t0:t0 + 128, :]), writes=[xn])
                x2n, x2 = x2r.next()
                for hf in range(2):
                    P.add("dve", lambda e, hf=hf, xt=xt, x2=x2: e.tensor_tensor(out=x2[:, hf * 512:(hf + 1) * 512], in0=xt[:, hf * 512:(hf + 1) * 512], in1=ps[4 + hf], op=ALU.add), reads=[xn, "ps%d" % (4 + hf)], writes=[x2n])
                P.dma(lambda e, x2=x2, t0=t0: e.dma_start(out=x2_s[t0:t0 + 128, :], in_=x2), reads=[x2n], writes=["x2_s"], queue="pool")
                sn, ss = ssr.next()
                P.add("act", lambda e, x2=x2, ss=ss: e.activation(out=junkE, in_=x2, func=AF.Square, accum_out=ss), reads=[x2n], writes=["junkE", sn])
                P.add("act", lambda e, ss=ss: e.activation(out=ss, in_=ss, func=AF.Sqrt, scale=1.0 / D, bias=EPS), reads=[sn], writes=[sn])
                P.add("dve", lambda e, ss=ss: e.reciprocal(out=ss, in_=ss), reads=[sn], writes=[sn])
                hbn, hb = hbr.next()
                P.add("dve", lambda e, x2=x2, ss=ss, hb=hb: e.tensor_scalar(out=hb, in0=x2, scalar1=ss, scalar2=None, op0=ALU.mult), reads=[x2n, sn], writes=[hbn])
                for c in range(8):
                    P.add("pe", lambda e, c=c, hb=hb: e.transpose(out=psb[6][:, c * 128:(c + 1) * 128], in_=hb[:, c * 128:(c + 1) * 128], identity=ident), reads=[hbn, "ident"], writes=["ps6"])
                P.add("act", lambda e, u=u, h2=h2: e.activation(out=h2[:, :, u * 128:(u + 1) * 128], in_=psb[6].rearrange("p (a b) -> p a b", a=8), func=AF.Copy), reads=["ps6"], writes=[h2n])
            P.dma(lambda e, h2=h2, c0=c0: e.dma_start(out=h2t_s[:, :, c0:c0 + 512].rearrange("c p t -> p c t"), in_=h2), reads=[h2n], writes=["h2t_s"], queue="pool")
        A.release(mE)
        P.fence()
        if stop_after == "E":
            P.emit(final_slots=["x2_s", "h2t_s"])
            return nc, P, A

        wq = A.alloc(8 * D, BF16).rearrange("p (a b) -> p a b", a=8)
        g2t = A.alloc(8)
        skbd = A.alloc(256, BF16)
        P.dma(lambda e: e.dma_start(out=g2t, in_=g2_d), writes=["g2t"])
        mF1 = A.mark()
        wst = mkring("wstF", 2, 1024)
        for a in range(8):
            sn, sa = wst.next()
            P.dma(lambda e, sa=sa, a=a: e.dma_start(out=sa, in_=wq_d[a * 128:(a + 1) * 128, :]), writes=[sn])
            P.add("dve", lambda e, sa=sa, a=a: e.tensor_scalar(out=wq[:, a, :], in0=sa, scalar1=g2t[:, a:a + 1], scalar2=None, op0=ALU.mult), reads=[sn, "g2t"], writes=["wq"])
        sn, sa = wst.next()
        P.dma(lambda e, sa=sa: e.dma_start(out=sa[:, 0:256], in_=skbd_d), writes=[sn])
        P.add("dve", lambda e, sa=sa: e.tensor_copy(out=skbd, in_=sa[:, 0:256]), reads=[sn], writes=["skbd"])
        A.release(mF1)
        P.fence()
        hg = A.alloc(8 * 256, BF16).rearrange("p (a b) -> p a b", a=8)
        qtg = A.alloc(8 * 256, BF16).rearrange("p (a b) -> p a b", a=8)
        ssb = A.alloc(8 * 256).rearrange("p (h n) -> p h n", h=8)
        stmp = A.alloc(256)
        T16 = A.alloc(256).rearrange("p (a b) -> p a b", a=16)
        negm = A.alloc(16)
        ab = A.alloc(8 * 256).rearrange("p (h n) -> p h n", h=8)
        abt = A.alloc(256).rearrange("p (a b) -> p a b", a=16)
        Pc = A.alloc(8 * 256).rearrange("p (h n) -> p h n", h=8)
        P16 = A.alloc(128).rearrange("p (h n) -> p h n", h=8)
        Zs = A.alloc(8)
        a2 = A.alloc(8 * 128).rearrange("p (h n) -> p h n", h=8)
        a2t = A.alloc(128).rearrange("p (h n) -> p h n", h=8)
        pdr = mkring("Pd", 2, 1024)
        whr = mkring("Wh", 16, 1024, BF16)
        WT = A.alloc(128 * 256, BF16).rearrange("p (e t) -> p e t", e=128)
        usr = mkring("Us", 4, 1024, BF16)
        vsr = mkring("Vs", 4, 1024, BF16)
        ggr = mkring("Gg", 2, 256, BF16)
        gtr = mkring("GT", 2, 256, BF16)
        xr = mkring("xtG", 2, 1024)
        wps = Ring([2, 3])
        aps = Ring([0, 1])
        NG = S // 256
        for G in range(NG):
            g0 = G * 256
            P.dma(lambda e, g0=g0: e.dma_start(out=hg, in_=h2t_s[:, :, g0:g0 + 256].rearrange("c p t -> p c t")), reads=["h2t_s"], writes=["hg"])
            for h in range(8):
                pi = aps.next()
                for a in range(8):
                    P.add("pe", lambda e, pi=pi, a=a, h=h: e.matmul(ps[pi][:, 0:256], lhsT=wq[:, a, h * 128:(h + 1) * 128], rhs=hg[:, a, :], start=(a == 0), stop=(a == 7)), reads=["wq", "hg"], writes=["ps%d" % pi])
                P.add("act", lambda e, pi=pi, h=h: e.activation(out=qtg[:, h, :], in_=ps[pi][:, 0:256], func=AF.Copy), reads=["ps%d" % pi], writes=["qtg"])
            for w in range(2):
                for h in range(8):
                    pi = 4 + h // 2
                    P.add("pe", lambda e, pi=pi, h=h, w=w: e.matmul(ps[pi][:, (h % 2) * 256:(h % 2) * 256 + 256], lhsT=qtg[:, h, w * 128:(w + 1) * 128], rhs=skbd, start=True, stop=True), reads=["qtg", "skbd"], writes=["ps%d" % pi])
                for k in range(4):
                    P.add("act", lambda e, k=k: e.activation(out=ssb[:, 2 * k:2 * k + 2, :], in_=ps[4 + k].rearrange("p (a n) -> p a n", a=2), func=AF.Copy), reads=["ps%d" % (4 + k)], writes=["ssb"])
                for hc in range(16):
                    h, c = hc // 2, hc % 2
                    src = ssb[:, h, c * 128:(c + 1) * 128]
                    P.add("dve", lambda e, src=src, hc=hc: e.max(out=T16[:, hc, 0:8], in_=src), reads=["ssb"], writes=["T16"])
                    P.add("dve", lambda e, src=src, hc=hc: e.match_replace(out=stmp[:, 0:128], in_to_replace=T16[:, hc, 0:8], in_values=src, imm_value=-1e30), reads=["ssb", "T16"], writes=["stmp"])
                    P.add("dve", lambda e, hc=hc: e.max(out=T16[:, hc, 8:16], in_=stmp[:, 0:128]), reads=["stmp"], writes=["T16"])
                P.add("dve", lambda e: e.tensor_scalar(out=negm, in0=T16[:, :, 0], scalar1=-1.0, scalar2=None, op0=ALU.mult), reads=["T16"], writes=["negm"])
                for hc in range(16):
                    h, c = hc // 2, hc % 2
                    P.add("act", lambda e, h=h, c=c, hc=hc: e.activation(out=ab[:, h, c * 128:(c + 1) * 128], in_=ssb[:, h, c * 128:(c + 1) * 128], func=AF.Exp, bias=negm[:, hc:hc + 1]), reads=["ssb", "negm"], writes=["ab"])
                    P.add("act", lambda e, hc=hc: e.activation(out=abt[:, hc, :], in_=T16[:, hc, :], func=AF.Exp, bias=negm[:, hc:hc + 1]), reads=["T16", "negm"], writes=["abt"])
                abt4 = abt.rearrange("p (h c) n -> p h c n", c=2)

                def cand_top(at_ap, outname):
                    P.add("dve", lambda e: e.tensor_tensor(out=Pc.rearrange("p h (i j) -> p h i j", i=16), in0=at_ap.unsqueeze(3).to_broadcast([128, 8, 16, 16]), in1=abt4[:, :, 1, :].unsqueeze(2).to_broadcast([128, 8, 16, 16]), op=ALU.mult), reads=["abt", "a2t"], writes=["Pc"])
                    for h in range(8):
                        P.add("dve", lambda e, h=h: e.max(out=P16[:, h, 0:8], in_=Pc[:, h, :]), reads=["Pc"], writes=["P16"])
                        P.add("dve", lambda e, h=h: e.match_replace(out=stmp, in_to_replace=P16[:, h, 0:8], in_values=Pc[:, h, :], imm_value=-1.0), reads=["Pc", "P16"], writes=["stmp"])
                        P.add("dve", lambda e, h=h: e.max(out=P16[:, h, 8:16], in_=stmp), reads=["stmp"], writes=["P16"])

                cand_top(abt4[:, :, 0, :], "P16")
                P.add("dve", lambda e: e.reduce_sum(out=Zs, in_=P16, axis=AX.X), reads=["P16"], writes=["Zs"])
                P.add("dve", lambda e: e.reciprocal(out=Zs, in_=Zs), reads=["Zs"], writes=["Zs"])
                P.add("dve", lambda e: e.tensor_tensor(out=a2, in0=ab[:, :, 0:128], in1=Zs.unsqueeze(2).to_broadcast([128, 8, 128]), op=ALU.mult), reads=["ab", "Zs"], writes=["a2"])
                P.add("dve", lambda e: e.tensor_tensor(out=a2t, in0=abt4[:, :, 0, :], in1=Zs.unsqueeze(2).to_broadcast([128, 8, 16]), op=ALU.mult), reads=["abt", "Zs"], writes=["a2t"])
                cand_top(a2t, "P16")
                for eb in range(16):
                    whs = []
                    for h in range(8):
                        pn, pd = pdr.next()
                        P.add("dve", lambda e, pd=pd, h=h, eb=eb: e.tensor_tensor(out=pd.rearrange("p (i j) -> p i j", i=8), in0=a2[:, h, eb * 8:(eb + 1) * 8].unsqueeze(2).to_broadcast([128, 8, 128]), in1=ab[:, h, 128:256].unsqueeze(1).to_broadcast([128, 8, 128]), op=ALU.mult), reads=["a2", "ab"], writes=[pn])
                        wn, wh = whr.next()
                        P.add("dve", lambda e, pd=pd, wh=wh, h=h: e.scalar_tensor_tensor(out=wh, in0=pd, scalar=P16[:, h, 15:16], in1=pd, op0=ALU.is_ge, op1=ALU.mult), reads=[pn, "P16"], writes=[wn])
                        whs.append((wn, wh))
                    for q4 in range(2):
                        pi = wps.next()
                        for ci in range(4):
                            cc = q4 * 4 + ci
                            for h in range(8):
                                wn, wh = whs[h]
                                P.add("pe", lambda e, pi=pi, ci=ci, cc=cc, wh=wh, h=h: e.matmul(ps[pi][:, ci * 128:(ci + 1) * 128], lhsT=wh[:, cc * 128:(cc + 1) * 128], rhs=ident, start=(h == 0), stop=(h == 7)), reads=[wn, "ident"], writes=["ps%d" % pi])
                        e1 = eb * 8 + q4 * 4
                        P.add("act", lambda e, pi=pi, e1=e1, w=w: e.activation(out=WT[:, e1:e1 + 4, w * 128:(w + 1) * 128], in_=ps[pi].rearrange("p (a t) -> p a t", a=4), func=AF.Copy), reads=["ps%d" % pi], writes=["WT"])
            def gfront(e1):
                un, us_ = usr.next()
                us = us_.rearrange("p (a b) -> p a b", a=8)
                vn, vs = vsr.next()
                P.dma(lambda e, us_=us_, e1=e1: e.dma_start(out=us_, in_=utb_s[e1]), reads=["utb_s"], writes=[un])
                P.dma(lambda e, vs=vs, e1=e1: e.dma_start(out=vs, in_=vb_s[e1]), reads=["vb_s"], writes=[vn])
                pi = aps.next()
                for a in range(8):
                    P.add("pe", lambda e, pi=pi, a=a, us=us: e.matmul(ps[pi][:, 0:256], lhsT=us[:, a, :], rhs=hg[:, a, :], start=(a == 0), stop=(a == 7)), reads=[un, "hg"], writes=["ps%d" % pi])
                gn, gg = ggr.next()
                P.add("act", lambda e, pi=pi, gg=gg: e.activation(out=gg, in_=ps[pi][:, 0:256], func=AF.Gelu_apprx_tanh), reads=["ps%d" % pi], writes=[gn])
                tn, gt = gtr.next()
                P.add("dve", lambda e, gg=gg, gt=gt, e1=e1: e.tensor_tensor(out=gt, in0=gg, in1=WT[:, e1, :], op=ALU.mult), reads=[gn, "WT"], writes=[tn])
                return tn, gt, vn, vs

            def gback(e1, tn, gt, vn, vs):
                for w in range(2):
                    for hf in range(2):
                        pj = 4 + w * 2 + hf
                        P.add("pe", lambda e, pj=pj, w=w, hf=hf, gt=gt, vs=vs, e1=e1: e.matmul(ps[pj], lhsT=gt[:, w * 128:(w + 1) * 128], rhs=vs[:, hf * 512:(hf + 1) * 512], start=(e1 == 0), stop=(e1 == 127)), reads=[tn, vn], writes=["ps%d" % pj])

            pend = gfront(0)
            for e1 in range(128):
                nxt = gfront(e1 + 1) if e1 + 1 < 128 else None
                gback(e1, *pend)
                pend = nxt
            for w in range(2):
                t0 = g0 + w * 128
                xn, xt = xr.next()
                P.dma(lambda e, xt=xt, t0=t0: e.dma_start(out=xt, in_=x2_s[t0:t0 + 128, :]), reads=["x2_s"], writes=[xn])
                for hf in range(2):
                    pj = 4 + w * 2 + hf
                    P.add("dve", lambda e, xt=xt, hf=hf, pj=pj: e.tensor_tensor(out=xt[:, hf * 512:(hf + 1) * 512], in0=xt[:, hf * 512:(hf + 1) * 512], in1=ps[pj], op=ALU.add), reads=[xn, "ps%d" % pj], writes=[xn])
                P.dma(lambda e, xt=xt, t0=t0: e.dma_start(out=out_d[t0:t0 + 128, :], in_=xt), reads=[xn], writes=["out"], queue="pool")
        P.emit(final_slots=["out"])
    return nc, P, A


def _consts():
    bf = ml_dtypes.bfloat16
    c = {}
    c["ident"] = np.eye(128, dtype=np.float32).astype(bf)
    bd = np.zeros((128, 128), np.float32)
    bd[:64, :64] = 1
    bd[64:, 64:] = 1
    c["bdones"] = bd.astype(bf)
    Rm = np.zeros((128, 128), np.float32)
    for hb in (0, 64):
        for i in range(8):
            Rm[hb + i, hb + i + 8] = -1.0
            Rm[hb + i + 8, hb + i] = 1.0
    c["rotT"] = np.ascontiguousarray(Rm.T).astype(bf)
    half = 8
    inv = (500000.0 ** (-(np.arange(half, dtype=np.float32) * 2.0 / 16))).astype(np.float32)

    def tables(pos):
        ang = pos.astype(np.float32)[None, :] * inv[:, None]
        cs = np.ones((128, pos.shape[0]), np.float32)
        sn = np.zeros((128, pos.shape[0]), np.float32)
        for hb in (0, 64):
            cs[hb:hb + 8] = np.cos(ang)
            cs[hb + 8:hb + 16] = np.cos(ang)
            sn[hb:hb + 8] = np.sin(ang)
            sn[hb + 8:hb + 16] = np.sin(ang)
        return cs, sn

    c["cosT"], c["sinT"] = tables(np.arange(S))
    pc = np.arange(256) * 16 + 31
    c["cosC"], c["sinC"] = tables(pc)
    t = np.arange(S)
    cm = np.zeros((2, 128, S), np.float32)
    for ct in range(2):
        cidx = ct * 128 + np.arange(128)
        cm[ct] = ((cidx[:, None] * 16 + 31 <= t[None, :]) & (cidx[:, None] < 255)).astype(np.float32)
    c["cmask"] = cm.astype(bf)
    k = np.arange(128)[:, None]
    q = np.arange(128)[None, :]
    ms = np.zeros((128, 9, 128), np.float32)
    ms[:, 0] = k <= q
    ms[:, 1] = k > q
    ms[:, 2] = k >= q
    for b, d in ((3, 4), (6, 16)):
        res = ((q - k) % d) == 0
        ms[:, b] = res
        ms[:, b + 1] = res & (k <= q)
        ms[:, b + 2] = res & (k >= q)
    c["masks"] = ms.astype(bf)
    n_cmp = 255
    s0 = np.arange(n_cmp) * 16
    s1 = s0 + 32
    b0 = np.arange(64) * 64
    b1 = b0 + 64
    ovm = np.clip(np.minimum(s1[:, None], b1[None, :]) - np.maximum(s0[:, None], b0[None, :]), 0, None) / 32.0
    ovp = np.zeros((256, 64), np.float32)
    ovp[:255] = ovm
    c["ov"] = ovp.reshape(2, 128, 64).astype(bf)
    blk = np.arange(64)[None, :]
    cur = (t // 64)[:, None]
    forced = (blk == 0) | (blk == cur) | (blk == cur - 1)
    causal = blk * 64 <= t[:, None]
    c["selmul"] = (causal & ~forced).astype(np.float32)
    c["seladd"] = np.where(forced, 1e3, np.where(causal, 0.0, -1.0)).astype(np.float32)
    es = np.zeros((64, S), np.float32)
    es[(t // 64), t] = 1.0
    c["esel"] = es.astype(bf)
    return c


def _layout_weights(inp):
    w_in = np.asarray(inp["w_in"], np.float32)
    o = {}
    cols = []
    for cch in range(4):
        cols.append(np.arange(cch * 128, (cch + 1) * 128))
    cols.append(np.arange(512, 640))
    cols.append(np.arange(640, 768))
    for base in (768, 1024):
        for g in range(2):
            cc = base + g * 64 + np.arange(64)
            cols.append(np.concatenate([cc, cc]))
    for g in range(3):
        for r in range(2):
            for pr in range(2):
                cols.append(1304 + g * 768 + r * 256 + pr * 128 + np.arange(128))
    fm_cols = np.concatenate(cols)
    assert fm_cols.shape[0] == NFM * 128
    o["wfm"] = np.ascontiguousarray(w_in[:, fm_cols])
    tm_cols = np.concatenate([np.arange(896, 1024), np.arange(1152, 1280)] +
                             [1304 + g * 768 + 512 + np.arange(256) for g in range(3)] + [np.arange(1280, 1304)])
    assert tm_cols.shape[0] == NTM
    o["wtm"] = np.ascontiguousarray(w_in[:, tm_cols])
    o["wmg"] = np.ascontiguousarray(w_in[:, 3608:5656])
    o["g1"] = np.ascontiguousarray(np.asarray(inp["norm1_g"], np.float32).reshape(8, 128).T)
    o["g2"] = np.ascontiguousarray(np.asarray(inp["norm2_g"], np.float32).reshape(8, 128).T)
    gains = np.ones((128, NFM + 1), np.float32)
    qn = np.asarray(inp["nsa_q_norm"], np.float32)
    kn = np.asarray(inp["nsa_k_norm"], np.float32)
    dq = np.asarray(inp["dil_q_norm"], np.float32)
    dk = np.asarray(inp["dil_k_norm"], np.float32)
    for cch in range(4):
        gains[:, cch] = np.tile(qn, 2)
    for g in range(2):
        gains[:, CID_KS + g] = np.tile(kn[1], 2)
        gains[:, CID_KW + g] = np.tile(kn[2], 2)
    for g in range(3):
        for pr in range(2):
            gains[:, CID_DIL + g * 4 + pr] = np.tile(dq[g], 2)
            gains[:, CID_DIL + g * 4 + 2 + pr] = np.tile(dk[g], 2)
    gains[:, NFM] = np.tile(kn[0], 2)
    o["gains"] = gains
    o["w1k"] = np.asarray(inp["cmp_w1_k"], np.float32)
    o["w1v"] = np.asarray(inp["cmp_w1_v"], np.float32)
    o["w2k"] = np.asarray(inp["cmp_w2_k"], np.float32)
    o["w2v"] = np.asarray(inp["cmp_w2_v"], np.float32)
    o["pek"] = np.ascontiguousarray(np.asarray(inp["cmp_pe_k"], np.float32).reshape(16, 128).T)
    o["pev"] = np.ascontiguousarray(np.asarray(inp["cmp_pe_v"], np.float32).reshape(16, 128).T)
    o["wun"] = np.asarray(inp["w_up_nsa"], np.float32)
    o["wud"] = np.asarray(inp["w_up_dil"], np.float32)
    o["wo"] = np.asarray(inp["w_o"], np.float32)
    o["wq"] = np.asarray(inp["peer_wq"], np.float32)
    sk = np.asarray(inp["peer_subkeys"], np.float32)
    skbd = np.zeros((128, 256), np.float32)
    skbd[0:64, 0:128] = sk[0].T
    skbd[64:128, 128:256] = sk[1].T
    o["skbd"] = skbd
    u = np.asarray(inp["peer_u"], np.float32)
    o["ut"] = np.ascontiguousarray(u.reshape(128, 128, 8, 128).transpose(0, 3, 2, 1)).reshape(128, 128, 1024)
    o["pv"] = np.asarray(inp["peer_v"], np.float32).reshape(128, 128, 1024)
    return o


_CACHE = {}


def kernel(**inputs):
    if "nc" not in _CACHE:
        _CACHE["nc"] = build_program()[0]
        _CACHE["consts"] = _consts()
    nc = _CACHE["nc"]
    shared = dict(_CACHE["consts"])
    shared.update(_layout_weights(inputs))
    x = np.asarray(inputs["x"], np.float32)
    in_maps = []
    for b in range(8):
        m = dict(shared)
        m["x"] = np.ascontiguousarray(x[b])
        in_maps.append(m)
    res = run_bass_kernel_spmd(nc, in_maps, core_ids=list(range(8)))
    return np.stack([np.asarray(r["out"], np.float32) for r in res.results], axis=0)
```
